# Optimizing a Trainium2 kernel written in Bass

```python
import math
import jax, jax.numpy as jnp
from jax import lax
import numpy as np

D_MODEL = 2048
BATCH = 1
SEQ = 16384
DEPTH = 1

HEAD_DIM = 64
N_Q_HEADS = 16
N_KV_HEADS = 2
Q_PER_KV = N_Q_HEADS // N_KV_HEADS
D_ATTN = N_Q_HEADS * HEAD_DIM
D_KV = N_KV_HEADS * HEAD_DIM
D_CONV = D_MODEL - D_ATTN
D_MIX = D_ATTN + D_CONV
D_IN = D_ATTN + 2 * D_KV + 2 * D_CONV
WINDOW = 128
BLOCK = 128
CONV_WIDTH = 31
N_BUCKETS = 32
MAX_DISTANCE = 128
N_EXPERTS = 32
TOP_K = 4
D_FF = D_MODEL
SWIGLU_LIMIT = 7.0
SWIGLU_ALPHA = 1.702
MOE_BLOCK = 128
EPS = 1e-6
NEG_INF = -1e30

kernel_name = "hymba_swa_conformer_moe_adaln"


def rms_norm(x, g):
    xf = x.astype(jnp.float32)
    y = xf * lax.rsqrt(jnp.mean(xf * xf, axis=-1, keepdims=True) + EPS)
    return (y * g.astype(jnp.float32)).astype(x.dtype)


def layer_norm(x, g, b):
    xf = x.astype(jnp.float32)
    mu = jnp.mean(xf, axis=-1, keepdims=True)
    var = jnp.mean(jnp.square(xf - mu), axis=-1, keepdims=True)
    y = (xf - mu) * lax.rsqrt(var + EPS)
    return (y * g.astype(jnp.float32) + b.astype(jnp.float32)).astype(x.dtype)


def t5_causal_bucket(dist):
    max_exact = N_BUCKETS // 2
    d = jnp.maximum(dist, 0)
    log_ratio = jnp.log(jnp.maximum(d, max_exact).astype(jnp.float32) / max_exact)
    large = max_exact + (log_ratio / math.log(MAX_DISTANCE / max_exact)
                         * (N_BUCKETS - max_exact)).astype(jnp.int32)
    large = jnp.minimum(large, N_BUCKETS - 1)
    return jnp.where(d < max_exact, d, large)


def sliding_window_attention(q, k, v, g_q, g_k, sinks, rel_bias):
    B, T = q.shape[0], q.shape[1]
    nb = T // BLOCK
    q = rms_norm(q, g_q)
    k = rms_norm(k, g_k)
    qb = q.reshape(B, nb, BLOCK, N_KV_HEADS, Q_PER_KV, HEAD_DIM)

    def with_prev(t):
        tb = t.reshape(B, nb, BLOCK, N_KV_HEADS, HEAD_DIM)
        prev = jnp.pad(tb[:, :-1], ((0, 0), (1, 0), (0, 0), (0, 0), (0, 0)))
        return jnp.concatenate([prev, tb], axis=2)

    kb = with_prev(k)
    vb = with_prev(v)
    scores = jnp.einsum('bnqhgd,bnkhd->bnhgqk', qb, kb).astype(jnp.float32) * (HEAD_DIM ** -0.5)

    q_local = jnp.arange(BLOCK, dtype=jnp.int32) + BLOCK
    k_local = jnp.arange(2 * BLOCK, dtype=jnp.int32)
    dist = q_local[:, None] - k_local[None, :]
    band = (dist >= 0) & (dist < WINDOW)
    has_prev = (jnp.arange(nb)[:, None, None] > 0) | (k_local >= BLOCK)[None, None, :]
    valid = band[None] & has_prev

    bias = rel_bias.astype(jnp.float32)[t5_causal_bucket(dist)]
    bias = jnp.transpose(bias, (2, 0, 1)).reshape(N_KV_HEADS, Q_PER_KV, BLOCK, 2 * BLOCK)
    scores = jnp.where(valid[None, :, None, None], scores + bias[None, None], NEG_INF)

    s = sinks.astype(jnp.float32).reshape(N_KV_HEADS, Q_PER_KV)[None, None, :, :, None, None]
    m = jnp.maximum(jnp.max(scores, axis=-1, keepdims=True), s)
    p = jnp.exp(scores - m)
    p = p / (jnp.sum(p, axis=-1, keepdims=True) + jnp.exp(s - m))
    out = jnp.einsum('bnhgqk,bnkhd->bnqhgd', p.astype(vb.dtype), vb)
    return out.reshape(B, T, N_Q_HEADS * HEAD_DIM)


def conformer_conv(u, w_dw, b_dw, ln_g, ln_b):
    a, gate = jnp.split(u, 2, axis=-1)
    h = a * jax.nn.sigmoid(gate)
    h = lax.conv_general_dilated(
        h, w_dw[:, None, :].astype(h.dtype), window_strides=(1,),
        padding=[(CONV_WIDTH - 1, 0)],
        dimension_numbers=('NWC', 'WIO', 'NWC'),
        feature_group_count=D_CONV) + b_dw
    h = layer_norm(h, ln_g, ln_b)
    return jax.nn.silu(h)


def moe_ffn(h, w_router, b_router, w_gate, b_gate, w_up, b_up, w_down, b_down):
    B, T, D = h.shape
    n_tok = B * T
    xt = h.reshape(n_tok, D)
    logits = (xt @ w_router + b_router).astype(jnp.float32)
    top_val, top_idx = lax.top_k(logits, TOP_K)
    top_w = jax.nn.softmax(top_val, axis=-1)

    n_assign = n_tok * TOP_K
    expert_flat = top_idx.reshape(-1).astype(jnp.int32)
    token_flat = jnp.repeat(jnp.arange(n_tok, dtype=jnp.int32), TOP_K)
    weight_flat = top_w.reshape(-1)
    order = jnp.argsort(expert_flat)
    exp_sorted = expert_flat[order]
    tok_sorted = token_flat[order]
    w_sorted = weight_flat[order]

    counts = jnp.bincount(expert_flat, length=N_EXPERTS)
    padded = ((counts + MOE_BLOCK - 1) // MOE_BLOCK) * MOE_BLOCK
    start = jnp.cumsum(counts) - counts
    pend = jnp.cumsum(padded)
    pstart = pend - padded
    dest = pstart[exp_sorted] + (jnp.arange(n_assign, dtype=jnp.int32) - start[exp_sorted])

    n_rows = ((n_assign + MOE_BLOCK - 1) // MOE_BLOCK) * MOE_BLOCK + N_EXPERTS * MOE_BLOCK
    n_blocks = n_rows // MOE_BLOCK
    row_tok = jnp.full((n_rows,), n_tok, jnp.int32).at[dest].set(tok_sorted)
    row_w = jnp.zeros((n_rows,), jnp.float32).at[dest].set(w_sorted)
    block_expert = jnp.minimum(
        jnp.searchsorted(pend, jnp.arange(n_blocks, dtype=jnp.int32) * MOE_BLOCK, side='right'),
        N_EXPERTS - 1).astype(jnp.int32)

    x_pad = jnp.concatenate([xt, jnp.zeros((1, D), xt.dtype)], axis=0)

    def expert_block(args):
        tok, e = args
        xb = x_pad[tok]
        g = xb @ w_gate[e] + b_gate[e]
        lin = xb @ w_up[e] + b_up[e]
        g = jnp.minimum(g, SWIGLU_LIMIT)
        lin = jnp.clip(lin, -SWIGLU_LIMIT, SWIGLU_LIMIT)
        act = g * jax.nn.sigmoid(SWIGLU_ALPHA * g) * (lin + 1.0)
        return act @ w_down[e] + b_down[e]

    out_rows = lax.map(expert_block, (row_tok.reshape(n_blocks, MOE_BLOCK), block_expert))
    out_rows = out_rows.reshape(n_rows, D) * row_w[:, None].astype(out_rows.dtype)
    y = jnp.zeros((n_tok + 1, D), out_rows.dtype).at[row_tok].add(out_rows)[:n_tok]
    return y.reshape(B, T, D)


def setup_inputs(seed: int = 0) -> dict:
    key = jax.random.key(seed)
    ks = jax.random.split(key, 32)
    f32 = jnp.float32
    L, D, E, F = DEPTH, D_MODEL, N_EXPERTS, D_FF

    def nrm(k, shape, scale):
        return jax.random.normal(k, shape, f32) * scale

    return {
        "x": nrm(ks[0], (BATCH, SEQ, D), 1.0),
        "c": nrm(ks[1], (BATCH, D), 1.0),
        "w_ada": nrm(ks[2], (L, D, 6 * D), 0.5 * D ** -0.5),
        "b_ada": nrm(ks[3], (L, 6 * D), 0.02),
        "g_norm1": 1.0 + nrm(ks[4], (L, D), 0.02),
        "w_in": nrm(ks[5], (L, D, D_IN), D ** -0.5),
        "b_in": nrm(ks[6], (L, D_IN), 0.02),
        "g_q": 1.0 + nrm(ks[7], (L, HEAD_DIM), 0.02),
        "g_k": 1.0 + nrm(ks[8], (L, HEAD_DIM), 0.02),
        "sinks": nrm(ks[9], (L, N_Q_HEADS), 1.0),
        "rel_bias": nrm(ks[10], (N_BUCKETS, N_Q_HEADS), 0.5),
        "w_dw": nrm(ks[11], (L, CONV_WIDTH, D_CONV), CONV_WIDTH ** -0.5),
        "b_dw": nrm(ks[12], (L, D_CONV), 0.02),
        "ln_g": 1.0 + nrm(ks[13], (L, D_CONV), 0.02),
        "ln_b": nrm(ks[14], (L, D_CONV), 0.02),
        "g_out_attn": 1.0 + nrm(ks[15], (L, D_ATTN), 0.02),
        "g_out_conv": 1.0 + nrm(ks[16], (L, D_CONV), 0.02),
        "w_out": nrm(ks[17], (L, D_MIX, D), D_MIX ** -0.5),
        "b_out": nrm(ks[18], (L, D), 0.02),
        "g_norm2": 1.0 + nrm(ks[19], (L, D), 0.02),
        "w_router": nrm(ks[20], (L, D, E), D ** -0.5),
        "b_router": nrm(ks[21], (L, E), 0.01),
        "w_gate": nrm(ks[22], (L, E, D, F), D ** -0.5),
        "b_gate": nrm(ks[23], (L, E, F), 0.02),
        "w_up": nrm(ks[24], (L, E, D, F), D ** -0.5),
        "b_up": nrm(ks[25], (L, E, F), 0.02),
        "w_down": nrm(ks[26], (L, E, F, D), F ** -0.5),
        "b_down": nrm(ks[27], (L, E, D), 0.02),
    }


def reference(x, c, w_ada, b_ada, g_norm1, w_in, b_in, g_q, g_k, sinks, rel_bias,
              w_dw, b_dw, ln_g, ln_b, g_out_attn, g_out_conv, w_out, b_out, g_norm2,
              w_router, b_router, w_gate, b_gate, w_up, b_up, w_down, b_down):
    B, T, _ = x.shape
    for l in range(DEPTH):
        mod = jax.nn.silu(c) @ w_ada[l] + b_ada[l]
        shift1, scale1, gate1, shift2, scale2, gate2 = jnp.split(mod[:, None, :], 6, axis=-1)

        h = rms_norm(x, g_norm1[l]) * (1.0 + scale1) + shift1
        u = h @ w_in[l] + b_in[l]
        q = u[..., :D_ATTN].reshape(B, T, N_Q_HEADS, HEAD_DIM)
        k = u[..., D_ATTN:D_ATTN + D_KV].reshape(B, T, N_KV_HEADS, HEAD_DIM)
        v = u[..., D_ATTN + D_KV:D_ATTN + 2 * D_KV].reshape(B, T, N_KV_HEADS, HEAD_DIM)
        u_conv = u[..., D_ATTN + 2 * D_KV:]

        y_attn = sliding_window_attention(q, k, v, g_q[l], g_k[l], sinks[l], rel_bias)
        y_conv = conformer_conv(u_conv, w_dw[l], b_dw[l], ln_g[l], ln_b[l])
        mixed = jnp.concatenate([rms_norm(y_attn, g_out_attn[l]),
                                 rms_norm(y_conv, g_out_conv[l])], axis=-1)
        x = x + gate1 * (mixed @ w_out[l] + b_out[l])

        h2 = rms_norm(x, g_norm2[l]) * (1.0 + scale2) + shift2
        y_moe = moe_ffn(h2, w_router[l], b_router[l], w_gate[l], b_gate[l],
                        w_up[l], b_up[l], w_down[l], b_down[l])
        x = x + gate2 * y_moe
    return x
```

```python
import math
from contextlib import ExitStack
import numpy as np
import concourse.bass as bass
import concourse.mybir as mybir
from concourse.bass_utils import run_bass_kernel_spmd

F32 = mybir.dt.float32
BF16 = mybir.dt.bfloat16
ALU = mybir.AluOpType
AF = mybir.ActivationFunctionType
AX = mybir.AxisListType

D = 2048
NCORE = 8
TOK = 2048
NT = 16
E = 32
EPS = 1e-6
PV_C, PV_G1, PV_BIN, PV_GQ, PV_GK = 0, 16, 32, 58, 59
PV_WDW, PV_BDW, PV_LNG, PV_LNB, PV_GOC = 60, 308, 316, 324, 332
PV_BG, PV_BU, PV_WR = 340, 852, 1364
NPV = 1364 + 512
RV_GOA, RV_BOUT, RV_G2, RV_BR, RV_SINK = 0, 1024, 3072, 5120, 5152
NRV = 5168


class Trk:
    def __init__(s, nc):
        s.nc = nc
        s.eng = dict(pe=nc.tensor, act=nc.scalar, dve=nc.vector, pool=nc.gpsimd, sp=nc.sync)
        s.csem = {e: nc.alloc_semaphore("c_" + e) for e in s.eng}
        s.ND = 24
        s.dsem = [nc.alloc_semaphore(f"dq{i}") for i in range(s.ND)]
        s.reset_state()

    def reset_state(s):
        s.cnt = {e: 0 for e in s.eng}
        s.seen = {c: {p: 0 for p in s.eng} for c in s.eng}
        s.dval = [0] * s.ND
        s.dseen = {c: [0] * s.ND for c in s.eng}
        s.drr = 0
        s.lastw = {}
        s.readers = {}

    def _wait(s, c, tok):
        if tok[0] == 'e':
            _, p, seq = tok
            if p == c and p == 'pe':
                return
            if s.seen[c][p] >= seq:
                return
            s.eng[c].wait_ge(s.csem[p], seq)
            s.seen[c][p] = seq
        else:
            _, k, val = tok
            if s.dseen[c][k] >= val:
                return
            s.eng[c].wait_ge(s.dsem[k], val)
            s.dseen[c][k] = val

    def _deps(s, c, r, w):
        for b in r:
            t = s.lastw.get(b)
            if t:
                s._wait(c, t)
        for b in w:
            t = s.lastw.get(b)
            if t:
                s._wait(c, t)
            rd = s.readers.get(b)
            if rd:
                for p, seq in rd[0].items():
                    if p != c:
                        s._wait(c, ('e', p, seq))
                for t in rd[1]:
                    s._wait(c, t)

    def _record(s, tok, r, w):
        for b in r:
            rd = s.readers.setdefault(b, [{}, []])
            if tok[0] == 'e':
                rd[0][tok[1]] = tok[2]
            else:
                rd[1].append(tok)
        for b in w:
            s.lastw[b] = tok
            s.readers[b] = [{}, []]

    def op(s, c, fn, r=(), w=()):
        s._deps(c, r, w)
        ins = fn(s.eng[c])
        s.cnt[c] += 1
        ins.then_inc(s.csem[c], 1)
        s._record(('e', c, s.cnt[c]), r, w)

    def dma(s, c, fn, r=(), w=()):
        s._deps(c, r, w)
        k = s.drr
        s.drr = (s.drr + 1) % s.ND
        if s.dval[k] > 0:
            s._wait(c, ('d', k, s.dval[k]))
        ins = fn(s.eng[c])
        s.dval[k] += 16
        ins.then_inc(s.dsem[k], 16)
        s._record(('d', k, s.dval[k]), r, w)

    def barrier(s):
        for c in s.eng:
            for p in s.eng:
                if s.cnt[p] > 0:
                    s._wait(c, ('e', p, s.cnt[p]))
            for k in range(s.ND):
                if s.dval[k] > 0:
                    s._wait(c, ('d', k, s.dval[k]))
        s.lastw = {}
        s.readers = {}


def build_nc(dbg=None):
    dbg = dbg or {}
    STOP = dbg.get('stop')
    HALVES = dbg.get('halves', [0, 1])
    GROUPS = dbg.get('groups', [0, 1])
    NEXP = dbg.get('n_exp', E)
    TAPS = dbg.get('taps', False)
    nc = bass.Bass("TRN2", target_bir_lowering=False)

    def din(name, shape, dt=F32):
        return nc.dram_tensor(name, list(shape), dt, kind="ExternalInput").ap()

    xh_d = din("xh", [17 * 128, D])
    hv_d = din("hv", [128, 1])
    pv_d = din("pv", [128, NPV])
    rv_d = din("rv", [128, NRV])
    bada_d = din("bada", [1, 6 * D])
    wada_d = din("wada", [D, 6 * D])
    win_d = din("win", [D, 3328])
    wout_d = din("wout", [D, D])
    bias_d = din("biast", [128, 4096])
    wg_d = din("wg", [NEXP, D, D])
    wu_d = din("wu", [NEXP, D, D])
    wd_d = din("wd", [NEXP, D, D])
    bd_d = din("bd", [NEXP, D])
    identf_d = din("identf", [128, 128])
    blk_d = din("blk", [128, 128])
    out_d = nc.dram_tensor("out", [TOK, D], F32, kind="ExternalOutput").ap()
    x1_d = nc.dram_tensor("x1s", [TOK, D], F32, kind=("ExternalOutput" if TAPS else "Internal")).ap()

    t = Trk(nc)

    def tap(name, tens, shape, dt, key):
        if not TAPS:
            return
        dd = nc.dram_tensor("tap_" + name, list(shape), dt, kind="ExternalOutput").ap()
        t.dma('sp', lambda e: e.dma_start(out=dd, in_=tens[:]), r=[key], w=['tap_' + name])
    _uid = [0]

    def SB(name, shape, dt):
        _uid[0] += 1
        return nc.alloc_sbuf_tensor(f"{name}_s{_uid[0]}", shape, dt)

    def SBT(name, shape, dt):
        _uid[0] += 1
        return nc.sbuf_tensor(f"{name}_s{_uid[0]}", shape, dt)

    pv = SB("pv", [128, NPV], F32)
    hv = SB("hv", [128, 1], F32)
    identf = SB("identf", [128, 128], F32)
    identb = SB("identb", [128, 128], BF16)
    blkb = SB("blkb", [128, 128], BF16)
    onesf = SB("onesf", [128, 128], F32)
    sT = SB("sT", [128, 16], BF16)
    modT = SB("modT", [128, 2, 16], F32)
    A1 = SB("A1", [128, 16], F32)
    bc = [SB(f"bc{i}", [128, D], F32) for i in range(4)]
    GB1 = SB("GB1", [128, D], F32)
    goa = SB("goa", [128, 1024], F32)
    brt = SB("brt", [128, 32], F32)
    esink = SB("esink", [128, 16], F32)
    ebt = SB("ebt", [128, 4096], BF16)
    ebt0 = SB("ebt0", [128, 2048], BF16)
    mixedT = SB("mixedT", [128, 16, 1024], BF16)
    wrb = SB("wrb", [128, 512], BF16)
    ssq = SB("ssq", [128, 1], F32)
    rstd = SB("rstd", [128, 1], F32)

    ps = [nc.alloc_psum_tensor(f"ps{i}", [128, 512], F32) for i in range(6)]
    psT = [nc.alloc_psum_tensor(f"psT{i}", [128, 1024], BF16) for i in range(2)]

    def P(i):
        return f"ps{i}"

    t.dma('sp', lambda e: e.dma_start(out=pv[:], in_=pv_d), w=['pv'])
    t.dma('sp', lambda e: e.dma_start(out=hv[:], in_=hv_d), w=['hv'])
    t.dma('sp', lambda e: e.dma_start(out=identf[:], in_=identf_d), w=['identf'])
    t.dma('pool', lambda e: e.dma_start(out=identb[:], in_=identf_d), w=['identb'])
    t.dma('pool', lambda e: e.dma_start(out=blkb[:], in_=blk_d), w=['blkb'])
    t.op('dve', lambda e: e.memset(onesf[:], 1.0), w=['onesf'])
    t.op('dve', lambda e: e.tensor_copy(out=wrb[:], in_=pv[:, PV_WR:PV_WR + 512]), r=['pv'], w=['wrb'])
    t.op('act', lambda e: e.activation(out=sT[:], in_=pv[:, PV_C:PV_C + 16], func=AF.Silu), r=['pv'], w=['sT'])

    with ExitStack() as _es:
        rvt = _es.enter_context(SBT("rvt", [128, NRV], F32))
        biasf = _es.enter_context(SBT("biasf", [128, 4096], F32))
        wr0 = _es.enter_context(SBT("wr0", [128, 16, 512], BF16))
        wr1 = _es.enter_context(SBT("wr1", [128, 16, 512], BF16))
        brow0 = _es.enter_context(SBT("brow0", [1, 512], F32))
        brow1 = _es.enter_context(SBT("brow1", [1, 512], F32))
        mrow = _es.enter_context(SBT("mrow", [1, 512], F32))
        wr = [wr0, wr1]
        brow = [brow0, brow1]
        t.dma('sp', lambda e: e.dma_start(out=rvt[:], in_=rv_d), w=['rvt'])
        t.dma('sp', lambda e: e.dma_start(out=biasf[:], in_=bias_d), w=['biasf'])
        t.op('act', lambda e: e.activation(out=ebt[:], in_=biasf[:], func=AF.Exp), r=['biasf'], w=['ebt'])
        t.op('dve', lambda e: e.tensor_scalar(out=ebt0[:], in0=ebt[:, 0:2048], scalar1=hv[:, 0:1], scalar2=None,
                                              op0=ALU.mult), r=['ebt', 'hv'], w=['ebt0'])
        t.op('act', lambda e: e.activation(out=esink[:], in_=rvt[:, RV_SINK:RV_SINK + 16], func=AF.Exp),
             r=['rvt'], w=['esink'])
        t.op('dve', lambda e: e.tensor_copy(out=goa[:], in_=rvt[:, RV_GOA:RV_GOA + 1024]), r=['rvt'], w=['goa'])
        t.op('dve', lambda e: e.tensor_copy(out=brt[:], in_=rvt[:, RV_BR:RV_BR + 32]), r=['rvt'], w=['brt'])

        wada_v = wada_d.rearrange("(kc p) f -> p kc f", p=128)
        for n in range(24):
            b = n % 2
            t.dma('pool', lambda e: e.dma_start(out=wr[b][:], in_=wada_v[:, :, n * 512:(n + 1) * 512]), w=[f'wr{b}'])
            t.dma('sp', lambda e: e.dma_start(out=brow[b][:], in_=bada_d[0:1, n * 512:(n + 1) * 512]), w=[f'brow{b}'])
            for kc in range(16):
                t.op('pe', lambda e: e.matmul(ps[0][0:1, :], lhsT=sT[:, kc:kc + 1], rhs=wr[b][:, kc, :],
                                              start=(kc == 0), stop=(kc == 15)), r=['sT', f'wr{b}'], w=[P(0)])
            t.op('dve', lambda e: e.tensor_tensor(out=mrow[:], in0=ps[0][0:1, :], in1=brow[b][:], op=ALU.add),
                 r=[P(0), f'brow{b}'], w=['mrow'])
            which, q = n // 4, n % 4
            if which in (0, 1):
                for j in range(4):
                    t.op('pe', lambda e: e.matmul(ps[1][:, j:j + 1], lhsT=mrow[0:1, j * 128:(j + 1) * 128],
                                                  rhs=onesf[0:1, 0:1], start=True, stop=True), r=['mrow', 'onesf'], w=[P(1)])
                t.op('dve', lambda e: e.tensor_copy(out=modT[:, which, q * 4:(q + 1) * 4], in_=ps[1][:, 0:4]),
                     r=[P(1)], w=['modT'])
            else:
                bi = {2: 0, 3: 1, 4: 2, 5: 3}[which]
                t.op('pe', lambda e: e.matmul(ps[2][:, :], lhsT=onesf[0:1, 0:128], rhs=mrow[0:1, :],
                                              start=True, stop=True), r=['mrow', 'onesf'], w=[P(2)])
                t.op('act', lambda e: e.activation(out=bc[bi][:, q * 512:(q + 1) * 512], in_=ps[2][:, :], func=AF.Identity),
                     r=[P(2)], w=[f'bc{bi}'])
        t.op('dve', lambda e: e.scalar_tensor_tensor(out=A1[:], in0=modT[:, 1, :], scalar=1.0, in1=pv[:, PV_G1:PV_G1 + 16],
                                                     op0=ALU.add, op1=ALU.mult), r=['modT', 'pv'], w=['A1'])
        t.op('dve', lambda e: e.scalar_tensor_tensor(out=bc[2][:], in0=bc[2][:], scalar=1.0, in1=rvt[:, RV_G2:RV_G2 + D],
                                                     op0=ALU.add, op1=ALU.mult), r=['bc2', 'rvt'], w=['bc2'])
        t.op('dve', lambda e: e.tensor_tensor(out=GB1[:], in0=bc[0][:], in1=rvt[:, RV_BOUT:RV_BOUT + D], op=ALU.mult),
             r=['bc0', 'rvt'], w=['GB1'])
        tap('modT', modT, [128, 2, 16], F32, 'modT')
        tap('A1', A1, [128, 16], F32, 'A1')
        tap('bc0', bc[0], [128, D], F32, 'bc0')
        tap('bc2', bc[2], [128, D], F32, 'bc2')
        tap('ebt', ebt, [128, 4096], BF16, 'ebt')
        t.barrier()
        if STOP == 'ada':
            return nc
    G1, B2bc, A2bc, G2 = bc

    win_v = win_d.rearrange("(kc p) f -> p kc f", p=128)
    wout_v = wout_d.rearrange("(kc p) f -> p kc f", p=128)

    def rsqrt_into(dst, src_ap, scale, r, w):
        t.op('act', lambda e: e.activation(out=dst, in_=src_ap, func=AF.Sqrt, bias=EPS, scale=scale), r=r, w=w)
        t.op('dve', lambda e: e.reciprocal(out=dst, in_=dst), r=w, w=w)

    for hf in HALVES:
        with ExitStack() as _es:
            qT = _es.enter_context(SBT("qT", [128, 8, 1152], BF16))
            kT = _es.enter_context(SBT("kT", [128, 1152], BF16))
            v1 = _es.enter_context(SBT("v1", [128, 9, 2, 65], BF16))
            hglu = _es.enter_context(SBT("hglu", [128, 8, 1152], BF16))
            t.op('pool', lambda e: e.memset(v1[:], 1.0), w=['v1'])
            with ExitStack() as _es:
                hT = _es.enter_context(SBT("hT", [128, 16, 1152], BF16))
                xb0 = _es.enter_context(SBT("xb0", [128, D], F32))
                xs = _es.enter_context(SBT("xs", [128, D], BF16))
                wi0 = _es.enter_context(SBT("wi0", [128, 16, 256], BF16))
                wi1 = _es.enter_context(SBT("wi1", [128, 16, 256], BF16))
                tA = xb0[:, 0:512]
                tB = xs[:, 0:512]
                tC = xb0[:, 512:1024]
                xb = [xb0, xb0]; junk = xs
                wi = [wi0, wi1]
                for tt in range(9):
                    xt = xb[tt % 2]
                    xk = 'xb0'
                    r0 = (hf * 8 + tt) * 128
                    t.dma('sp', lambda e: e.dma_start(out=xt[:], in_=xh_d[r0:r0 + 128, :]), w=[xk])
                    t.op('dve', lambda e: e.memset(ssq[:], 0.0), w=['ssq'])
                    t.op('act', lambda e: e.activation(out=junk[:], in_=xt[:], func=AF.Square, accum_out=ssq[:, 0:1]),
                         r=[xk, 'ssq'], w=['xs', 'ssq'])
                    rsqrt_into(rstd[:], ssq[:], 1.0 / D, ['ssq'], ['rstd'])
                    t.op('dve', lambda e: e.tensor_scalar(out=xs[:], in0=xt[:], scalar1=rstd[:, 0:1], scalar2=None, op0=ALU.mult),
                         r=[xk, 'rstd'], w=['xs'])
                    for c in range(16):
                        t.op('pe', lambda e: e.transpose(out=psT[c // 8][:, (c % 8) * 128:(c % 8 + 1) * 128],
                                                         in_=xs[:, c * 128:(c + 1) * 128], identity=identb[:]),
                             r=['xs', 'identb'], w=[f'psT{c // 8}'])
                    for c in range(16):
                        t.op('dve', lambda e: e.tensor_scalar(out=hT[:, c, tt * 128:(tt + 1) * 128],
                                                              in0=psT[c // 8][:, (c % 8) * 128:(c % 8 + 1) * 128],
                                                              scalar1=A1[:, c:c + 1], scalar2=modT[:, 0, c:c + 1],
                                                              op0=ALU.mult, op1=ALU.add),
                             r=[f'psT{c // 8}', 'A1', 'modT'], w=['hT'])
                if STOP == 'norm':
                    tap(f'hT{hf}', hT, [128, 16, 1152], BF16, 'hT')
                    t.barrier()
                    return nc
                t.barrier()
                chunks = [(0, 128), (128, 512), (640, 512)]
                for j in dbg.get('jlist', range(26)):
                    wc = j // 2
                    b = wc % 2
                    if j % 2 == 0 or 'jlist' in dbg:
                        t.dma('pool', lambda e: e.dma_start(out=wi[b][:], in_=win_v[:, :, wc * 256:wc * 256 + 256]),
                              w=[f'wi{b}'])
                    jo = (j % 2) * 128
                    bias = pv[:, PV_BIN + j:PV_BIN + j + 1]
                    for ci, (c0, cn) in enumerate(chunks):
                        pb = ps[ci % 2]
                        for kc in range(0 if dbg.get('nomm') else 16):
                            t.op('pe', lambda e: e.matmul(pb[:, 0:cn], lhsT=wi[b][:, kc, jo:jo + 128], rhs=hT[:, kc, c0:c0 + cn],
                                                          start=(kc == 0), stop=(kc == 15)), r=[f'wi{b}', 'hT'], w=[P(ci % 2)])
                        if dbg.get('noevac'):
                            continue
                        if j < 9:
                            gcol = PV_GQ if j < 8 else PV_GK
                            dst = qT[:, j, c0:c0 + cn] if j < 8 else kT[:, c0:c0 + cn]
                            dk = 'qT' if j < 8 else 'kT'
                            t.op('act', lambda e: e.activation(out=tB[:, 0:cn], in_=pb[:, 0:cn], func=AF.Square, bias=bias),
                                 r=[P(ci % 2), 'pv'], w=['tB'])
                            t.op('act', lambda e: e.activation(out=tA[:, 0:cn], in_=pb[:, 0:cn], func=AF.Identity, bias=bias),
                                 r=[P(ci % 2), 'pv'], w=['tA'])
                            t.op('pe', lambda e: e.matmul(ps[2][:, 0:cn], lhsT=blkb[:], rhs=tB[:, 0:cn], start=True, stop=True),
                                 r=['blkb', 'tB'], w=[P(2)])
                            rsqrt_into(tC[:, 0:cn], ps[2][:, 0:cn], 1.0 / 64, [P(2)], ['tC'])
                            t.op('dve', lambda e: e.scalar_tensor_tensor(out=dst, in0=tA[:, 0:cn], scalar=pv[:, gcol:gcol + 1],
                                                                         in1=tC[:, 0:cn], op0=ALU.mult, op1=ALU.mult),
                                 r=['tA', 'tC', 'pv'], w=[dk])
                        elif j == 9:
                            t.op('act', lambda e: e.activation(out=tB[:, 0:cn], in_=pb[:, 0:cn], func=AF.Identity, bias=bias),
                                 r=[P(ci % 2), 'pv'], w=['tB'])
                            for s_ in range(cn // 128):
                                tt = c0 // 128 + s_
                                t.op('pe', lambda e: e.transpose(out=psT[0][:, 0:128], in_=tB[:, s_ * 128:(s_ + 1) * 128],
                                                                 identity=identb[:]), r=['tB', 'identb'], w=['psT0'])
                                t.op('dve', lambda e: e.tensor_copy(out=v1[:, tt, :, 0:64],
                                                                    in_=psT[0][:, 0:128].rearrange("p (g d) -> p g d", g=2)),
                                     r=['psT0'], w=['v1'])
                        elif j < 18:
                            c = j - 10
                            t.op('dve', lambda e: e.tensor_scalar(out=hglu[:, c, c0:c0 + cn], in0=pb[:, 0:cn], scalar1=bias,
                                                                  scalar2=None, op0=ALU.add), r=[P(ci % 2), 'pv'], w=['hglu'])
                        else:
                            c = j - 18
                            t.op('act', lambda e: e.activation(out=tB[:, 0:cn], in_=pb[:, 0:cn], func=AF.Sigmoid, bias=bias),
                                 r=[P(ci % 2), 'pv'], w=['tB'])
                            t.op('pool', lambda e: e.tensor_tensor(out=hglu[:, c, c0:c0 + cn], in0=hglu[:, c, c0:c0 + cn],
                                                                   in1=tB[:, 0:cn], op=ALU.mult), r=['tB', 'hglu'], w=['hglu'])
                if hf == 0 and not dbg.get('nohv'):
                    for c in range(8):
                        t.op('pool', lambda e: e.tensor_scalar(out=hglu[:, c, 0:128], in0=hglu[:, c, 0:128], scalar1=hv[:, 0:1],
                                                               scalar2=None, op0=ALU.mult), r=['hglu', 'hv'], w=['hglu'])
                tap(f'hT{hf}', hT, [128, 16, 1152], BF16, 'hT')
                tap(f'qT{hf}', qT, [128, 8, 1152], BF16, 'qT')
                tap(f'kT{hf}', kT, [128, 1152], BF16, 'kT')
                tap(f'v1{hf}', v1, [128, 9, 2, 65], BF16, 'v1')
                tap(f'hglu{hf}', hglu, [128, 8, 1152], BF16, 'hglu')
                t.barrier()
                if STOP == 'inproj':
                    return nc

            with ExitStack() as _es:
                acc = _es.enter_context(SBT("acc", [128, 8, 1024], F32))
                sq = _es.enter_context(SBT("sq", [128, 512], F32))
                mean = _es.enter_context(SBT("mean", [128, 512], F32))
                var = _es.enter_context(SBT("var", [128, 512], F32))
                tmp = _es.enter_context(SBT("tmp", [128, 512], F32))
                yatt = _es.enter_context(SBT("yatt", [128, 1024], F32))
                ynb = _es.enter_context(SBT("ynb", [128, 1024], BF16))
                pe0 = _es.enter_context(SBT("pe0", [128, 512], F32))
                pe1 = _es.enter_context(SBT("pe1", [128, 512], F32))
                pT0 = _es.enter_context(SBT("pT0", [128, 1024], BF16))
                pT1 = _es.enter_context(SBT("pT1", [128, 1024], BF16))
                den = _es.enter_context(SBT("den", [128, 8], F32))
                junk2 = _es.enter_context(SBT("junk2", [128, 1024], BF16))
                ctmp = _es.enter_context(SBT("ctmp", [128, 1024], F32))
                for c in range(8):
                    en = 'dve'
                    ak = f'acc{c}'
                    wc0 = PV_WDW + c * 31
                    t.op(en, lambda e: e.tensor_scalar(out=acc[:, c, :], in0=hglu[:, c, 98:98 + 1024], scalar1=pv[:, wc0:wc0 + 1],
                                                       scalar2=pv[:, PV_BDW + c:PV_BDW + c + 1], op0=ALU.mult, op1=ALU.add),
                         r=['hglu', 'pv'], w=[ak])
                    for j in range(1, 31):
                        if en == 'dve':
                            t.op(en, lambda e: e.scalar_tensor_tensor(out=acc[:, c, :], in0=hglu[:, c, 98 + j:98 + j + 1024],
                                                                      scalar=pv[:, wc0 + j:wc0 + j + 1], in1=acc[:, c, :],
                                                                      op0=ALU.mult, op1=ALU.add), r=['hglu', 'pv', ak], w=[ak])
                        else:
                            t.op(en, lambda e: e.tensor_scalar(out=ctmp[:], in0=hglu[:, c, 98 + j:98 + j + 1024],
                                                               scalar1=pv[:, wc0 + j:wc0 + j + 1], scalar2=None, op0=ALU.mult),
                                 r=['hglu', 'pv'], w=['ctmp'])
                            t.op(en, lambda e: e.tensor_tensor(out=acc[:, c, :], in0=acc[:, c, :], in1=ctmp[:], op=ALU.add),
                                 r=['ctmp', ak], w=[ak])
                for ch in range(2):
                    cs = slice(ch * 512, (ch + 1) * 512)
                    for c in range(8):
                        t.op('act', lambda e: e.activation(out=sq[:], in_=acc[:, c, cs], func=AF.Square), r=[f'acc{c}'], w=['sq'])
                        t.op('pe', lambda e: e.matmul(ps[0][:], lhsT=onesf[:], rhs=acc[:, c, cs], start=(c == 0), stop=(c == 7)),
                             r=['onesf', f'acc{c}'], w=[P(0)])
                        t.op('pe', lambda e: e.matmul(ps[1][:], lhsT=onesf[:], rhs=sq[:], start=(c == 0), stop=(c == 7)),
                             r=['onesf', 'sq'], w=[P(1)])
                    t.op('dve', lambda e: e.tensor_scalar(out=mean[:], in0=ps[0][:], scalar1=1.0 / 1024, scalar2=None, op0=ALU.mult),
                         r=[P(0)], w=['mean'])
                    t.op('dve', lambda e: e.tensor_tensor(out=tmp[:], in0=mean[:], in1=mean[:], op=ALU.mult), r=['mean'], w=['tmp'])
                    t.op('dve', lambda e: e.scalar_tensor_tensor(out=var[:], in0=ps[1][:], scalar=1.0 / 1024, in1=tmp[:],
                                                                 op0=ALU.mult, op1=ALU.subtract), r=[P(1), 'tmp'], w=['var'])
                    rsqrt_into(var[:], var[:], 1.0, ['var'], ['var'])
                    for c in range(8):
                        ak = f'acc{c}'
                        t.op('dve', lambda e: e.tensor_tensor(out=tmp[:], in0=acc[:, c, cs], in1=mean[:], op=ALU.subtract),
                             r=[ak, 'mean'], w=['tmp'])
                        t.op('dve', lambda e: e.tensor_tensor(out=tmp[:], in0=tmp[:], in1=var[:], op=ALU.mult), r=['tmp', 'var'], w=['tmp'])
                        t.op('act', lambda e: e.activation(out=acc[:, c, cs], in_=tmp[:], func=AF.Silu,
                                                           bias=pv[:, PV_LNB + c:PV_LNB + c + 1], scale=pv[:, PV_LNG + c:PV_LNG + c + 1]),
                             r=['tmp', 'pv'], w=[ak])
                        t.op('act', lambda e: e.activation(out=sq[:], in_=acc[:, c, cs], func=AF.Square), r=[ak], w=['sq'])
                        t.op('pe', lambda e: e.matmul(ps[2][:], lhsT=onesf[:], rhs=sq[:], start=(c == 0), stop=(c == 7)),
                             r=['onesf', 'sq'], w=[P(2)])
                    rsqrt_into(var[:], ps[2][:], 1.0 / 1024, [P(2)], ['var'])
                    for c in range(8):
                        t.op('dve', lambda e: e.scalar_tensor_tensor(out=mixedT[:, 8 + c, cs], in0=acc[:, c, cs],
                                                                     scalar=pv[:, PV_GOC + c:PV_GOC + c + 1], in1=var[:],
                                                                     op0=ALU.mult, op1=ALU.mult), r=[f'acc{c}', 'var', 'pv'], w=['mixedT'])

                pes = [pe0, pe1]
                pTs = [pT0, pT1]
                for n in range(8):
                    tt = n + 1
                    for g in range(2):
                        gp = slice(g * 64, (g + 1) * 64)
                        for kt in range(2):
                            kc0 = (tt - 1 + kt) * 128
                            for hh in range(2):
                                pi = 2 + hh
                                t.op('pe', lambda e: e.matmul(ps[pi][:], lhsT=kT[gp, kc0:kc0 + 128],
                                                              rhs=qT[gp, 4 * hh:4 * hh + 4, tt * 128:(tt + 1) * 128],
                                                              start=True, stop=True), r=['kT', 'qT'], w=[P(pi)])
                                t.op('act', lambda e: e.activation(out=pes[hh][:], in_=ps[pi][:], func=AF.Exp, scale=0.125),
                                     r=[P(pi)], w=[f'pe{hh}'])
                                if kt == 0 and n == 0 and hf == 0:
                                    eb = ebt0[:, g * 1024 + hh * 512:g * 1024 + (hh + 1) * 512]
                                else:
                                    o = kt * 2048 + g * 1024 + hh * 512
                                    eb = ebt[:, o:o + 512]
                                t.op('dve', lambda e: e.tensor_tensor(out=pTs[kt][:, hh * 512:(hh + 1) * 512], in0=pes[hh][:], in1=eb,
                                                                      op=ALU.mult), r=[f'pe{hh}', 'ebt', 'ebt0'], w=[f'pT{kt}'])
                        for jj in range(8):
                            pi = 4 + jj // 4
                            oc = (jj % 4) * 65
                            for kt in range(2):
                                t.op('pe', lambda e: e.matmul(ps[pi][:, oc:oc + 65], lhsT=pTs[kt][:, jj * 128:(jj + 1) * 128],
                                                              rhs=v1[:, tt - 1 + kt, g, :], start=(kt == 0), stop=(kt == 1)),
                                     r=[f'pT{kt}', 'v1'], w=[P(pi)])
                        for half in range(2):
                            pi = 4 + half
                            pv3 = ps[pi][:, 0:260].rearrange("p (h d) -> p h d", d=65)
                            hs = 8 * g + 4 * half
                            t.op('dve', lambda e: e.tensor_tensor(out=den[:, 0:4], in0=pv3[:, :, 64], in1=esink[:, hs:hs + 4], op=ALU.add),
                                 r=[P(pi), 'esink'], w=['den'])
                            t.op('dve', lambda e: e.reciprocal(out=den[:, 0:4], in_=den[:, 0:4]), r=['den'], w=['den'])
                            for j4 in range(4):
                                h = hs + j4
                                t.op('dve', lambda e: e.tensor_scalar(out=yatt[:, h * 64:(h + 1) * 64], in0=pv3[:, j4, 0:64],
                                                                      scalar1=den[:, j4:j4 + 1], scalar2=None, op0=ALU.mult),
                                     r=[P(pi), 'den'], w=['yatt'])
                    t.op('dve', lambda e: e.memset(ssq[:], 0.0), w=['ssq'])
                    t.op('act', lambda e: e.activation(out=junk2[:], in_=yatt[:], func=AF.Square, accum_out=ssq[:, 0:1]),
                         r=['yatt', 'ssq'], w=['junk2', 'ssq'])
                    rsqrt_into(rstd[:], ssq[:], 1.0 / 1024, ['ssq'], ['rstd'])
                    t.op('dve', lambda e: e.scalar_tensor_tensor(out=ynb[:], in0=yatt[:], scalar=rstd[:, 0:1], in1=goa[:],
                                                                 op0=ALU.mult, op1=ALU.mult), r=['yatt', 'rstd', 'goa'], w=['ynb'])
                    for c in range(8):
                        t.op('pe', lambda e: e.transpose(out=psT[1][:, c * 128:(c + 1) * 128], in_=ynb[:, c * 128:(c + 1) * 128],
                                                         identity=identb[:]), r=['ynb', 'identb'], w=['psT1'])
                    t.op('dve', lambda e: e.tensor_copy(out=mixedT[:, 0:8, n * 128:(n + 1) * 128],
                                                        in_=psT[1][:, :].rearrange("p (c q) -> p c q", c=8)), r=['psT1'], w=['mixedT'])
                tap(f'mixedT{hf}', mixedT, [128, 16, 1024], BF16, 'mixedT')
                t.barrier()
                if STOP == 'mixer':
                    return nc

        for grp in GROUPS:
            with ExitStack() as _es:
                h2T = _es.enter_context(SBT("h2T", [128, 16, 512], BF16))
                wt = _es.enter_context(SBT("wt", [128, 4, 32], F32))
                with ExitStack() as _es:
                    wo = _es.enter_context(SBT("wo", [128, 16, D], BF16))
                    xt2 = _es.enter_context(SBT("xt2", [128, D], F32))
                    x1 = _es.enter_context(SBT("x1", [128, D], F32))
                    h2 = _es.enter_context(SBT("h2", [128, D], F32))
                    h2b = _es.enter_context(SBT("h2b", [128, D], BF16))
                    lg = _es.enter_context(SBT("lg", [128, 32], F32))
                    top8 = _es.enter_context(SBT("top8", [128, 8], F32))
                    msk = _es.enter_context(SBT("msk", [128, 32], F32))
                    sm = _es.enter_context(SBT("sm", [128, 2], F32))
                    for q in range(4):
                        t.dma('pool', lambda e: e.dma_start(out=wo[:, :, q * 512:(q + 1) * 512], in_=wout_v[:, :, q * 512:(q + 1) * 512]),
                              w=['wo'])
                    for ti in range(4):
                        lt = grp * 4 + ti
                        gt = hf * 8 + lt
                        t.dma('sp', lambda e: e.dma_start(out=xt2[:], in_=xh_d[(gt + 1) * 128:(gt + 2) * 128, :]), w=['xt2'])
                        t.op('pool', lambda e: e.tensor_tensor(out=xt2[:], in0=xt2[:], in1=GB1[:], op=ALU.add), r=['xt2', 'GB1'], w=['xt2'])
                        for nq in range(4):
                            for cc in range(16):
                                t.op('pe', lambda e: e.matmul(ps[nq][:], lhsT=mixedT[:, cc, lt * 128:(lt + 1) * 128],
                                                              rhs=wo[:, cc, nq * 512:(nq + 1) * 512], start=(cc == 0), stop=(cc == 15)),
                                     r=['mixedT', 'wo'], w=[P(nq)])
                            ns = slice(nq * 512, (nq + 1) * 512)
                            t.op('dve', lambda e: e.tensor_tensor(out=x1[:, ns], in0=ps[nq][:], in1=G1[:, ns], op=ALU.mult),
                                 r=[P(nq), 'bc0'], w=['x1'])
                        t.op('dve', lambda e: e.tensor_tensor(out=x1[:], in0=x1[:], in1=xt2[:], op=ALU.add), r=['x1', 'xt2'], w=['x1'])
                        t.dma('sp', lambda e: e.dma_start(out=x1_d[gt * 128:(gt + 1) * 128, :], in_=x1[:]), r=['x1'], w=['x1d'])
                        if dbg.get('p6', 9) < 2: continue
                        t.op('dve', lambda e: e.memset(ssq[:], 0.0), w=['ssq'])
                        t.op('act', lambda e: e.activation(out=h2[:], in_=x1[:], func=AF.Square, accum_out=ssq[:, 0:1]),
                             r=['x1', 'ssq'], w=['h2', 'ssq'])
                        rsqrt_into(rstd[:], ssq[:], 1.0 / D, ['ssq'], ['rstd'])
                        t.op('dve', lambda e: e.scalar_tensor_tensor(out=h2[:], in0=x1[:], scalar=rstd[:, 0:1], in1=A2bc[:],
                                                                     op0=ALU.mult, op1=ALU.mult), r=['x1', 'rstd', 'bc2'], w=['h2'])
                        t.op('pool', lambda e: e.tensor_tensor(out=h2b[:], in0=h2[:], in1=B2bc[:], op=ALU.add), r=['h2', 'bc1'], w=['h2b'])
                        if dbg.get('p6', 9) < 3: continue
                        for cc in range(16):
                            t.op('pe', lambda e: e.transpose(out=psT[cc // 8][:, (cc % 8) * 128:(cc % 8 + 1) * 128],
                                                             in_=h2b[:, cc * 128:(cc + 1) * 128], identity=identb[:]),
                                 r=['h2b', 'identb'], w=[f'psT{cc // 8}'])
                        t.op('act', lambda e: e.activation(out=h2T[:, 0:8, ti * 128:(ti + 1) * 128],
                                                           in_=psT[0][:, :].rearrange("p (c q) -> p c q", c=8), func=AF.Identity),
                             r=['psT0'], w=['h2T'])
                        t.op('dve', lambda e: e.tensor_copy(out=h2T[:, 8:16, ti * 128:(ti + 1) * 128],
                                                            in_=psT[1][:, :].rearrange("p (c q) -> p c q", c=8)),
                             r=['psT1'], w=['h2T'])
                        for cc in range(16):
                            t.op('pe', lambda e: e.matmul(ps[5][:, 0:32], lhsT=h2T[:, cc, ti * 128:(ti + 1) * 128],
                                                          rhs=wrb[:, cc * 32:(cc + 1) * 32],
                                                          start=(cc == 0), stop=(cc == 15)), r=['h2T', 'wrb'], w=[P(5)])
                        if dbg.get('p6', 9) < 4: continue
                        t.op('dve', lambda e: e.tensor_tensor(out=lg[:], in0=ps[5][:, 0:32], in1=brt[:], op=ALU.add), r=[P(5), 'brt'], w=['lg'])
                        if dbg.get('p6', 9) < 5: continue
                        t.op('dve', lambda e: e.max(out=top8[:], in_=lg[:]), r=['lg'], w=['top8'])
                        t.op('dve', lambda e: e.tensor_scalar(out=msk[:], in0=lg[:], scalar1=top8[:, 3:4], scalar2=None, op0=ALU.is_ge),
                             r=['lg', 'top8'], w=['msk'])
                        t.op('dve', lambda e: e.tensor_scalar(out=sm[:, 0:1], in0=top8[:, 0:1], scalar1=-1.0, scalar2=None, op0=ALU.mult),
                             r=['top8'], w=['sm'])
                        t.op('act', lambda e: e.activation(out=lg[:], in_=lg[:], func=AF.Exp, bias=sm[:, 0:1]), r=['lg', 'sm'], w=['lg'])
                        t.op('dve', lambda e: e.tensor_tensor(out=lg[:], in0=lg[:], in1=msk[:], op=ALU.mult), r=['lg', 'msk'], w=['lg'])
                        t.op('dve', lambda e: e.reduce_sum(out=sm[:, 1:2], in_=lg[:], axis=AX.X), r=['lg'], w=['sm'])
                        t.op('dve', lambda e: e.reciprocal(out=sm[:, 1:2], in_=sm[:, 1:2]), r=['sm'], w=['sm'])
                        t.op('dve', lambda e: e.tensor_scalar(out=wt[:, ti, :], in0=lg[:], scalar1=sm[:, 1:2], scalar2=None, op0=ALU.mult),
                             r=['lg', 'sm'], w=['wt'])
                    tap(f'wt{hf}{grp}', wt, [128, 4, 32], F32, 'wt')
                    tap(f'h2T{hf}{grp}', h2T, [128, 16, 512], BF16, 'h2T')
                    t.barrier()
                    if STOP == 'outproj':
                        return nc

                with ExitStack() as _es:
                    yacc = _es.enter_context(SBT("yacc", [128, 4, D], F32))
                    actT = _es.enter_context(SBT("actT", [128, 16, 512], BF16))
                    wm0 = _es.enter_context(SBT("wm0", [128, 16, 256], BF16))
                    wm1 = _es.enter_context(SBT("wm1", [128, 16, 256], BF16))
                    wm2 = _es.enter_context(SBT("wm2", [128, 16, 256], BF16))
                    wm3 = _es.enter_context(SBT("wm3", [128, 16, 256], BF16))
                    bdb0 = _es.enter_context(SBT("bdb0", [128, 256], F32))
                    bdb1 = _es.enter_context(SBT("bdb1", [128, 256], F32))
                    g1 = _es.enter_context(SBT("g1", [128, 512], F32))
                    sg = _es.enter_context(SBT("sg", [128, 512], F32))
                    u1 = _es.enter_context(SBT("u1", [128, 512], F32))
                    wm = [wm0, wm1, wm2, wm3]
                    bdb = [bdb0, bdb1]
                    wmi = 0
                    t.op('dve', lambda e: e.memset(yacc[:], 0.0), w=['yacc'])
                    for ex in range(NEXP):
                        wgv = wg_d[ex].rearrange("(kc p) f -> p kc f", p=128)
                        wuv = wu_d[ex].rearrange("(kc p) f -> p kc f", p=128)
                        wdv = wd_d[ex].rearrange("(kc p) f -> p kc f", p=128)
                        for qf in range(8):
                            bg_, bu_ = wmi % 4, (wmi + 1) % 4
                            wmi += 2
                            t.dma('pool', lambda e: e.dma_start(out=wm[bg_][:], in_=wgv[:, :, qf * 256:(qf + 1) * 256]), w=[f'wm{bg_}'])
                            t.dma('pool', lambda e: e.dma_start(out=wm[bu_][:], in_=wuv[:, :, qf * 256:(qf + 1) * 256]), w=[f'wm{bu_}'])
                            for ft in range(2):
                                f = qf * 2 + ft
                                pg, pu = ps[(f % 2) * 2], ps[(f % 2) * 2 + 1]
                                kg, ku = P((f % 2) * 2), P((f % 2) * 2 + 1)
                                for kc in range(16):
                                    t.op('pe', lambda e: e.matmul(pg[:], lhsT=wm[bg_][:, kc, ft * 128:(ft + 1) * 128], rhs=h2T[:, kc, :],
                                                                  start=(kc == 0), stop=(kc == 15)), r=[f'wm{bg_}', 'h2T'], w=[kg])
                                for kc in range(16):
                                    t.op('pe', lambda e: e.matmul(pu[:], lhsT=wm[bu_][:, kc, ft * 128:(ft + 1) * 128], rhs=h2T[:, kc, :],
                                                                  start=(kc == 0), stop=(kc == 15)), r=[f'wm{bu_}', 'h2T'], w=[ku])
                                if dbg.get('moe', 9) < 2: continue
                                bgc = pv[:, PV_BG + ex * 16 + f:PV_BG + ex * 16 + f + 1]
                                buc = pv[:, PV_BU + ex * 16 + f:PV_BU + ex * 16 + f + 1]
                                t.op('dve', lambda e: e.tensor_scalar(out=g1[:], in0=pg[:], scalar1=bgc, scalar2=7.0, op0=ALU.add, op1=ALU.min),
                                     r=[kg, 'pv'], w=['g1'])
                                t.op('act', lambda e: e.activation(out=sg[:], in_=g1[:], func=AF.Sigmoid, scale=1.702), r=['g1'], w=['sg'])
                                t.op('dve', lambda e: e.tensor_scalar(out=u1[:], in0=pu[:], scalar1=buc, scalar2=7.0, op0=ALU.add, op1=ALU.min),
                                     r=[ku, 'pv'], w=['u1'])
                                t.op('dve', lambda e: e.tensor_scalar(out=u1[:], in0=u1[:], scalar1=-7.0, scalar2=1.0, op0=ALU.max, op1=ALU.add),
                                     r=['u1'], w=['u1'])
                                t.op('pool', lambda e: e.tensor_tensor(out=g1[:], in0=g1[:], in1=sg[:], op=ALU.mult), r=['g1', 'sg'], w=['g1'])
                                t.op('dve', lambda e: e.tensor_tensor(out=actT[:, f, :], in0=g1[:], in1=u1[:], op=ALU.mult),
                                     r=['g1', 'u1'], w=['actT'])
                        for dq in range(8 if dbg.get('moe', 9) >= 3 else 0):
                            bd_ = wmi % 4
                            wmi += 1
                            bb = dq % 2
                            ds_ = slice(dq * 256, (dq + 1) * 256)
                            t.dma('pool', lambda e: e.dma_start(out=wm[bd_][:], in_=wdv[:, :, ds_]), w=[f'wm{bd_}'])
                            if dbg.get('moe', 9) >= 3:
                                t.dma('sp', lambda e: e.dma_start(out=bdb[bb][:], in_=bd_d[ex:ex + 1, ds_].partition_broadcast(128)),
                                      w=[f'bdb{bb}'])
                            for ti in range(4):
                                pd, kd = ps[4 + ti % 2], P(4 + ti % 2)
                                for fc in range(16):
                                    t.op('pe', lambda e: e.matmul(pd[:, 0:256], lhsT=actT[:, fc, ti * 128:(ti + 1) * 128], rhs=wm[bd_][:, fc, :],
                                                                  start=(fc == 0), stop=(fc == 15)), r=['actT', f'wm{bd_}'], w=[kd])
                                t.op('dve', lambda e: e.tensor_tensor(out=sg[:, 0:256], in0=pd[:, 0:256], in1=bdb[bb][:], op=ALU.add),
                                     r=[kd, f'bdb{bb}'], w=['sg'])
                                t.op('dve', lambda e: e.scalar_tensor_tensor(out=yacc[:, ti, ds_], in0=sg[:, 0:256], scalar=wt[:, ti, ex:ex + 1],
                                                                             in1=yacc[:, ti, ds_], op0=ALU.mult, op1=ALU.add),
                                     r=['sg', 'wt', 'yacc'], w=['yacc'])
                    for ti in range(4 if dbg.get('moe', 9) >= 4 else 0):
                        gt = hf * 8 + grp * 4 + ti
                        for q in range(4):
                            qs = slice(q * 512, (q + 1) * 512)
                            t.dma('sp', lambda e: e.dma_start(out=g1[:], in_=x1_d[gt * 128:(gt + 1) * 128, qs]), r=['x1d'], w=['g1'])
                            t.op('dve', lambda e: e.tensor_tensor(out=u1[:], in0=yacc[:, ti, qs], in1=G2[:, qs], op=ALU.mult),
                                 r=['yacc', 'bc3'], w=['u1'])
                            t.op('dve', lambda e: e.tensor_tensor(out=g1[:], in0=g1[:], in1=u1[:], op=ALU.add), r=['g1', 'u1'], w=['g1'])
                            t.dma('sp', lambda e: e.dma_start(out=out_d[gt * 128:(gt + 1) * 128, qs], in_=g1[:]), r=['g1'], w=['outd'])
                    t.barrier()
    return nc


def _t5_bucket(dist):
    max_exact = 16
    d = np.maximum(dist, 0)
    lr = np.log(np.maximum(d, max_exact).astype(np.float32) / max_exact)
    large = max_exact + (lr / math.log(128 / max_exact) * (32 - max_exact)).astype(np.int32)
    large = np.minimum(large, 31)
    return np.where(d < max_exact, d, large)


def _prep(inputs):
    f = np.float32
    x = np.asarray(inputs["x"], f)[0]
    g = lambda k: np.asarray(inputs[k], f)[0]
    pp = lambda v: np.ascontiguousarray(v.reshape(-1, 128).T)
    perm = []
    for jj in range(8):
        perm += list(range(jj * 64, jj * 64 + 64)) + list(range((8 + jj) * 64, (8 + jj) * 64 + 64))
    perm += list(range(1024, 3328))
    perm = np.array(perm)
    win = np.ascontiguousarray(g("w_in")[:, perm])
    b_in = g("b_in")[perm]
    pv = np.zeros((128, NPV), f)
    pv[:, PV_C:PV_C + 16] = pp(np.asarray(inputs["c"], f)[0])
    pv[:, PV_G1:PV_G1 + 16] = pp(g("g_norm1"))
    pv[:, PV_BIN:PV_BIN + 26] = pp(b_in)
    pv[:, PV_GQ] = np.tile(g("g_q"), 2)
    pv[:, PV_GK] = np.tile(g("g_k"), 2)
    pv[:, PV_WDW:PV_WDW + 248] = g("w_dw").reshape(31, 8, 128).transpose(2, 1, 0).reshape(128, 248)
    pv[:, PV_BDW:PV_BDW + 8] = pp(g("b_dw"))
    pv[:, PV_LNG:PV_LNG + 8] = pp(g("ln_g"))
    pv[:, PV_LNB:PV_LNB + 8] = pp(g("ln_b"))
    pv[:, PV_GOC:PV_GOC + 8] = pp(g("g_out_conv"))
    pv[:, PV_BG:PV_BG + 512] = g("b_gate").reshape(32, 16, 128).transpose(2, 0, 1).reshape(128, 512)
    pv[:, PV_BU:PV_BU + 512] = g("b_up").reshape(32, 16, 128).transpose(2, 0, 1).reshape(128, 512)
    pv[:, PV_WR:PV_WR + 512] = g("w_router").reshape(16, 128, 32).transpose(1, 0, 2).reshape(128, 512)
    rv = np.zeros((1, NRV), f)
    rv[0, RV_GOA:RV_GOA + 1024] = g("g_out_attn")
    rv[0, RV_BOUT:RV_BOUT + D] = g("b_out")
    rv[0, RV_G2:RV_G2 + D] = g("g_norm2")
    rv[0, RV_BR:RV_BR + 32] = g("b_router")
    rv[0, RV_SINK:RV_SINK + 16] = g("sinks")
    rv = np.ascontiguousarray(np.broadcast_to(rv, (128, NRV)))
    rb = np.asarray(inputs["rel_bias"], f)
    kk = np.arange(128)[:, None]
    qq = np.arange(128)[None, :]
    bt = np.zeros((128, 2, 2, 8, 128), f)
    for kt in range(2):
        dist = qq + 128 - kk if kt == 0 else qq - kk
        valid = (dist >= 0) & (dist < 128)
        bk = _t5_bucket(dist)
        for gg in range(2):
            for jj in range(8):
                bt[:, kt, gg, jj, :] = np.where(valid, rb[bk, 8 * gg + jj], f(-30000.0))
    bt = bt.reshape(128, 4096)
    blk = np.zeros((128, 128), f)
    blk[:64, :64] = 1
    blk[64:, 64:] = 1
    common = dict(pv=pv, rv=rv, bada=g("b_ada")[None, :], wada=g("w_ada"), win=win, wout=g("w_out"), biast=bt,
                  wg=g("w_gate"), wu=g("w_up"), wd=g("w_down"), bd=g("b_down"),
                  identf=np.eye(128, dtype=f), blk=blk)
    in_maps = []
    for c in range(NCORE):
        xh = np.zeros((17 * 128, D), f)
        if c == 0:
            xh[128:] = x[0:TOK]
        else:
            xh[:] = x[c * TOK - 128:(c + 1) * TOK]
        hvv = np.full((128, 1), 0.0 if c == 0 else 1.0, f)
        m = dict(common)
        m["xh"] = xh
        m["hv"] = hvv
        in_maps.append(m)
    return in_maps


def kernel(**inputs):
    in_maps = _prep(inputs)
    nc = build_nc()
    res = run_bass_kernel_spmd(nc, in_maps, core_ids=list(range(NCORE)))
    out = np.concatenate([np.asarray(r["out"], np.float32) for r in res.results], axis=0)
    return out.reshape(1, NCORE * TOK, D)
```

```python
import math
from contextlib import ExitStack
import numpy as np
import concourse.bass as bass
import concourse.mybir as mybir
from concourse.bass_utils import run_bass_kernel_spmd

F32 = mybir.dt.float32
BF16 = mybir.dt.bfloat16
ALU = mybir.AluOpType
AF = mybir.ActivationFunctionType
AX = mybir.AxisListType

D = 2048
NCORE = 8
TOK = 2048
NT = 16
E = 32
EPS = 1e-6
PV_C, PV_G1, PV_BIN, PV_GQ, PV_GK = 0, 16, 32, 58, 59
PV_WDW, PV_BDW, PV_LNG, PV_LNB, PV_GOC = 60, 308, 316, 324, 332
PV_BG, PV_BU, PV_WR = 340, 852, 1364
PV_G2 = 1364 + 512
NPV = 1364 + 512 + 16
RV_GOA, RV_BOUT, RV_G2, RV_BR, RV_SINK = 0, 1024, 3072, 5120, 5152
NRV = 5168


class Trk:
    def __init__(s, nc):
        s.nc = nc
        s.eng = dict(pe=nc.tensor, act=nc.scalar, dve=nc.vector, pool=nc.gpsimd, sp=nc.sync)
        s.csem = {e: nc.alloc_semaphore("c_" + e) for e in s.eng}
        s.ND = 24
        s.dsem = [nc.alloc_semaphore(f"dq{i}") for i in range(s.ND)]
        s.reset_state()

    def reset_state(s):
        s.cnt = {e: 0 for e in s.eng}
        s.seen = {c: {p: 0 for p in s.eng} for c in s.eng}
        s.dval = [0] * s.ND
        s.dseen = {c: [0] * s.ND for c in s.eng}
        s.drr = 0
        s.lastw = {}
        s.readers = {}

    def _wait(s, c, tok):
        if tok[0] == 'e':
            _, p, seq = tok
            if p == c and p == 'pe':
                return
            if s.seen[c][p] >= seq:
                return
            s.eng[c].wait_ge(s.csem[p], seq)
            s.seen[c][p] = seq
        else:
            _, k, val = tok
            if s.dseen[c][k] >= val:
                return
            s.eng[c].wait_ge(s.dsem[k], val)
            s.dseen[c][k] = val

    def _deps(s, c, r, w):
        for b in r:
            t = s.lastw.get(b)
            if t:
                s._wait(c, t)
        for b in w:
            t = s.lastw.get(b)
            if t:
                s._wait(c, t)
            rd = s.readers.get(b)
            if rd:
                for p, seq in rd[0].items():
                    if p != c:
                        s._wait(c, ('e', p, seq))
                for t in rd[1]:
                    s._wait(c, t)

    def _record(s, tok, r, w):
        for b in r:
            rd = s.readers.setdefault(b, [{}, []])
            if tok[0] == 'e':
                rd[0][tok[1]] = tok[2]
            else:
                rd[1].append(tok)
        for b in w:
            s.lastw[b] = tok
            s.readers[b] = [{}, []]

    def op(s, c, fn, r=(), w=()):
        s._deps(c, r, w)
        ins = fn(s.eng[c])
        s.cnt[c] += 1
        ins.then_inc(s.csem[c], 1)
        s._record(('e', c, s.cnt[c]), r, w)

    def dma(s, c, fn, r=(), w=()):
        s._deps(c, r, w)
        k = s.drr
        s.drr = (s.drr + 1) % s.ND
        if s.dval[k] > 0:
            s._wait(c, ('d', k, s.dval[k]))
        ins = fn(s.eng[c])
        s.dval[k] += 16
        ins.then_inc(s.dsem[k], 16)
        s._record(('d', k, s.dval[k]), r, w)

    def barrier(s):
        for c in s.eng:
            for p in s.eng:
                if s.cnt[p] > 0:
                    s._wait(c, ('e', p, s.cnt[p]))
            for k in range(s.ND):
                if s.dval[k] > 0:
                    s._wait(c, ('d', k, s.dval[k]))
        s.lastw = {}
        s.readers = {}


def build_nc(dbg=None):
    dbg = dbg or {}
    STOP = dbg.get('stop')
    HALVES = dbg.get('halves', [0, 1])
    GROUPS = dbg.get('groups', [0, 1])
    NEXP = dbg.get('n_exp', E)
    TAPS = dbg.get('taps', False)
    nc = bass.Bass("TRN2", target_bir_lowering=False)

    def din(name, shape, dt=F32):
        return nc.dram_tensor(name, list(shape), dt, kind="ExternalInput").ap()

    xh_d = din("xh", [17 * 128, D])
    hv_d = din("hv", [128, 1])
    pv_d = din("pv", [128, NPV])
    rv_d = din("rv", [128, NRV])
    bada_d = din("bada", [1, 6 * D])
    wada_d = din("wada", [D, 6 * D])
    win_d = din("win", [D, 3328])
    wout_d = din("wout", [D, D])
    bias_d = din("biast", [128, 4096])
    wg_d = din("wg", [NEXP, D, D])
    wu_d = din("wu", [NEXP, D, D])
    wd_d = din("wd", [NEXP, D, D])
    bd_d = din("bd", [NEXP, D])
    identf_d = din("identf", [128, 128])
    blk_d = din("blk", [128, 128])
    out_d = nc.dram_tensor("out", [TOK, D], F32, kind="ExternalOutput").ap()
    x1_d = nc.dram_tensor("x1s", [TOK, D], F32, kind=("ExternalOutput" if TAPS else "Internal")).ap()
    g1s_d = nc.dram_tensor("g1s", [2, 128, D], F32, kind="Internal").ap()

    t = Trk(nc)

    def tap(name, tens, shape, dt, key):
        if not TAPS:
            return
        dd = nc.dram_tensor("tap_" + name, list(shape), dt, kind="ExternalOutput").ap()
        t.dma('sp', lambda e: e.dma_start(out=dd, in_=tens[:]), r=[key], w=['tap_' + name])
    _uid = [0]

    def SB(name, shape, dt):
        _uid[0] += 1
        return nc.alloc_sbuf_tensor(f"{name}_s{_uid[0]}", shape, dt)

    def SBT(name, shape, dt):
        _uid[0] += 1
        return nc.sbuf_tensor(f"{name}_s{_uid[0]}", shape, dt)

    pv = SB("pv", [128, NPV], F32)
    hv = SB("hv", [128, 1], F32)
    identf = SB("identf", [128, 128], F32)
    identb = SB("identb", [128, 128], BF16)
    blkb = SB("blkb", [128, 128], BF16)
    onesf = SB("onesf", [128, 128], F32)
    sT = SB("sT", [128, 16], BF16)
    modT = SB("modT", [128, 4, 16], F32)
    A2 = SB("A2", [128, 16], F32)
    A1 = SB("A1", [128, 16], F32)
    G2 = SB("G2", [128, D], F32)
    goa = SB("goa", [128, 1024], F32)
    brt = SB("brt", [128, 32], F32)
    esink = SB("esink", [128, 16], F32)
    ebt = SB("ebt", [128, 4096], BF16)
    ebt0 = SB("ebt0", [128, 2048], BF16)
    wrb = SB("wrb", [128, 512], BF16)
    ssq = SB("ssq", [128, 1], F32)
    rstd = SB("rstd", [128, 1], F32)

    ps = [nc.alloc_psum_tensor(f"ps{i}", [128, 512], F32) for i in range(6)]
    psT = [nc.alloc_psum_tensor(f"psT{i}", [128, 1024], BF16) for i in range(2)]

    def P(i):
        return f"ps{i}"

    t.dma('sp', lambda e: e.dma_start(out=pv[:], in_=pv_d), w=['pv'])
    t.dma('sp', lambda e: e.dma_start(out=hv[:], in_=hv_d), w=['hv'])
    t.dma('sp', lambda e: e.dma_start(out=identf[:], in_=identf_d), w=['identf'])
    t.dma('pool', lambda e: e.dma_start(out=identb[:], in_=identf_d), w=['identb'])
    t.dma('pool', lambda e: e.dma_start(out=blkb[:], in_=blk_d), w=['blkb'])
    t.op('dve', lambda e: e.memset(onesf[:], 1.0), w=['onesf'])
    t.op('dve', lambda e: e.tensor_copy(out=wrb[:], in_=pv[:, PV_WR:PV_WR + 512]), r=['pv'], w=['wrb'])
    t.op('act', lambda e: e.activation(out=sT[:], in_=pv[:, PV_C:PV_C + 16], func=AF.Silu), r=['pv'], w=['sT'])

    with ExitStack() as _es:
        rvt = _es.enter_context(SBT("rvt", [128, NRV], F32))
        biasf = _es.enter_context(SBT("biasf", [128, 4096], F32))
        wr0 = _es.enter_context(SBT("wr0", [128, 16, 512], BF16))
        wr1 = _es.enter_context(SBT("wr1", [128, 16, 512], BF16))
        brow0 = _es.enter_context(SBT("brow0", [1, 512], F32))
        brow1 = _es.enter_context(SBT("brow1", [1, 512], F32))
        mrow = _es.enter_context(SBT("mrow", [1, 512], F32))
        bcg = _es.enter_context(SBT("bcg", [128, D], F32))
        gb1 = _es.enter_context(SBT("gb1", [128, D], F32))
        wr = [wr0, wr1]
        brow = [brow0, brow1]
        t.dma('sp', lambda e: e.dma_start(out=rvt[:], in_=rv_d), w=['rvt'])
        t.dma('sp', lambda e: e.dma_start(out=biasf[:], in_=bias_d), w=['biasf'])
        t.op('act', lambda e: e.activation(out=ebt[:], in_=biasf[:], func=AF.Exp), r=['biasf'], w=['ebt'])
        t.op('dve', lambda e: e.tensor_scalar(out=ebt0[:], in0=ebt[:, 0:2048], scalar1=hv[:, 0:1], scalar2=None,
                                              op0=ALU.mult), r=['ebt', 'hv'], w=['ebt0'])
        t.op('act', lambda e: e.activation(out=esink[:], in_=rvt[:, RV_SINK:RV_SINK + 16], func=AF.Exp),
             r=['rvt'], w=['esink'])
        t.op('dve', lambda e: e.tensor_copy(out=goa[:], in_=rvt[:, RV_GOA:RV_GOA + 1024]), r=['rvt'], w=['goa'])
        t.op('dve', lambda e: e.tensor_copy(out=brt[:], in_=rvt[:, RV_BR:RV_BR + 32]), r=['rvt'], w=['brt'])

        wada_v = wada_d.rearrange("(kc p) f -> p kc f", p=128)
        for n in range(24):
            b = n % 2
            t.dma('pool', lambda e: e.dma_start(out=wr[b][:], in_=wada_v[:, :, n * 512:(n + 1) * 512]), w=[f'wr{b}'])
            t.dma('sp', lambda e: e.dma_start(out=brow[b][:], in_=bada_d[0:1, n * 512:(n + 1) * 512]), w=[f'brow{b}'])
            for kc in range(16):
                t.op('pe', lambda e: e.matmul(ps[0][0:1, :], lhsT=sT[:, kc:kc + 1], rhs=wr[b][:, kc, :],
                                              start=(kc == 0), stop=(kc == 15)), r=['sT', f'wr{b}'], w=[P(0)])
            t.op('dve', lambda e: e.tensor_tensor(out=mrow[:], in0=ps[0][0:1, :], in1=brow[b][:], op=ALU.add),
                 r=[P(0), f'brow{b}'], w=['mrow'])
            which, q = n // 4, n % 4
            if which in (0, 1, 3, 4):
                mi = {0: 0, 1: 1, 3: 2, 4: 3}[which]
                for j in range(4):
                    t.op('pe', lambda e: e.matmul(ps[1][:, j:j + 1], lhsT=mrow[0:1, j * 128:(j + 1) * 128],
                                                  rhs=onesf[0:1, 0:1], start=True, stop=True), r=['mrow', 'onesf'], w=[P(1)])
                t.op('dve', lambda e: e.tensor_copy(out=modT[:, mi, q * 4:(q + 1) * 4], in_=ps[1][:, 0:4]),
                     r=[P(1)], w=['modT'])
            else:
                dstt, dk = (bcg, 'bcg') if which == 2 else (G2, 'G2')
                t.op('pe', lambda e: e.matmul(ps[2][:, :], lhsT=onesf[0:1, 0:128], rhs=mrow[0:1, :],
                                              start=True, stop=True), r=['mrow', 'onesf'], w=[P(2)])
                t.op('act', lambda e: e.activation(out=dstt[:, q * 512:(q + 1) * 512], in_=ps[2][:, :], func=AF.Identity),
                     r=[P(2)], w=[dk])
        t.op('dve', lambda e: e.scalar_tensor_tensor(out=A1[:], in0=modT[:, 1, :], scalar=1.0, in1=pv[:, PV_G1:PV_G1 + 16],
                                                     op0=ALU.add, op1=ALU.mult), r=['modT', 'pv'], w=['A1'])
        t.op('dve', lambda e: e.scalar_tensor_tensor(out=A2[:], in0=modT[:, 3, :], scalar=1.0, in1=pv[:, PV_G2:PV_G2 + 16],
                                                     op0=ALU.add, op1=ALU.mult), r=['modT', 'pv'], w=['A2'])
        t.op('dve', lambda e: e.tensor_tensor(out=gb1[:], in0=bcg[:], in1=rvt[:, RV_BOUT:RV_BOUT + D], op=ALU.mult),
             r=['bcg', 'rvt'], w=['gb1'])
        t.dma('sp', lambda e: e.dma_start(out=g1s_d[0], in_=bcg[:]), r=['bcg'], w=['g1sd'])
        t.dma('sp', lambda e: e.dma_start(out=g1s_d[1], in_=gb1[:]), r=['gb1'], w=['g1sd'])
        tap('modT', modT, [128, 4, 16], F32, 'modT')
        tap('A1', A1, [128, 16], F32, 'A1')
        tap('bc0', bcg, [128, D], F32, 'bcg')
        tap('ebt', ebt, [128, 4096], BF16, 'ebt')
        t.barrier()
        if STOP == 'ada':
            return nc

    win_v = win_d.rearrange("(kc p) f -> p kc f", p=128)
    wout_v = wout_d.rearrange("(kc p) f -> p kc f", p=128)

    def rsqrt_into(dst, src_ap, scale, r, w):
        t.op('act', lambda e: e.activation(out=dst, in_=src_ap, func=AF.Sqrt, bias=EPS, scale=scale), r=r, w=w)
        t.op('dve', lambda e: e.reciprocal(out=dst, in_=dst), r=w, w=w)

    for hf in HALVES:
        with ExitStack() as _esh:
            h2T = _esh.enter_context(SBT("h2T", [128, 16, 1024], BF16))
            wt = _esh.enter_context(SBT("wt", [128, 8, 32], F32))
            with ExitStack() as _esm:
                mixedT = _esm.enter_context(SBT("mixedT", [128, 16, 1024], BF16))
                with ExitStack() as _es:
                    qT = _es.enter_context(SBT("qT", [128, 8, 1152], BF16))
                    kT = _es.enter_context(SBT("kT", [128, 1152], BF16))
                    v1 = _es.enter_context(SBT("v1", [128, 9, 2, 65], BF16))
                    hglu = _es.enter_context(SBT("hglu", [128, 8, 1152], BF16))
                    t.op('pool', lambda e: e.memset(v1[:], 1.0), w=['v1'])
                    with ExitStack() as _es:
                        hT = _es.enter_context(SBT("hT", [128, 16, 1152], BF16))
                        xb0 = _es.enter_context(SBT("xb0", [128, D], F32))
                        xs = _es.enter_context(SBT("xs", [128, D], BF16))
                        wi0 = _es.enter_context(SBT("wi0", [128, 16, 256], BF16))
                        wi1 = _es.enter_context(SBT("wi1", [128, 16, 256], BF16))
                        tA = xb0[:, 0:512]
                        tB = xs[:, 0:512]
                        tC = xb0[:, 512:1024]
                        xb = [xb0, xb0]; junk = xs
                        wi = [wi0, wi1]
                        for tt in range(9):
                            xt = xb[tt % 2]
                            xk = 'xb0'
                            r0 = (hf * 8 + tt) * 128
                            t.dma('sp', lambda e: e.dma_start(out=xt[:], in_=xh_d[r0:r0 + 128, :]), w=[xk])
                            t.op('dve', lambda e: e.memset(ssq[:], 0.0), w=['ssq'])
                            t.op('act', lambda e: e.activation(out=junk[:], in_=xt[:], func=AF.Square, accum_out=ssq[:, 0:1]),
                                 r=[xk, 'ssq'], w=['xs', 'ssq'])
                            rsqrt_into(rstd[:], ssq[:], 1.0 / D, ['ssq'], ['rstd'])
                            t.op('dve', lambda e: e.tensor_scalar(out=xs[:], in0=xt[:], scalar1=rstd[:, 0:1], scalar2=None, op0=ALU.mult),
                                 r=[xk, 'rstd'], w=['xs'])
                            for c in range(16):
                                t.op('pe', lambda e: e.transpose(out=psT[c // 8][:, (c % 8) * 128:(c % 8 + 1) * 128],
                                                                 in_=xs[:, c * 128:(c + 1) * 128], identity=identb[:]),
                                     r=['xs', 'identb'], w=[f'psT{c // 8}'])
                            for c in range(16):
                                t.op('dve', lambda e: e.tensor_scalar(out=hT[:, c, tt * 128:(tt + 1) * 128],
                                                                      in0=psT[c // 8][:, (c % 8) * 128:(c % 8 + 1) * 128],
                                                                      scalar1=A1[:, c:c + 1], scalar2=modT[:, 0, c:c + 1],
                                                                      op0=ALU.mult, op1=ALU.add),
                                     r=[f'psT{c // 8}', 'A1', 'modT'], w=['hT'])
                        if STOP == 'norm':
                            tap(f'hT{hf}', hT, [128, 16, 1152], BF16, 'hT')
                            t.barrier()
                            return nc
                        t.barrier()
                        chunks = [(0, 128), (128, 512), (640, 512)]
                        for j in dbg.get('jlist', range(26)):
                            wc = j // 2
                            b = wc % 2
                            if j % 2 == 0 or 'jlist' in dbg:
                                t.dma('pool', lambda e: e.dma_start(out=wi[b][:], in_=win_v[:, :, wc * 256:wc * 256 + 256]),
                                      w=[f'wi{b}'])
                            jo = (j % 2) * 128
                            bias = pv[:, PV_BIN + j:PV_BIN + j + 1]
                            for ci, (c0, cn) in enumerate(chunks):
                                pb = ps[ci % 2]
                                for kc in range(0 if dbg.get('nomm') else 16):
                                    t.op('pe', lambda e: e.matmul(pb[:, 0:cn], lhsT=wi[b][:, kc, jo:jo + 128], rhs=hT[:, kc, c0:c0 + cn],
                                                                  start=(kc == 0), stop=(kc == 15)), r=[f'wi{b}', 'hT'], w=[P(ci % 2)])
                                if dbg.get('noevac'):
                                    continue
                                if j < 9:
                                    gcol = PV_GQ if j < 8 else PV_GK
                                    dst = qT[:, j, c0:c0 + cn] if j < 8 else kT[:, c0:c0 + cn]
                                    dk = 'qT' if j < 8 else 'kT'
                                    t.op('act', lambda e: e.activation(out=tB[:, 0:cn], in_=pb[:, 0:cn], func=AF.Square, bias=bias),
                                         r=[P(ci % 2), 'pv'], w=['tB'])
                                    t.op('act', lambda e: e.activation(out=tA[:, 0:cn], in_=pb[:, 0:cn], func=AF.Identity, bias=bias),
                                         r=[P(ci % 2), 'pv'], w=['tA'])
                                    t.op('pe', lambda e: e.matmul(ps[2][:, 0:cn], lhsT=blkb[:], rhs=tB[:, 0:cn], start=True, stop=True),
                                         r=['blkb', 'tB'], w=[P(2)])
                                    rsqrt_into(tC[:, 0:cn], ps[2][:, 0:cn], 1.0 / 64, [P(2)], ['tC'])
                                    t.op('dve', lambda e: e.scalar_tensor_tensor(out=dst, in0=tA[:, 0:cn], scalar=pv[:, gcol:gcol + 1],
                                                                                 in1=tC[:, 0:cn], op0=ALU.mult, op1=ALU.mult),
                                         r=['tA', 'tC', 'pv'], w=[dk])
                                elif j == 9:
                                    t.op('act', lambda e: e.activation(out=tB[:, 0:cn], in_=pb[:, 0:cn], func=AF.Identity, bias=bias),
                                         r=[P(ci % 2), 'pv'], w=['tB'])
                                    for s_ in range(cn // 128):
                                        tt = c0 // 128 + s_
                                        t.op('pe', lambda e: e.transpose(out=psT[0][:, 0:128], in_=tB[:, s_ * 128:(s_ + 1) * 128],
                                                                         identity=identb[:]), r=['tB', 'identb'], w=['psT0'])
                                        t.op('dve', lambda e: e.tensor_copy(out=v1[:, tt, :, 0:64],
                                                                            in_=psT[0][:, 0:128].rearrange("p (g d) -> p g d", g=2)),
                                             r=['psT0'], w=['v1'])
                                elif j < 18:
                                    c = j - 10
                                    t.op('dve', lambda e: e.tensor_scalar(out=hglu[:, c, c0:c0 + cn], in0=pb[:, 0:cn], scalar1=bias,
                                                                          scalar2=None, op0=ALU.add), r=[P(ci % 2), 'pv'], w=['hglu'])
                                else:
                                    c = j - 18
                                    t.op('act', lambda e: e.activation(out=tB[:, 0:cn], in_=pb[:, 0:cn], func=AF.Sigmoid, bias=bias),
                                         r=[P(ci % 2), 'pv'], w=['tB'])
                                    t.op('pool', lambda e: e.tensor_tensor(out=hglu[:, c, c0:c0 + cn], in0=hglu[:, c, c0:c0 + cn],
                                                                           in1=tB[:, 0:cn], op=ALU.mult), r=['tB', 'hglu'], w=['hglu'])
                        if hf == 0 and not dbg.get('nohv'):
                            for c in range(8):
                                t.op('pool', lambda e: e.tensor_scalar(out=hglu[:, c, 0:128], in0=hglu[:, c, 0:128], scalar1=hv[:, 0:1],
                                                                       scalar2=None, op0=ALU.mult), r=['hglu', 'hv'], w=['hglu'])
                        tap(f'hT{hf}', hT, [128, 16, 1152], BF16, 'hT')
                        tap(f'qT{hf}', qT, [128, 8, 1152], BF16, 'qT')
                        tap(f'kT{hf}', kT, [128, 1152], BF16, 'kT')
                        tap(f'v1{hf}', v1, [128, 9, 2, 65], BF16, 'v1')
                        tap(f'hglu{hf}', hglu, [128, 8, 1152], BF16, 'hglu')
                        t.barrier()
                        if STOP == 'inproj':
                            return nc

                    with ExitStack() as _es:
                        acc = _es.enter_context(SBT("acc", [128, 8, 1024], F32))
                        sq = _es.enter_context(SBT("sq", [128, 512], F32))
                        mean = _es.enter_context(SBT("mean", [128, 512], F32))
                        var = _es.enter_context(SBT("var", [128, 512], F32))
                        tmp = _es.enter_context(SBT("tmp", [128, 512], F32))
                        yatt = _es.enter_context(SBT("yatt", [128, 1024], F32))
                        ynb = _es.enter_context(SBT("ynb", [128, 1024], BF16))
                        pe0 = _es.enter_context(SBT("pe0", [128, 512], F32))
                        pe1 = _es.enter_context(SBT("pe1", [128, 512], F32))
                        pT0 = _es.enter_context(SBT("pT0", [128, 1024], BF16))
                        pT1 = _es.enter_context(SBT("pT1", [128, 1024], BF16))
                        den = _es.enter_context(SBT("den", [128, 8], F32))
                        junk2 = _es.enter_context(SBT("junk2", [128, 1024], BF16))
                        for c in range(8):
                            en = 'dve'
                            ak = f'acc{c}'
                            wc0 = PV_WDW + c * 31
                            t.op(en, lambda e: e.tensor_scalar(out=acc[:, c, :], in0=hglu[:, c, 98:98 + 1024], scalar1=pv[:, wc0:wc0 + 1],
                                                               scalar2=pv[:, PV_BDW + c:PV_BDW + c + 1], op0=ALU.mult, op1=ALU.add),
                                 r=['hglu', 'pv'], w=[ak])
                            for j in range(1, 31):
                                if en == 'dve':
                                    t.op(en, lambda e: e.scalar_tensor_tensor(out=acc[:, c, :], in0=hglu[:, c, 98 + j:98 + j + 1024],
                                                                              scalar=pv[:, wc0 + j:wc0 + j + 1], in1=acc[:, c, :],
                                                                              op0=ALU.mult, op1=ALU.add), r=['hglu', 'pv', ak], w=[ak])
                                else:
                                    t.op(en, lambda e: e.tensor_scalar(out=ctmp[:], in0=hglu[:, c, 98 + j:98 + j + 1024],
                                                                       scalar1=pv[:, wc0 + j:wc0 + j + 1], scalar2=None, op0=ALU.mult),
                                         r=['hglu', 'pv'], w=['ctmp'])
                                    t.op(en, lambda e: e.tensor_tensor(out=acc[:, c, :], in0=acc[:, c, :], in1=ctmp[:], op=ALU.add),
                                         r=['ctmp', ak], w=[ak])
                        for ch in range(2):
                            cs = slice(ch * 512, (ch + 1) * 512)
                            for c in range(8):
                                t.op('act', lambda e: e.activation(out=sq[:], in_=acc[:, c, cs], func=AF.Square), r=[f'acc{c}'], w=['sq'])
                                t.op('pe', lambda e: e.matmul(ps[0][:], lhsT=onesf[:], rhs=acc[:, c, cs], start=(c == 0), stop=(c == 7)),
                                     r=['onesf', f'acc{c}'], w=[P(0)])
                                t.op('pe', lambda e: e.matmul(ps[1][:], lhsT=onesf[:], rhs=sq[:], start=(c == 0), stop=(c == 7)),
                                     r=['onesf', 'sq'], w=[P(1)])
                            t.op('dve', lambda e: e.tensor_scalar(out=mean[:], in0=ps[0][:], scalar1=1.0 / 1024, scalar2=None, op0=ALU.mult),
                                 r=[P(0)], w=['mean'])
                            t.op('dve', lambda e: e.tensor_tensor(out=tmp[:], in0=mean[:], in1=mean[:], op=ALU.mult), r=['mean'], w=['tmp'])
                            t.op('dve', lambda e: e.scalar_tensor_tensor(out=var[:], in0=ps[1][:], scalar=1.0 / 1024, in1=tmp[:],
                                                                         op0=ALU.mult, op1=ALU.subtract), r=[P(1), 'tmp'], w=['var'])
                            rsqrt_into(var[:], var[:], 1.0, ['var'], ['var'])
                            for c in range(8):
                                ak = f'acc{c}'
                                t.op('dve', lambda e: e.tensor_tensor(out=tmp[:], in0=acc[:, c, cs], in1=mean[:], op=ALU.subtract),
                                     r=[ak, 'mean'], w=['tmp'])
                                t.op('dve', lambda e: e.tensor_tensor(out=tmp[:], in0=tmp[:], in1=var[:], op=ALU.mult), r=['tmp', 'var'], w=['tmp'])
                                t.op('act', lambda e: e.activation(out=acc[:, c, cs], in_=tmp[:], func=AF.Silu,
                                                                   bias=pv[:, PV_LNB + c:PV_LNB + c + 1], scale=pv[:, PV_LNG + c:PV_LNG + c + 1]),
                                     r=['tmp', 'pv'], w=[ak])
                                t.op('act', lambda e: e.activation(out=sq[:], in_=acc[:, c, cs], func=AF.Square), r=[ak], w=['sq'])
                                t.op('pe', lambda e: e.matmul(ps[2][:], lhsT=onesf[:], rhs=sq[:], start=(c == 0), stop=(c == 7)),
                                     r=['onesf', 'sq'], w=[P(2)])
                            rsqrt_into(var[:], ps[2][:], 1.0 / 1024, [P(2)], ['var'])
                            for c in range(8):
                                t.op('dve', lambda e: e.scalar_tensor_tensor(out=mixedT[:, 8 + c, cs], in0=acc[:, c, cs],
                                                                             scalar=pv[:, PV_GOC + c:PV_GOC + c + 1], in1=var[:],
                                                                             op0=ALU.mult, op1=ALU.mult), r=[f'acc{c}', 'var', 'pv'], w=['mixedT'])

                        pes = [pe0, pe1]
                        pTs = [pT0, pT1]
                        for n in range(8):
                            tt = n + 1
                            for g in range(2):
                                gp = slice(g * 64, (g + 1) * 64)
                                for kt in range(2):
                                    kc0 = (tt - 1 + kt) * 128
                                    for hh in range(2):
                                        pi = 2 + hh
                                        t.op('pe', lambda e: e.matmul(ps[pi][:], lhsT=kT[gp, kc0:kc0 + 128],
                                                                      rhs=qT[gp, 4 * hh:4 * hh + 4, tt * 128:(tt + 1) * 128],
                                                                      start=True, stop=True), r=['kT', 'qT'], w=[P(pi)])
                                        t.op('act', lambda e: e.activation(out=pes[hh][:], in_=ps[pi][:], func=AF.Exp, scale=0.125),
                                             r=[P(pi)], w=[f'pe{hh}'])
                                        if kt == 0 and n == 0 and hf == 0:
                                            eb = ebt0[:, g * 1024 + hh * 512:g * 1024 + (hh + 1) * 512]
                                        else:
                                            o = kt * 2048 + g * 1024 + hh * 512
                                            eb = ebt[:, o:o + 512]
                                        t.op('dve', lambda e: e.tensor_tensor(out=pTs[kt][:, hh * 512:(hh + 1) * 512], in0=pes[hh][:], in1=eb,
                                                                              op=ALU.mult), r=[f'pe{hh}', 'ebt', 'ebt0'], w=[f'pT{kt}'])
                                for jj in range(8):
                                    pi = 4 + jj // 4
                                    oc = (jj % 4) * 65
                                    for kt in range(2):
                                        t.op('pe', lambda e: e.matmul(ps[pi][:, oc:oc + 65], lhsT=pTs[kt][:, jj * 128:(jj + 1) * 128],
                                                                      rhs=v1[:, tt - 1 + kt, g, :], start=(kt == 0), stop=(kt == 1)),
                                             r=[f'pT{kt}', 'v1'], w=[P(pi)])
                                for half in range(2):
                                    pi = 4 + half
                                    pv3 = ps[pi][:, 0:260].rearrange("p (h d) -> p h d", d=65)
                                    hs = 8 * g + 4 * half
                                    t.op('dve', lambda e: e.tensor_tensor(out=den[:, 0:4], in0=pv3[:, :, 64], in1=esink[:, hs:hs + 4], op=ALU.add),
                                         r=[P(pi), 'esink'], w=['den'])
                                    t.op('dve', lambda e: e.reciprocal(out=den[:, 0:4], in_=den[:, 0:4]), r=['den'], w=['den'])
                                    for j4 in range(4):
                                        h = hs + j4
                                        t.op('dve', lambda e: e.tensor_scalar(out=yatt[:, h * 64:(h + 1) * 64], in0=pv3[:, j4, 0:64],
                                                                              scalar1=den[:, j4:j4 + 1], scalar2=None, op0=ALU.mult),
                                             r=[P(pi), 'den'], w=['yatt'])
                            t.op('dve', lambda e: e.memset(ssq[:], 0.0), w=['ssq'])
                            t.op('act', lambda e: e.activation(out=junk2[:], in_=yatt[:], func=AF.Square, accum_out=ssq[:, 0:1]),
                                 r=['yatt', 'ssq'], w=['junk2', 'ssq'])
                            rsqrt_into(rstd[:], ssq[:], 1.0 / 1024, ['ssq'], ['rstd'])
                            t.op('dve', lambda e: e.scalar_tensor_tensor(out=ynb[:], in0=yatt[:], scalar=rstd[:, 0:1], in1=goa[:],
                                                                         op0=ALU.mult, op1=ALU.mult), r=['yatt', 'rstd', 'goa'], w=['ynb'])
                            for c in range(8):
                                t.op('pe', lambda e: e.transpose(out=psT[1][:, c * 128:(c + 1) * 128], in_=ynb[:, c * 128:(c + 1) * 128],
                                                                 identity=identb[:]), r=['ynb', 'identb'], w=['psT1'])
                            t.op('dve', lambda e: e.tensor_copy(out=mixedT[:, 0:8, n * 128:(n + 1) * 128],
                                                                in_=psT[1][:, :].rearrange("p (c q) -> p c q", c=8)), r=['psT1'], w=['mixedT'])
                        tap(f'mixedT{hf}', mixedT, [128, 16, 1024], BF16, 'mixedT')
                        t.barrier()
                        if STOP == 'mixer':
                            return nc


                with ExitStack() as _es:
                    wo = _es.enter_context(SBT("wo", [128, 16, D], BF16))
                    G1 = _es.enter_context(SBT("G1", [128, D], F32))
                    GB1 = _es.enter_context(SBT("GB1", [128, D], F32))
                    xt2 = _es.enter_context(SBT("xt2", [128, D], F32))
                    x1 = _es.enter_context(SBT("x1", [128, D], F32))
                    h2b = _es.enter_context(SBT("h2b", [128, D], BF16))
                    lg = _es.enter_context(SBT("lg", [128, 32], F32))
                    top8 = _es.enter_context(SBT("top8", [128, 8], F32))
                    msk = _es.enter_context(SBT("msk", [128, 32], F32))
                    sm = _es.enter_context(SBT("sm", [128, 2], F32))
                    t.dma('sp', lambda e: e.dma_start(out=G1[:], in_=g1s_d[0]), r=['g1sd'], w=['G1'])
                    t.dma('sp', lambda e: e.dma_start(out=GB1[:], in_=g1s_d[1]), r=['g1sd'], w=['GB1'])
                    for q in range(4):
                        t.dma('pool', lambda e: e.dma_start(out=wo[:, :, q * 512:(q + 1) * 512], in_=wout_v[:, :, q * 512:(q + 1) * 512]),
                              w=['wo'])
                    for lt in range(8):
                        gt = hf * 8 + lt
                        t.dma('sp', lambda e: e.dma_start(out=xt2[:], in_=xh_d[(gt + 1) * 128:(gt + 2) * 128, :]), w=['xt2'])
                        t.op('pool', lambda e: e.tensor_tensor(out=xt2[:], in0=xt2[:], in1=GB1[:], op=ALU.add), r=['xt2', 'GB1'], w=['xt2'])
                        for nq in range(4):
                            for cc in range(16):
                                t.op('pe', lambda e: e.matmul(ps[nq][:], lhsT=mixedT[:, cc, lt * 128:(lt + 1) * 128],
                                                              rhs=wo[:, cc, nq * 512:(nq + 1) * 512], start=(cc == 0), stop=(cc == 15)),
                                     r=['mixedT', 'wo'], w=[P(nq)])
                            ns = slice(nq * 512, (nq + 1) * 512)
                            t.op('dve', lambda e: e.tensor_tensor(out=x1[:, ns], in0=ps[nq][:], in1=G1[:, ns], op=ALU.mult),
                                 r=[P(nq), 'G1'], w=['x1'])
                        t.op('dve', lambda e: e.tensor_tensor(out=x1[:], in0=x1[:], in1=xt2[:], op=ALU.add), r=['x1', 'xt2'], w=['x1'])
                        t.dma('sp', lambda e: e.dma_start(out=x1_d[gt * 128:(gt + 1) * 128, :], in_=x1[:]), r=['x1'], w=['x1d'])
                        t.op('dve', lambda e: e.memset(ssq[:], 0.0), w=['ssq'])
                        t.op('act', lambda e: e.activation(out=h2b[:], in_=x1[:], func=AF.Square, accum_out=ssq[:, 0:1]),
                             r=['x1', 'ssq'], w=['h2b', 'ssq'])
                        rsqrt_into(rstd[:], ssq[:], 1.0 / D, ['ssq'], ['rstd'])
                        t.op('dve', lambda e: e.tensor_scalar(out=h2b[:], in0=x1[:], scalar1=rstd[:, 0:1], scalar2=None, op0=ALU.mult),
                             r=['x1', 'rstd'], w=['h2b'])
                        for cc in range(16):
                            t.op('pe', lambda e: e.transpose(out=psT[cc // 8][:, (cc % 8) * 128:(cc % 8 + 1) * 128],
                                                             in_=h2b[:, cc * 128:(cc + 1) * 128], identity=identb[:]),
                                 r=['h2b', 'identb'], w=[f'psT{cc // 8}'])
                        for cc in range(16):
                            pin = psT[cc // 8][:, (cc % 8) * 128:(cc % 8 + 1) * 128]
                            if cc % 2 == 0:
                                t.op('dve', lambda e: e.tensor_scalar(out=h2T[:, cc, lt * 128:(lt + 1) * 128], in0=pin,
                                                                      scalar1=A2[:, cc:cc + 1], scalar2=modT[:, 2, cc:cc + 1],
                                                                      op0=ALU.mult, op1=ALU.add),
                                     r=[f'psT{cc // 8}', 'A2', 'modT'], w=['h2T'])
                            else:
                                t.op('act', lambda e: e.activation(out=h2T[:, cc, lt * 128:(lt + 1) * 128], in_=pin, func=AF.Identity,
                                                                   bias=modT[:, 2, cc:cc + 1], scale=A2[:, cc:cc + 1]),
                                     r=[f'psT{cc // 8}', 'A2', 'modT'], w=['h2T'])
                        for cc in range(16):
                            t.op('pe', lambda e: e.matmul(ps[5][:, 0:32], lhsT=h2T[:, cc, lt * 128:(lt + 1) * 128],
                                                          rhs=wrb[:, cc * 32:(cc + 1) * 32],
                                                          start=(cc == 0), stop=(cc == 15)), r=['h2T', 'wrb'], w=[P(5)])
                        t.op('dve', lambda e: e.tensor_tensor(out=lg[:], in0=ps[5][:, 0:32], in1=brt[:], op=ALU.add), r=[P(5), 'brt'], w=['lg'])
                        t.op('dve', lambda e: e.max(out=top8[:], in_=lg[:]), r=['lg'], w=['top8'])
                        t.op('dve', lambda e: e.tensor_scalar(out=msk[:], in0=lg[:], scalar1=top8[:, 3:4], scalar2=None, op0=ALU.is_ge),
                             r=['lg', 'top8'], w=['msk'])
                        t.op('dve', lambda e: e.tensor_scalar(out=sm[:, 0:1], in0=top8[:, 0:1], scalar1=-1.0, scalar2=None, op0=ALU.mult),
                             r=['top8'], w=['sm'])
                        t.op('act', lambda e: e.activation(out=lg[:], in_=lg[:], func=AF.Exp, bias=sm[:, 0:1]), r=['lg', 'sm'], w=['lg'])
                        t.op('dve', lambda e: e.tensor_tensor(out=lg[:], in0=lg[:], in1=msk[:], op=ALU.mult), r=['lg', 'msk'], w=['lg'])
                        t.op('dve', lambda e: e.reduce_sum(out=sm[:, 1:2], in_=lg[:], axis=AX.X), r=['lg'], w=['sm'])
                        t.op('dve', lambda e: e.reciprocal(out=sm[:, 1:2], in_=sm[:, 1:2]), r=['sm'], w=['sm'])
                        t.op('dve', lambda e: e.tensor_scalar(out=wt[:, lt, :], in0=lg[:], scalar1=sm[:, 1:2], scalar2=None, op0=ALU.mult),
                             r=['lg', 'sm'], w=['wt'])
                    t.barrier()
                    if STOP == 'outproj':
                        return nc

            with ExitStack() as _es:
                yacc = _es.enter_context(SBT("yacc", [128, 8, D], F32))
                actT = _es.enter_context(SBT("actT", [128, 16, 1024], BF16))
                wm0 = _es.enter_context(SBT("wm0", [128, 16, 256], BF16))
                wm1 = _es.enter_context(SBT("wm1", [128, 16, 256], BF16))
                wm2 = _es.enter_context(SBT("wm2", [128, 16, 256], BF16))
                wm3 = _es.enter_context(SBT("wm3", [128, 16, 256], BF16))
                bdb0 = _es.enter_context(SBT("bdb0", [128, 256], F32))
                bdb1 = _es.enter_context(SBT("bdb1", [128, 256], F32))
                g1 = _es.enter_context(SBT("g1", [128, 512], F32))
                sg = _es.enter_context(SBT("sg", [128, 512], F32))
                u1 = _es.enter_context(SBT("u1", [128, 512], F32))
                wm = [wm0, wm1, wm2, wm3]
                bdb = [bdb0, bdb1]
                wmi = 0
                t.op('dve', lambda e: e.memset(yacc[:], 0.0), w=['yacc'])
                for ex in range(NEXP):
                    wgv = wg_d[ex].rearrange("(kc p) f -> p kc f", p=128)
                    wuv = wu_d[ex].rearrange("(kc p) f -> p kc f", p=128)
                    wdv = wd_d[ex].rearrange("(kc p) f -> p kc f", p=128)
                    for qf in range(8):
                        bg_, bu_ = wmi % 4, (wmi + 1) % 4
                        wmi += 2
                        t.dma('pool', lambda e: e.dma_start(out=wm[bg_][:], in_=wgv[:, :, qf * 256:(qf + 1) * 256]), w=[f'wm{bg_}'])
                        t.dma('pool', lambda e: e.dma_start(out=wm[bu_][:], in_=wuv[:, :, qf * 256:(qf + 1) * 256]), w=[f'wm{bu_}'])
                        for ft in range(2):
                            f = qf * 2 + ft
                            bgc = pv[:, PV_BG + ex * 16 + f:PV_BG + ex * 16 + f + 1]
                            buc = pv[:, PV_BU + ex * 16 + f:PV_BU + ex * 16 + f + 1]
                            for ch in range(2):
                                cs = slice(ch * 512, (ch + 1) * 512)
                                pg, pu = ps[2 * ch], ps[2 * ch + 1]
                                kg, ku = P(2 * ch), P(2 * ch + 1)
                                for kc in range(16):
                                    t.op('pe', lambda e: e.matmul(pg[:], lhsT=wm[bg_][:, kc, ft * 128:(ft + 1) * 128], rhs=h2T[:, kc, cs],
                                                                  start=(kc == 0), stop=(kc == 15)), r=[f'wm{bg_}', 'h2T'], w=[kg])
                                for kc in range(16):
                                    t.op('pe', lambda e: e.matmul(pu[:], lhsT=wm[bu_][:, kc, ft * 128:(ft + 1) * 128], rhs=h2T[:, kc, cs],
                                                                  start=(kc == 0), stop=(kc == 15)), r=[f'wm{bu_}', 'h2T'], w=[ku])
                                t.op('dve', lambda e: e.tensor_scalar(out=g1[:], in0=pg[:], scalar1=bgc, scalar2=7.0, op0=ALU.add, op1=ALU.min),
                                     r=[kg, 'pv'], w=['g1'])
                                t.op('act', lambda e: e.activation(out=sg[:], in_=g1[:], func=AF.Sigmoid, scale=1.702), r=['g1'], w=['sg'])
                                t.op('dve', lambda e: e.tensor_scalar(out=u1[:], in0=pu[:], scalar1=buc, scalar2=7.0, op0=ALU.add, op1=ALU.min),
                                     r=[ku, 'pv'], w=['u1'])
                                t.op('dve', lambda e: e.tensor_scalar(out=u1[:], in0=u1[:], scalar1=-7.0, scalar2=1.0, op0=ALU.max, op1=ALU.add),
                                     r=['u1'], w=['u1'])
                                t.op('pool', lambda e: e.tensor_tensor(out=g1[:], in0=g1[:], in1=sg[:], op=ALU.mult), r=['g1', 'sg'], w=['g1'])
                                t.op('dve', lambda e: e.tensor_tensor(out=actT[:, f, cs], in0=g1[:], in1=u1[:], op=ALU.mult),
                                     r=['g1', 'u1'], w=['actT'])
                    for dq in range(8):
                        bd_ = wmi % 4
                        wmi += 1
                        bb = dq % 2
                        ds_ = slice(dq * 256, (dq + 1) * 256)
                        t.dma('pool', lambda e: e.dma_start(out=wm[bd_][:], in_=wdv[:, :, ds_]), w=[f'wm{bd_}'])
                        t.dma('sp', lambda e: e.dma_start(out=bdb[bb][:], in_=bd_d[ex:ex + 1, ds_].partition_broadcast(128)),
                              w=[f'bdb{bb}'])
                        for ti in range(8):
                            pd, kd = ps[4 + ti % 2], P(4 + ti % 2)
                            for fc in range(16):
                                t.op('pe', lambda e: e.matmul(pd[:, 0:256], lhsT=actT[:, fc, ti * 128:(ti + 1) * 128], rhs=wm[bd_][:, fc, :],
                                                              start=(fc == 0), stop=(fc == 15)), r=['actT', f'wm{bd_}'], w=[kd])
                            t.op('dve', lambda e: e.tensor_tensor(out=sg[:, 0:256], in0=pd[:, 0:256], in1=bdb[bb][:], op=ALU.add),
                                 r=[kd, f'bdb{bb}'], w=['sg'])
                            t.op('dve', lambda e: e.scalar_tensor_tensor(out=yacc[:, ti, ds_], in0=sg[:, 0:256], scalar=wt[:, ti, ex:ex + 1],
                                                                         in1=yacc[:, ti, ds_], op0=ALU.mult, op1=ALU.add),
                                 r=['sg', 'wt', 'yacc'], w=['yacc'])
                for ti in range(8):
                    gt = hf * 8 + ti
                    for q in range(4):
                        qs = slice(q * 512, (q + 1) * 512)
                        t.dma('sp', lambda e: e.dma_start(out=g1[:], in_=x1_d[gt * 128:(gt + 1) * 128, qs]), r=['x1d'], w=['g1'])
                        t.op('dve', lambda e: e.tensor_tensor(out=u1[:], in0=yacc[:, ti, qs], in1=G2[:, qs], op=ALU.mult),
                             r=['yacc', 'G2'], w=['u1'])
                        t.op('dve', lambda e: e.tensor_tensor(out=g1[:], in0=g1[:], in1=u1[:], op=ALU.add), r=['g1', 'u1'], w=['g1'])
                        t.dma('sp', lambda e: e.dma_start(out=out_d[gt * 128:(gt + 1) * 128, qs], in_=g1[:]), r=['g1'], w=['outd'])
                t.barrier()
    return nc


def _t5_bucket(dist):
    max_exact = 16
    d = np.maximum(dist, 0)
    lr = np.log(np.maximum(d, max_exact).astype(np.float32) / max_exact)
    large = max_exact + (lr / math.log(128 / max_exact) * (32 - max_exact)).astype(np.int32)
    large = np.minimum(large, 31)
    return np.where(d < max_exact, d, large)


def _prep(inputs):
    f = np.float32
    x = np.asarray(inputs["x"], f)[0]
    g = lambda k: np.asarray(inputs[k], f)[0]
    pp = lambda v: np.ascontiguousarray(v.reshape(-1, 128).T)
    perm = []
    for jj in range(8):
        perm += list(range(jj * 64, jj * 64 + 64)) + list(range((8 + jj) * 64, (8 + jj) * 64 + 64))
    perm += list(range(1024, 3328))
    perm = np.array(perm)
    win = np.ascontiguousarray(g("w_in")[:, perm])
    b_in = g("b_in")[perm]
    pv = np.zeros((128, NPV), f)
    pv[:, PV_C:PV_C + 16] = pp(np.asarray(inputs["c"], f)[0])
    pv[:, PV_G1:PV_G1 + 16] = pp(g("g_norm1"))
    pv[:, PV_BIN:PV_BIN + 26] = pp(b_in)
    pv[:, PV_GQ] = np.tile(g("g_q"), 2)
    pv[:, PV_GK] = np.tile(g("g_k"), 2)
    pv[:, PV_WDW:PV_WDW + 248] = g("w_dw").reshape(31, 8, 128).transpose(2, 1, 0).reshape(128, 248)
    pv[:, PV_BDW:PV_BDW + 8] = pp(g("b_dw"))
    pv[:, PV_LNG:PV_LNG + 8] = pp(g("ln_g"))
    pv[:, PV_LNB:PV_LNB + 8] = pp(g("ln_b"))
    pv[:, PV_GOC:PV_GOC + 8] = pp(g("g_out_conv"))
    pv[:, PV_BG:PV_BG + 512] = g("b_gate").reshape(32, 16, 128).transpose(2, 0, 1).reshape(128, 512)
    pv[:, PV_BU:PV_BU + 512] = g("b_up").reshape(32, 16, 128).transpose(2, 0, 1).reshape(128, 512)
    pv[:, PV_G2:PV_G2 + 16] = pp(g("g_norm2"))
    pv[:, PV_WR:PV_WR + 512] = g("w_router").reshape(16, 128, 32).transpose(1, 0, 2).reshape(128, 512)
    rv = np.zeros((1, NRV), f)
    rv[0, RV_GOA:RV_GOA + 1024] = g("g_out_attn")
    rv[0, RV_BOUT:RV_BOUT + D] = g("b_out")
    rv[0, RV_G2:RV_G2 + D] = g("g_norm2")
    rv[0, RV_BR:RV_BR + 32] = g("b_router")
    rv[0, RV_SINK:RV_SINK + 16] = g("sinks")
    rv = np.ascontiguousarray(np.broadcast_to(rv, (128, NRV)))
    rb = np.asarray(inputs["rel_bias"], f)
    kk = np.arange(128)[:, None]
    qq = np.arange(128)[None, :]
    bt = np.zeros((128, 2, 2, 8, 128), f)
    for kt in range(2):
        dist = qq + 128 - kk if kt == 0 else qq - kk
        valid = (dist >= 0) & (dist < 128)
        bk = _t5_bucket(dist)
        for gg in range(2):
            for jj in range(8):
                bt[:, kt, gg, jj, :] = np.where(valid, rb[bk, 8 * gg + jj], f(-30000.0))
    bt = bt.reshape(128, 4096)
    blk = np.zeros((128, 128), f)
    blk[:64, :64] = 1
    blk[64:, 64:] = 1
    common = dict(pv=pv, rv=rv, bada=g("b_ada")[None, :], wada=g("w_ada"), win=win, wout=g("w_out"), biast=bt,
                  wg=g("w_gate"), wu=g("w_up"), wd=g("w_down"), bd=g("b_down"),
                  identf=np.eye(128, dtype=f), blk=blk)
    in_maps = []
    for c in range(NCORE):
        xh = np.zeros((17 * 128, D), f)
        if c == 0:
            xh[128:] = x[0:TOK]
        else:
            xh[:] = x[c * TOK - 128:(c + 1) * TOK]
        hvv = np.full((128, 1), 0.0 if c == 0 else 1.0, f)
        m = dict(common)
        m["xh"] = xh
        m["hv"] = hvv
        in_maps.append(m)
    return in_maps


def kernel(**inputs):
    in_maps = _prep(inputs)
    nc = build_nc()
    res = run_bass_kernel_spmd(nc, in_maps, core_ids=list(range(NCORE)))
    out = np.concatenate([np.asarray(r["out"], np.float32) for r in res.results], axis=0)
    return out.reshape(1, NCORE * TOK, D)
```

```python
import math
from contextlib import ExitStack
import numpy as np
import concourse.bass as bass
import concourse.mybir as mybir
from concourse.bass_utils import run_bass_kernel_spmd

F32 = mybir.dt.float32
BF16 = mybir.dt.bfloat16
ALU = mybir.AluOpType
AF = mybir.ActivationFunctionType
AX = mybir.AxisListType

D = 2048
NCORE = 8
TOK = 2048
NT = 16
E = 32
EPS = 1e-6
PV_C, PV_G1, PV_BIN, PV_GQ, PV_GK = 0, 16, 32, 58, 59
PV_WDW, PV_BDW, PV_LNG, PV_LNB, PV_GOC = 60, 308, 316, 324, 332
PV_BG, PV_BU, PV_WR = 340, 852, 1364
PV_G2 = 1364 + 512
NPV = 1364 + 512 + 16
RV_GOA, RV_BOUT, RV_G2, RV_BR, RV_SINK = 0, 1024, 3072, 5120, 5152
NRV = 5168


class Trk:
    def __init__(s, nc):
        s.nc = nc
        s.eng = dict(pe=nc.tensor, act=nc.scalar, dve=nc.vector, pool=nc.gpsimd, sp=nc.sync)
        s.csem = {e: nc.alloc_semaphore("c_" + e) for e in s.eng}
        s.ND = 24
        s.dsem = [nc.alloc_semaphore(f"dq{i}") for i in range(s.ND)]
        s.reset_state()

    def reset_state(s):
        s.cnt = {e: 0 for e in s.eng}
        s.seen = {c: {p: 0 for p in s.eng} for c in s.eng}
        s.dval = [0] * s.ND
        s.dseen = {c: [0] * s.ND for c in s.eng}
        s.drr = 0
        s.lastw = {}
        s.readers = {}

    def _wait(s, c, tok):
        if tok[0] == 'e':
            _, p, seq = tok
            if p == c and p == 'pe':
                return
            if s.seen[c][p] >= seq:
                return
            s.eng[c].wait_ge(s.csem[p], seq)
            s.seen[c][p] = seq
        else:
            _, k, val = tok
            if s.dseen[c][k] >= val:
                return
            s.eng[c].wait_ge(s.dsem[k], val)
            s.dseen[c][k] = val

    def _deps(s, c, r, w):
        for b in r:
            t = s.lastw.get(b)
            if t:
                s._wait(c, t)
        for b in w:
            t = s.lastw.get(b)
            if t:
                s._wait(c, t)
            rd = s.readers.get(b)
            if rd:
                for p, seq in rd[0].items():
                    if p != c:
                        s._wait(c, ('e', p, seq))
                for t in rd[1]:
                    s._wait(c, t)

    def _record(s, tok, r, w):
        for b in r:
            rd = s.readers.setdefault(b, [{}, []])
            if tok[0] == 'e':
                rd[0][tok[1]] = tok[2]
            else:
                rd[1].append(tok)
        for b in w:
            s.lastw[b] = tok
            s.readers[b] = [{}, []]

    def op(s, c, fn, r=(), w=()):
        s._deps(c, r, w)
        ins = fn(s.eng[c])
        s.cnt[c] += 1
        ins.then_inc(s.csem[c], 1)
        s._record(('e', c, s.cnt[c]), r, w)

    def dma(s, c, fn, r=(), w=()):
        s._deps(c, r, w)
        k = s.drr
        s.drr = (s.drr + 1) % s.ND
        if s.dval[k] > 0:
            s._wait(c, ('d', k, s.dval[k]))
        ins = fn(s.eng[c])
        s.dval[k] += 16
        ins.then_inc(s.dsem[k], 16)
        s._record(('d', k, s.dval[k]), r, w)

    def barrier(s):
        for c in s.eng:
            for p in s.eng:
                if s.cnt[p] > 0:
                    s._wait(c, ('e', p, s.cnt[p]))
            for k in range(s.ND):
                if s.dval[k] > 0:
                    s._wait(c, ('d', k, s.dval[k]))
        s.lastw = {}
        s.readers = {}


def build_nc(dbg=None):
    dbg = dbg or {}
    STOP = dbg.get('stop')
    HALVES = dbg.get('halves', [0, 1])
    GROUPS = dbg.get('groups', [0, 1])
    NEXP = dbg.get('n_exp', E)
    TAPS = dbg.get('taps', False)
    nc = bass.Bass("TRN2", target_bir_lowering=False)

    def din(name, shape, dt=F32):
        return nc.dram_tensor(name, list(shape), dt, kind="ExternalInput").ap()

    xh_d = din("xh", [17 * 128, D])
    hv_d = din("hv", [128, 1])
    pv_d = din("pv", [128, NPV])
    rv_d = din("rv", [128, NRV])
    bada_d = din("bada", [1, 6 * D])
    wada_d = din("wada", [D, 6 * D])
    win_d = din("win", [D, 3328])
    wout_d = din("wout", [D, D])
    bias_d = din("biast", [128, 4096])
    wg_d = din("wg", [NEXP, D, D])
    wu_d = din("wu", [NEXP, D, D])
    wd_d = din("wd", [NEXP, D, D])
    bd_d = din("bd", [NEXP, D])
    identf_d = din("identf", [128, 128])
    blk_d = din("blk", [128, 128])
    out_d = nc.dram_tensor("out", [TOK, D], F32, kind="ExternalOutput").ap()
    x1_d = nc.dram_tensor("x1s", [TOK, D], F32, kind=("ExternalOutput" if TAPS else "Internal")).ap()
    g1s_d = nc.dram_tensor("g1s", [2, 128, D], F32, kind="Internal").ap()

    t = Trk(nc)

    def tap(name, tens, shape, dt, key):
        if not TAPS:
            return
        dd = nc.dram_tensor("tap_" + name, list(shape), dt, kind="ExternalOutput").ap()
        t.dma('sp', lambda e: e.dma_start(out=dd, in_=tens[:]), r=[key], w=['tap_' + name])
    _uid = [0]

    def SB(name, shape, dt):
        _uid[0] += 1
        return nc.alloc_sbuf_tensor(f"{name}_s{_uid[0]}", shape, dt)

    def SBT(name, shape, dt):
        _uid[0] += 1
        return nc.sbuf_tensor(f"{name}_s{_uid[0]}", shape, dt)

    pv = SB("pv", [128, NPV], F32)
    hv = SB("hv", [128, 1], F32)
    identf = SB("identf", [128, 128], F32)
    identb = SB("identb", [128, 128], BF16)
    blkb = SB("blkb", [128, 128], BF16)
    onesf = SB("onesf", [128, 128], F32)
    sT = SB("sT", [128, 16], BF16)
    modT = SB("modT", [128, 4, 16], F32)
    A2 = SB("A2", [128, 16], F32)
    A1 = SB("A1", [128, 16], F32)
    G2 = SB("G2", [128, D], F32)
    brt = SB("brt", [128, 32], F32)
    esink = SB("esink", [128, 16], F32)
    ebt = SB("ebt", [128, 4096], BF16)
    ebt0 = SB("ebt0", [128, 2048], BF16)
    wrb = SB("wrb", [128, 512], BF16)
    ssq = SB("ssq", [128, 1], F32)
    rstd = SB("rstd", [128, 1], F32)

    ps = [nc.alloc_psum_tensor(f"ps{i}", [128, 512], F32) for i in range(6)]
    psT = [nc.alloc_psum_tensor(f"psT{i}", [128, 1024], BF16) for i in range(2)]

    def P(i):
        return f"ps{i}"

    t.dma('sp', lambda e: e.dma_start(out=pv[:], in_=pv_d), w=['pv'])
    t.dma('sp', lambda e: e.dma_start(out=hv[:], in_=hv_d), w=['hv'])
    t.dma('sp', lambda e: e.dma_start(out=identf[:], in_=identf_d), w=['identf'])
    t.dma('pool', lambda e: e.dma_start(out=identb[:], in_=identf_d), w=['identb'])
    t.dma('pool', lambda e: e.dma_start(out=blkb[:], in_=blk_d), w=['blkb'])
    t.op('dve', lambda e: e.memset(onesf[:], 1.0), w=['onesf'])
    t.op('dve', lambda e: e.tensor_copy(out=wrb[:], in_=pv[:, PV_WR:PV_WR + 512]), r=['pv'], w=['wrb'])
    t.op('act', lambda e: e.activation(out=sT[:], in_=pv[:, PV_C:PV_C + 16], func=AF.Silu), r=['pv'], w=['sT'])

    with ExitStack() as _es:
        rvt = _es.enter_context(SBT("rvt", [128, NRV], F32))
        biasf = _es.enter_context(SBT("biasf", [128, 4096], F32))
        wr0 = _es.enter_context(SBT("wr0", [128, 16, 512], BF16))
        wr1 = _es.enter_context(SBT("wr1", [128, 16, 512], BF16))
        brow0 = _es.enter_context(SBT("brow0", [1, 512], F32))
        brow1 = _es.enter_context(SBT("brow1", [1, 512], F32))
        mrow = _es.enter_context(SBT("mrow", [1, 512], F32))
        bcg = _es.enter_context(SBT("bcg", [128, D], F32))
        gb1 = _es.enter_context(SBT("gb1", [128, D], F32))
        wr = [wr0, wr1]
        brow = [brow0, brow1]
        t.dma('sp', lambda e: e.dma_start(out=rvt[:], in_=rv_d), w=['rvt'])
        t.dma('sp', lambda e: e.dma_start(out=biasf[:], in_=bias_d), w=['biasf'])
        t.op('act', lambda e: e.activation(out=ebt[:], in_=biasf[:], func=AF.Exp), r=['biasf'], w=['ebt'])
        t.op('dve', lambda e: e.tensor_scalar(out=ebt0[:], in0=ebt[:, 0:2048], scalar1=hv[:, 0:1], scalar2=None,
                                              op0=ALU.mult), r=['ebt', 'hv'], w=['ebt0'])
        t.op('act', lambda e: e.activation(out=esink[:], in_=rvt[:, RV_SINK:RV_SINK + 16], func=AF.Exp),
             r=['rvt'], w=['esink'])
        t.op('dve', lambda e: e.tensor_copy(out=brt[:], in_=rvt[:, RV_BR:RV_BR + 32]), r=['rvt'], w=['brt'])

        wada_v = wada_d.rearrange("(kc p) f -> p kc f", p=128)
        for n in range(24):
            b = n % 2
            t.dma('pool', lambda e: e.dma_start(out=wr[b][:], in_=wada_v[:, :, n * 512:(n + 1) * 512]), w=[f'wr{b}'])
            t.dma('sp', lambda e: e.dma_start(out=brow[b][:], in_=bada_d[0:1, n * 512:(n + 1) * 512]), w=[f'brow{b}'])
            for kc in range(16):
                t.op('pe', lambda e: e.matmul(ps[0][0:1, :], lhsT=sT[:, kc:kc + 1], rhs=wr[b][:, kc, :],
                                              start=(kc == 0), stop=(kc == 15)), r=['sT', f'wr{b}'], w=[P(0)])
            t.op('dve', lambda e: e.tensor_tensor(out=mrow[:], in0=ps[0][0:1, :], in1=brow[b][:], op=ALU.add),
                 r=[P(0), f'brow{b}'], w=['mrow'])
            which, q = n // 4, n % 4
            if which in (0, 1, 3, 4):
                mi = {0: 0, 1: 1, 3: 2, 4: 3}[which]
                for j in range(4):
                    t.op('pe', lambda e: e.matmul(ps[1][:, j:j + 1], lhsT=mrow[0:1, j * 128:(j + 1) * 128],
                                                  rhs=onesf[0:1, 0:1], start=True, stop=True), r=['mrow', 'onesf'], w=[P(1)])
                t.op('dve', lambda e: e.tensor_copy(out=modT[:, mi, q * 4:(q + 1) * 4], in_=ps[1][:, 0:4]),
                     r=[P(1)], w=['modT'])
            else:
                dstt, dk = (bcg, 'bcg') if which == 2 else (G2, 'G2')
                t.op('pe', lambda e: e.matmul(ps[2][:, :], lhsT=onesf[0:1, 0:128], rhs=mrow[0:1, :],
                                              start=True, stop=True), r=['mrow', 'onesf'], w=[P(2)])
                t.op('act', lambda e: e.activation(out=dstt[:, q * 512:(q + 1) * 512], in_=ps[2][:, :], func=AF.Identity),
                     r=[P(2)], w=[dk])
        t.op('dve', lambda e: e.scalar_tensor_tensor(out=A1[:], in0=modT[:, 1, :], scalar=1.0, in1=pv[:, PV_G1:PV_G1 + 16],
                                                     op0=ALU.add, op1=ALU.mult), r=['modT', 'pv'], w=['A1'])
        t.op('dve', lambda e: e.scalar_tensor_tensor(out=A2[:], in0=modT[:, 3, :], scalar=1.0, in1=pv[:, PV_G2:PV_G2 + 16],
                                                     op0=ALU.add, op1=ALU.mult), r=['modT', 'pv'], w=['A2'])
        t.op('dve', lambda e: e.tensor_tensor(out=gb1[:], in0=bcg[:], in1=rvt[:, RV_BOUT:RV_BOUT + D], op=ALU.mult),
             r=['bcg', 'rvt'], w=['gb1'])
        t.dma('sp', lambda e: e.dma_start(out=g1s_d[0], in_=bcg[:]), r=['bcg'], w=['g1sd'])
        t.dma('sp', lambda e: e.dma_start(out=g1s_d[1], in_=gb1[:]), r=['gb1'], w=['g1sd'])
        tap('modT', modT, [128, 4, 16], F32, 'modT')
        tap('A1', A1, [128, 16], F32, 'A1')
        tap('bc0', bcg, [128, D], F32, 'bcg')
        tap('ebt', ebt, [128, 4096], BF16, 'ebt')
        t.barrier()
        if STOP == 'ada':
            return nc

    win_v = win_d.rearrange("(kc p) f -> p kc f", p=128)
    wout_v = wout_d.rearrange("(kc p) f -> p kc f", p=128)

    def rsqrt_into(dst, src_ap, scale, r, w):
        t.op('act', lambda e: e.activation(out=dst, in_=src_ap, func=AF.Sqrt, bias=EPS, scale=scale), r=r, w=w)
        t.op('dve', lambda e: e.reciprocal(out=dst, in_=dst), r=w, w=w)

    for hf in HALVES:
        with ExitStack() as _esh:
            h2T = _esh.enter_context(SBT("h2T", [128, 16, 1024], BF16))
            wt = _esh.enter_context(SBT("wt", [128, 8, 32], F32))
            with ExitStack() as _esm:
                mixedT = _esm.enter_context(SBT("mixedT", [128, 16, 1024], BF16))
                with ExitStack() as _es:
                    qT = _es.enter_context(SBT("qT", [128, 8, 1152], BF16))
                    kT = _es.enter_context(SBT("kT", [128, 1152], BF16))
                    v1 = _es.enter_context(SBT("v1", [128, 9, 2, 65], BF16))
                    hglu = _es.enter_context(SBT("hglu", [128, 8, 1152], BF16))
                    t.op('pool', lambda e: e.memset(v1[:], 1.0), w=['v1'])
                    with ExitStack() as _es:
                        hT = _es.enter_context(SBT("hT", [128, 16, 1152], BF16))
                        xb0 = _es.enter_context(SBT("xb0", [128, D], F32))
                        xs = _es.enter_context(SBT("xs", [128, D], BF16))
                        wi0 = _es.enter_context(SBT("wi0", [128, 16, 256], BF16))
                        wi1 = _es.enter_context(SBT("wi1", [128, 16, 256], BF16))
                        tA = xb0[:, 0:512]
                        tB = xs[:, 0:512]
                        tC = xb0[:, 512:1024]
                        xb = [xb0, xb0]; junk = xs
                        wi = [wi0, wi1]
                        for tt in range(9):
                            xt = xb[tt % 2]
                            xk = 'xb0'
                            r0 = (hf * 8 + tt) * 128
                            t.dma('sp', lambda e: e.dma_start(out=xt[:], in_=xh_d[r0:r0 + 128, :]), w=[xk])
                            t.op('dve', lambda e: e.memset(ssq[:], 0.0), w=['ssq'])
                            t.op('act', lambda e: e.activation(out=junk[:], in_=xt[:], func=AF.Square, accum_out=ssq[:, 0:1]),
                                 r=[xk, 'ssq'], w=['xs', 'ssq'])
                            rsqrt_into(rstd[:], ssq[:], 1.0 / D, ['ssq'], ['rstd'])
                            t.op('dve', lambda e: e.tensor_scalar(out=xs[:], in0=xt[:], scalar1=rstd[:, 0:1], scalar2=None, op0=ALU.mult),
                                 r=[xk, 'rstd'], w=['xs'])
                            for c in range(16):
                                t.op('pe', lambda e: e.transpose(out=psT[c // 8][:, (c % 8) * 128:(c % 8 + 1) * 128],
                                                                 in_=xs[:, c * 128:(c + 1) * 128], identity=identb[:]),
                                     r=['xs', 'identb'], w=[f'psT{c // 8}'])
                            for c in range(16):
                                t.op('dve', lambda e: e.tensor_scalar(out=hT[:, c, tt * 128:(tt + 1) * 128],
                                                                      in0=psT[c // 8][:, (c % 8) * 128:(c % 8 + 1) * 128],
                                                                      scalar1=A1[:, c:c + 1], scalar2=modT[:, 0, c:c + 1],
                                                                      op0=ALU.mult, op1=ALU.add),
                                     r=[f'psT{c // 8}', 'A1', 'modT'], w=['hT'])
                        if STOP == 'norm':
                            tap(f'hT{hf}', hT, [128, 16, 1152], BF16, 'hT')
                            t.barrier()
                            return nc
                        t.barrier()
                        chunks = [(0, 128), (128, 512), (640, 512)]
                        for j in dbg.get('jlist', range(26)):
                            wc = j // 2
                            b = wc % 2
                            if j % 2 == 0 or 'jlist' in dbg:
                                t.dma('pool', lambda e: e.dma_start(out=wi[b][:], in_=win_v[:, :, wc * 256:wc * 256 + 256]),
                                      w=[f'wi{b}'])
                            jo = (j % 2) * 128
                            bias = pv[:, PV_BIN + j:PV_BIN + j + 1]
                            for ci, (c0, cn) in enumerate(chunks):
                                pb = ps[ci % 2]
                                for kc in range(0 if dbg.get('nomm') else 16):
                                    t.op('pe', lambda e: e.matmul(pb[:, 0:cn], lhsT=wi[b][:, kc, jo:jo + 128], rhs=hT[:, kc, c0:c0 + cn],
                                                                  start=(kc == 0), stop=(kc == 15)), r=[f'wi{b}', 'hT'], w=[P(ci % 2)])
                                if dbg.get('noevac'):
                                    continue
                                if j < 9:
                                    gcol = PV_GQ if j < 8 else PV_GK
                                    dst = qT[:, j, c0:c0 + cn] if j < 8 else kT[:, c0:c0 + cn]
                                    dk = 'qT' if j < 8 else 'kT'
                                    t.op('act', lambda e: e.activation(out=tB[:, 0:cn], in_=pb[:, 0:cn], func=AF.Square, bias=bias),
                                         r=[P(ci % 2), 'pv'], w=['tB'])
                                    t.op('act', lambda e: e.activation(out=tA[:, 0:cn], in_=pb[:, 0:cn], func=AF.Identity, bias=bias),
                                         r=[P(ci % 2), 'pv'], w=['tA'])
                                    t.op('pe', lambda e: e.matmul(ps[2][:, 0:cn], lhsT=blkb[:], rhs=tB[:, 0:cn], start=True, stop=True),
                                         r=['blkb', 'tB'], w=[P(2)])
                                    rsqrt_into(tC[:, 0:cn], ps[2][:, 0:cn], 1.0 / 64, [P(2)], ['tC'])
                                    t.op('dve', lambda e: e.scalar_tensor_tensor(out=dst, in0=tA[:, 0:cn], scalar=pv[:, gcol:gcol + 1],
                                                                                 in1=tC[:, 0:cn], op0=ALU.mult, op1=ALU.mult),
                                         r=['tA', 'tC', 'pv'], w=[dk])
                                elif j == 9:
                                    t.op('act', lambda e: e.activation(out=tB[:, 0:cn], in_=pb[:, 0:cn], func=AF.Identity, bias=bias),
                                         r=[P(ci % 2), 'pv'], w=['tB'])
                                    for s_ in range(cn // 128):
                                        tt = c0 // 128 + s_
                                        t.op('pe', lambda e: e.transpose(out=psT[0][:, 0:128], in_=tB[:, s_ * 128:(s_ + 1) * 128],
                                                                         identity=identb[:]), r=['tB', 'identb'], w=['psT0'])
                                        t.op('dve', lambda e: e.tensor_copy(out=v1[:, tt, :, 0:64],
                                                                            in_=psT[0][:, 0:128].rearrange("p (g d) -> p g d", g=2)),
                                             r=['psT0'], w=['v1'])
                                elif j < 18:
                                    c = j - 10
                                    t.op('dve', lambda e: e.tensor_scalar(out=hglu[:, c, c0:c0 + cn], in0=pb[:, 0:cn], scalar1=bias,
                                                                          scalar2=None, op0=ALU.add), r=[P(ci % 2), 'pv'], w=['hglu'])
                                else:
                                    c = j - 18
                                    t.op('act', lambda e: e.activation(out=tB[:, 0:cn], in_=pb[:, 0:cn], func=AF.Sigmoid, bias=bias),
                                         r=[P(ci % 2), 'pv'], w=['tB'])
                                    t.op('pool', lambda e: e.tensor_tensor(out=hglu[:, c, c0:c0 + cn], in0=hglu[:, c, c0:c0 + cn],
                                                                           in1=tB[:, 0:cn], op=ALU.mult), r=['tB', 'hglu'], w=['hglu'])
                        if hf == 0 and not dbg.get('nohv'):
                            for c in range(8):
                                t.op('pool', lambda e: e.tensor_scalar(out=hglu[:, c, 0:128], in0=hglu[:, c, 0:128], scalar1=hv[:, 0:1],
                                                                       scalar2=None, op0=ALU.mult), r=['hglu', 'hv'], w=['hglu'])
                        tap(f'hT{hf}', hT, [128, 16, 1152], BF16, 'hT')
                        tap(f'qT{hf}', qT, [128, 8, 1152], BF16, 'qT')
                        tap(f'kT{hf}', kT, [128, 1152], BF16, 'kT')
                        tap(f'v1{hf}', v1, [128, 9, 2, 65], BF16, 'v1')
                        tap(f'hglu{hf}', hglu, [128, 8, 1152], BF16, 'hglu')
                        t.barrier()
                        if STOP == 'inproj':
                            return nc

                    with ExitStack() as _es:
                        acc = _es.enter_context(SBT("acc", [128, 8, 1024], F32))
                        sq = _es.enter_context(SBT("sq", [128, 512], F32))
                        mean = _es.enter_context(SBT("mean", [128, 512], F32))
                        var = _es.enter_context(SBT("var", [128, 512], F32))
                        tmp = _es.enter_context(SBT("tmp", [128, 512], F32))
                        yatt = _es.enter_context(SBT("yatt", [128, 1024], F32))
                        ynb = _es.enter_context(SBT("ynb", [128, 1024], BF16))
                        pe0 = _es.enter_context(SBT("pe0", [128, 512], F32))
                        pe1 = _es.enter_context(SBT("pe1", [128, 512], F32))
                        pT0 = _es.enter_context(SBT("pT0", [128, 1024], BF16))
                        pT1 = _es.enter_context(SBT("pT1", [128, 1024], BF16))
                        den = _es.enter_context(SBT("den", [128, 8], F32))
                        junk2 = _es.enter_context(SBT("junk2", [128, 1024], BF16))
                        goa = _es.enter_context(SBT("goa", [128, 1024], F32))
                        t.dma('sp', lambda e: e.dma_start(out=goa[:], in_=rv_d[:, RV_GOA:RV_GOA + 1024]), w=['goa'])
                        for c in range(8):
                            en = 'dve'
                            ak = f'acc{c}'
                            wc0 = PV_WDW + c * 31
                            t.op(en, lambda e: e.tensor_scalar(out=acc[:, c, :], in0=hglu[:, c, 98:98 + 1024], scalar1=pv[:, wc0:wc0 + 1],
                                                               scalar2=pv[:, PV_BDW + c:PV_BDW + c + 1], op0=ALU.mult, op1=ALU.add),
                                 r=['hglu', 'pv'], w=[ak])
                            for j in range(1, 31):
                                if en == 'dve':
                                    t.op(en, lambda e: e.scalar_tensor_tensor(out=acc[:, c, :], in0=hglu[:, c, 98 + j:98 + j + 1024],
                                                                              scalar=pv[:, wc0 + j:wc0 + j + 1], in1=acc[:, c, :],
                                                                              op0=ALU.mult, op1=ALU.add), r=['hglu', 'pv', ak], w=[ak])
                                else:
                                    t.op(en, lambda e: e.tensor_scalar(out=ctmp[:], in0=hglu[:, c, 98 + j:98 + j + 1024],
                                                                       scalar1=pv[:, wc0 + j:wc0 + j + 1], scalar2=None, op0=ALU.mult),
                                         r=['hglu', 'pv'], w=['ctmp'])
                                    t.op(en, lambda e: e.tensor_tensor(out=acc[:, c, :], in0=acc[:, c, :], in1=ctmp[:], op=ALU.add),
                                         r=['ctmp', ak], w=[ak])
                        for ch in range(2):
                            cs = slice(ch * 512, (ch + 1) * 512)
                            for c in range(8):
                                t.op('act', lambda e: e.activation(out=sq[:], in_=acc[:, c, cs], func=AF.Square), r=[f'acc{c}'], w=['sq'])
                                t.op('pe', lambda e: e.matmul(ps[0][:], lhsT=onesf[:], rhs=acc[:, c, cs], start=(c == 0), stop=(c == 7)),
                                     r=['onesf', f'acc{c}'], w=[P(0)])
                                t.op('pe', lambda e: e.matmul(ps[1][:], lhsT=onesf[:], rhs=sq[:], start=(c == 0), stop=(c == 7)),
                                     r=['onesf', 'sq'], w=[P(1)])
                            t.op('dve', lambda e: e.tensor_scalar(out=mean[:], in0=ps[0][:], scalar1=1.0 / 1024, scalar2=None, op0=ALU.mult),
                                 r=[P(0)], w=['mean'])
                            t.op('dve', lambda e: e.tensor_tensor(out=tmp[:], in0=mean[:], in1=mean[:], op=ALU.mult), r=['mean'], w=['tmp'])
                            t.op('dve', lambda e: e.scalar_tensor_tensor(out=var[:], in0=ps[1][:], scalar=1.0 / 1024, in1=tmp[:],
                                                                         op0=ALU.mult, op1=ALU.subtract), r=[P(1), 'tmp'], w=['var'])
                            rsqrt_into(var[:], var[:], 1.0, ['var'], ['var'])
                            for c in range(8):
                                ak = f'acc{c}'
                                t.op('dve', lambda e: e.tensor_tensor(out=tmp[:], in0=acc[:, c, cs], in1=mean[:], op=ALU.subtract),
                                     r=[ak, 'mean'], w=['tmp'])
                                t.op('dve', lambda e: e.tensor_tensor(out=tmp[:], in0=tmp[:], in1=var[:], op=ALU.mult), r=['tmp', 'var'], w=['tmp'])
                                t.op('act', lambda e: e.activation(out=acc[:, c, cs], in_=tmp[:], func=AF.Silu,
                                                                   bias=pv[:, PV_LNB + c:PV_LNB + c + 1], scale=pv[:, PV_LNG + c:PV_LNG + c + 1]),
                                     r=['tmp', 'pv'], w=[ak])
                                t.op('act', lambda e: e.activation(out=sq[:], in_=acc[:, c, cs], func=AF.Square), r=[ak], w=['sq'])
                                t.op('pe', lambda e: e.matmul(ps[2][:], lhsT=onesf[:], rhs=sq[:], start=(c == 0), stop=(c == 7)),
                                     r=['onesf', 'sq'], w=[P(2)])
                            rsqrt_into(var[:], ps[2][:], 1.0 / 1024, [P(2)], ['var'])
                            for c in range(8):
                                t.op('dve', lambda e: e.scalar_tensor_tensor(out=mixedT[:, 8 + c, cs], in0=acc[:, c, cs],
                                                                             scalar=pv[:, PV_GOC + c:PV_GOC + c + 1], in1=var[:],
                                                                             op0=ALU.mult, op1=ALU.mult), r=[f'acc{c}', 'var', 'pv'], w=['mixedT'])

                        pes = [pe0, pe1]
                        pTs = [pT0, pT1]
                        for n in range(8):
                            tt = n + 1
                            for g in range(2):
                                gp = slice(g * 64, (g + 1) * 64)
                                for kt in range(2):
                                    kc0 = (tt - 1 + kt) * 128
                                    for hh in range(2):
                                        pi = 2 + hh
                                        t.op('pe', lambda e: e.matmul(ps[pi][:], lhsT=kT[gp, kc0:kc0 + 128],
                                                                      rhs=qT[gp, 4 * hh:4 * hh + 4, tt * 128:(tt + 1) * 128],
                                                                      start=True, stop=True), r=['kT', 'qT'], w=[P(pi)])
                                        t.op('act', lambda e: e.activation(out=pes[hh][:], in_=ps[pi][:], func=AF.Exp, scale=0.125),
                                             r=[P(pi)], w=[f'pe{hh}'])
                                        if kt == 0 and n == 0 and hf == 0:
                                            eb = ebt0[:, g * 1024 + hh * 512:g * 1024 + (hh + 1) * 512]
                                        else:
                                            o = kt * 2048 + g * 1024 + hh * 512
                                            eb = ebt[:, o:o + 512]
                                        t.op('dve', lambda e: e.tensor_tensor(out=pTs[kt][:, hh * 512:(hh + 1) * 512], in0=pes[hh][:], in1=eb,
                                                                              op=ALU.mult), r=[f'pe{hh}', 'ebt', 'ebt0'], w=[f'pT{kt}'])
                                for jj in range(8):
                                    pi = 4 + jj // 4
                                    oc = (jj % 4) * 65
                                    for kt in range(2):
                                        t.op('pe', lambda e: e.matmul(ps[pi][:, oc:oc + 65], lhsT=pTs[kt][:, jj * 128:(jj + 1) * 128],
                                                                      rhs=v1[:, tt - 1 + kt, g, :], start=(kt == 0), stop=(kt == 1)),
                                             r=[f'pT{kt}', 'v1'], w=[P(pi)])
                                for half in range(2):
                                    pi = 4 + half
                                    pv3 = ps[pi][:, 0:260].rearrange("p (h d) -> p h d", d=65)
                                    hs = 8 * g + 4 * half
                                    t.op('dve', lambda e: e.tensor_tensor(out=den[:, 0:4], in0=pv3[:, :, 64], in1=esink[:, hs:hs + 4], op=ALU.add),
                                         r=[P(pi), 'esink'], w=['den'])
                                    t.op('dve', lambda e: e.reciprocal(out=den[:, 0:4], in_=den[:, 0:4]), r=['den'], w=['den'])
                                    for j4 in range(4):
                                        h = hs + j4
                                        t.op('dve', lambda e: e.tensor_scalar(out=yatt[:, h * 64:(h + 1) * 64], in0=pv3[:, j4, 0:64],
                                                                              scalar1=den[:, j4:j4 + 1], scalar2=None, op0=ALU.mult),
                                             r=[P(pi), 'den'], w=['yatt'])
                            t.op('dve', lambda e: e.memset(ssq[:], 0.0), w=['ssq'])
                            t.op('act', lambda e: e.activation(out=junk2[:], in_=yatt[:], func=AF.Square, accum_out=ssq[:, 0:1]),
                                 r=['yatt', 'ssq'], w=['junk2', 'ssq'])
                            rsqrt_into(rstd[:], ssq[:], 1.0 / 1024, ['ssq'], ['rstd'])
                            t.op('dve', lambda e: e.scalar_tensor_tensor(out=ynb[:], in0=yatt[:], scalar=rstd[:, 0:1], in1=goa[:],
                                                                         op0=ALU.mult, op1=ALU.mult), r=['yatt', 'rstd', 'goa'], w=['ynb'])
                            for c in range(8):
                                t.op('pe', lambda e: e.transpose(out=psT[1][:, c * 128:(c + 1) * 128], in_=ynb[:, c * 128:(c + 1) * 128],
                                                                 identity=identb[:]), r=['ynb', 'identb'], w=['psT1'])
                            t.op('dve', lambda e: e.tensor_copy(out=mixedT[:, 0:8, n * 128:(n + 1) * 128],
                                                                in_=psT[1][:, :].rearrange("p (c q) -> p c q", c=8)), r=['psT1'], w=['mixedT'])
                        tap(f'mixedT{hf}', mixedT, [128, 16, 1024], BF16, 'mixedT')
                        t.barrier()
                        if STOP == 'mixer':
                            return nc


                with ExitStack() as _es:
                    wo = _es.enter_context(SBT("wo", [128, 16, D], BF16))
                    G1 = _es.enter_context(SBT("G1", [128, D], F32))
                    GB1 = _es.enter_context(SBT("GB1", [128, D], F32))
                    xt2 = _es.enter_context(SBT("xt2", [128, D], F32))
                    x1 = _es.enter_context(SBT("x1", [128, D], F32))
                    h2b = _es.enter_context(SBT("h2b", [128, D], BF16))
                    lg = _es.enter_context(SBT("lg", [128, 32], F32))
                    top8 = _es.enter_context(SBT("top8", [128, 8], F32))
                    msk = _es.enter_context(SBT("msk", [128, 32], F32))
                    sm = _es.enter_context(SBT("sm", [128, 2], F32))
                    t.dma('sp', lambda e: e.dma_start(out=G1[:], in_=g1s_d[0]), r=['g1sd'], w=['G1'])
                    t.dma('sp', lambda e: e.dma_start(out=GB1[:], in_=g1s_d[1]), r=['g1sd'], w=['GB1'])
                    for q in range(4):
                        t.dma('pool', lambda e: e.dma_start(out=wo[:, :, q * 512:(q + 1) * 512], in_=wout_v[:, :, q * 512:(q + 1) * 512]),
                              w=['wo'])
                    for lt in range(8):
                        gt = hf * 8 + lt
                        t.dma('sp', lambda e: e.dma_start(out=xt2[:], in_=xh_d[(gt + 1) * 128:(gt + 2) * 128, :]), w=['xt2'])
                        t.op('pool', lambda e: e.tensor_tensor(out=xt2[:], in0=xt2[:], in1=GB1[:], op=ALU.add), r=['xt2', 'GB1'], w=['xt2'])
                        for nq in range(4):
                            for cc in range(16):
                                t.op('pe', lambda e: e.matmul(ps[nq][:], lhsT=mixedT[:, cc, lt * 128:(lt + 1) * 128],
                                                              rhs=wo[:, cc, nq * 512:(nq + 1) * 512], start=(cc == 0), stop=(cc == 15)),
                                     r=['mixedT', 'wo'], w=[P(nq)])
                            ns = slice(nq * 512, (nq + 1) * 512)
                            t.op('dve', lambda e: e.tensor_tensor(out=x1[:, ns], in0=ps[nq][:], in1=G1[:, ns], op=ALU.mult),
                                 r=[P(nq), 'G1'], w=['x1'])
                        t.op('dve', lambda e: e.tensor_tensor(out=x1[:], in0=x1[:], in1=xt2[:], op=ALU.add), r=['x1', 'xt2'], w=['x1'])
                        t.dma('sp', lambda e: e.dma_start(out=x1_d[gt * 128:(gt + 1) * 128, :], in_=x1[:]), r=['x1'], w=['x1d'])
                        t.op('dve', lambda e: e.memset(ssq[:], 0.0), w=['ssq'])
                        t.op('act', lambda e: e.activation(out=h2b[:], in_=x1[:], func=AF.Square, accum_out=ssq[:, 0:1]),
                             r=['x1', 'ssq'], w=['h2b', 'ssq'])
                        rsqrt_into(rstd[:], ssq[:], 1.0 / D, ['ssq'], ['rstd'])
                        t.op('dve', lambda e: e.tensor_scalar(out=h2b[:], in0=x1[:], scalar1=rstd[:, 0:1], scalar2=None, op0=ALU.mult),
                             r=['x1', 'rstd'], w=['h2b'])
                        for cc in range(16):
                            t.op('pe', lambda e: e.transpose(out=psT[cc // 8][:, (cc % 8) * 128:(cc % 8 + 1) * 128],
                                                             in_=h2b[:, cc * 128:(cc + 1) * 128], identity=identb[:]),
                                 r=['h2b', 'identb'], w=[f'psT{cc // 8}'])
                        for cc in range(16):
                            pin = psT[cc // 8][:, (cc % 8) * 128:(cc % 8 + 1) * 128]
                            if cc % 2 == 0:
                                t.op('dve', lambda e: e.tensor_scalar(out=h2T[:, cc, lt * 128:(lt + 1) * 128], in0=pin,
                                                                      scalar1=A2[:, cc:cc + 1], scalar2=modT[:, 2, cc:cc + 1],
                                                                      op0=ALU.mult, op1=ALU.add),
                                     r=[f'psT{cc // 8}', 'A2', 'modT'], w=['h2T'])
                            else:
                                t.op('act', lambda e: e.activation(out=h2T[:, cc, lt * 128:(lt + 1) * 128], in_=pin, func=AF.Identity,
                                                                   bias=modT[:, 2, cc:cc + 1], scale=A2[:, cc:cc + 1]),
                                     r=[f'psT{cc // 8}', 'A2', 'modT'], w=['h2T'])
                        for cc in range(16):
                            t.op('pe', lambda e: e.matmul(ps[5][:, 0:32], lhsT=h2T[:, cc, lt * 128:(lt + 1) * 128],
                                                          rhs=wrb[:, cc * 32:(cc + 1) * 32],
                                                          start=(cc == 0), stop=(cc == 15)), r=['h2T', 'wrb'], w=[P(5)])
                        t.op('dve', lambda e: e.tensor_tensor(out=lg[:], in0=ps[5][:, 0:32], in1=brt[:], op=ALU.add), r=[P(5), 'brt'], w=['lg'])
                        t.op('dve', lambda e: e.max(out=top8[:], in_=lg[:]), r=['lg'], w=['top8'])
                        t.op('dve', lambda e: e.tensor_scalar(out=msk[:], in0=lg[:], scalar1=top8[:, 3:4], scalar2=None, op0=ALU.is_ge),
                             r=['lg', 'top8'], w=['msk'])
                        t.op('dve', lambda e: e.tensor_scalar(out=sm[:, 0:1], in0=top8[:, 0:1], scalar1=-1.0, scalar2=None, op0=ALU.mult),
                             r=['top8'], w=['sm'])
                        t.op('act', lambda e: e.activation(out=lg[:], in_=lg[:], func=AF.Exp, bias=sm[:, 0:1]), r=['lg', 'sm'], w=['lg'])
                        t.op('dve', lambda e: e.tensor_tensor(out=lg[:], in0=lg[:], in1=msk[:], op=ALU.mult), r=['lg', 'msk'], w=['lg'])
                        t.op('dve', lambda e: e.reduce_sum(out=sm[:, 1:2], in_=lg[:], axis=AX.X), r=['lg'], w=['sm'])
                        t.op('dve', lambda e: e.reciprocal(out=sm[:, 1:2], in_=sm[:, 1:2]), r=['sm'], w=['sm'])
                        t.op('dve', lambda e: e.tensor_scalar(out=wt[:, lt, :], in0=lg[:], scalar1=sm[:, 1:2], scalar2=None, op0=ALU.mult),
                             r=['lg', 'sm'], w=['wt'])
                    t.barrier()
                    if STOP == 'outproj':
                        return nc

            with ExitStack() as _es:
                yacc = _es.enter_context(SBT("yacc", [128, 8, D], F32))
                actT = _es.enter_context(SBT("actT", [128, 16, 1024], BF16))
                wmR = _es.enter_context(SBT("wmR", [128, 4, 16, 256], BF16))
                bdb0 = _es.enter_context(SBT("bdb0", [128, 512], F32))
                bdb1 = _es.enter_context(SBT("bdb1", [128, 512], F32))
                g1a = _es.enter_context(SBT("g1a", [128, 512], F32))
                g1b = _es.enter_context(SBT("g1b", [128, 512], F32))
                sga = _es.enter_context(SBT("sga", [128, 512], BF16))
                sgb = _es.enter_context(SBT("sgb", [128, 512], BF16))
                u1a = _es.enter_context(SBT("u1a", [128, 512], F32))
                u1b = _es.enter_context(SBT("u1b", [128, 512], F32))
                g1s, sgs, u1s = [g1a, g1b], [sga, sgb], [u1a, u1b]
                bdb = [bdb0, bdb1]
                wmi = 0
                t.op('dve', lambda e: e.memset(yacc[:], 0.0), w=['yacc'])
                for ex in range(NEXP):
                    wgv = wg_d[ex].rearrange("(kc p) f -> p kc f", p=128)
                    wuv = wu_d[ex].rearrange("(kc p) f -> p kc f", p=128)
                    wdv = wd_d[ex].rearrange("(kc p) f -> p kc f", p=128)
                    for qf in range(8):
                        bg_, bu_ = wmi % 4, (wmi + 1) % 4
                        wmi += 2
                        t.dma('pool', lambda e: e.dma_start(out=wmR[:, bg_], in_=wgv[:, :, qf * 256:(qf + 1) * 256]), w=[f'wm{bg_}'])
                        t.dma('pool', lambda e: e.dma_start(out=wmR[:, bu_], in_=wuv[:, :, qf * 256:(qf + 1) * 256]), w=[f'wm{bu_}'])
                        for ft in range(2):
                            f = qf * 2 + ft
                            bgc = pv[:, PV_BG + ex * 16 + f:PV_BG + ex * 16 + f + 1]
                            buc = pv[:, PV_BU + ex * 16 + f:PV_BU + ex * 16 + f + 1]
                            for ch in range(2):
                                cs = slice(ch * 512, (ch + 1) * 512)
                                pg, pu = ps[2 * ch], ps[2 * ch + 1]
                                kg, ku = P(2 * ch), P(2 * ch + 1)
                                g1, sg, u1 = g1s[ch], sgs[ch], u1s[ch]
                                k1, k2, k3 = f'g1{ch}', f'sg{ch}', f'u1{ch}'
                                for kc in range(16):
                                    t.op('pe', lambda e: e.matmul(pg[:], lhsT=wmR[:, bg_, kc, ft * 128:(ft + 1) * 128], rhs=h2T[:, kc, cs],
                                                                  start=(kc == 0), stop=(kc == 15)), r=[f'wm{bg_}', 'h2T'], w=[kg])
                                for kc in range(16):
                                    t.op('pe', lambda e: e.matmul(pu[:], lhsT=wmR[:, bu_, kc, ft * 128:(ft + 1) * 128], rhs=h2T[:, kc, cs],
                                                                  start=(kc == 0), stop=(kc == 15)), r=[f'wm{bu_}', 'h2T'], w=[ku])
                                t.op('dve', lambda e: e.tensor_scalar(out=g1[:], in0=pg[:], scalar1=bgc, scalar2=7.0, op0=ALU.add, op1=ALU.min),
                                     r=[kg, 'pv'], w=[k1])
                                t.op('act', lambda e: e.activation(out=sg[:], in_=g1[:], func=AF.Sigmoid, scale=1.702), r=[k1], w=[k2])
                                t.op('dve', lambda e: e.tensor_scalar(out=u1[:], in0=pu[:], scalar1=buc, scalar2=7.0, op0=ALU.add, op1=ALU.min),
                                     r=[ku, 'pv'], w=[k3])
                                t.op('dve', lambda e: e.tensor_scalar(out=u1[:], in0=u1[:], scalar1=-7.0, scalar2=1.0, op0=ALU.max, op1=ALU.add),
                                     r=[k3], w=[k3])
                                t.op('pool', lambda e: e.tensor_tensor(out=g1[:], in0=g1[:], in1=sg[:], op=ALU.mult), r=[k1, k2], w=[k1])
                                t.op('dve', lambda e: e.tensor_tensor(out=actT[:, f, cs], in0=g1[:], in1=u1[:], op=ALU.mult),
                                     r=[k1, k3], w=['actT'])
                    for dq in range(4):
                        s0, s1 = wmi % 4, (wmi + 1) % 4
                        wmi += 2
                        bb = dq % 2
                        ds_ = slice(dq * 512, (dq + 1) * 512)
                        t.dma('pool', lambda e: e.dma_start(out=wmR[:, s0], in_=wdv[:, :, dq * 512:dq * 512 + 256]), w=[f'wm{s0}'])
                        t.dma('pool', lambda e: e.dma_start(out=wmR[:, s1], in_=wdv[:, :, dq * 512 + 256:dq * 512 + 512]), w=[f'wm{s1}'])
                        t.dma('sp', lambda e: e.dma_start(out=bdb[bb][:], in_=bd_d[ex:ex + 1, ds_].partition_broadcast(128)),
                              w=[f'bdb{bb}'])
                        for ti in range(8):
                            pd, kd = ps[4 + ti % 2], P(4 + ti % 2)
                            tmpb, tk = g1s[ti % 2], f'g1{ti % 2}'
                            for fc in range(16):
                                t.op('pe', lambda e: e.matmul(pd[:], lhsT=actT[:, fc, ti * 128:(ti + 1) * 128], rhs=wmR[:, s0:s0 + 2, fc, :],
                                                              start=(fc == 0), stop=(fc == 15)), r=['actT', f'wm{s0}', f'wm{s1}'], w=[kd])
                            t.op('dve', lambda e: e.tensor_tensor(out=tmpb[:], in0=pd[:], in1=bdb[bb][:], op=ALU.add),
                                 r=[kd, f'bdb{bb}'], w=[tk])
                            t.op('dve', lambda e: e.scalar_tensor_tensor(out=yacc[:, ti, ds_], in0=tmpb[:], scalar=wt[:, ti, ex:ex + 1],
                                                                         in1=yacc[:, ti, ds_], op0=ALU.mult, op1=ALU.add),
                                 r=[tk, 'wt', 'yacc'], w=['yacc'])
                for ti in range(8):
                    gt = hf * 8 + ti
                    for q in range(4):
                        qs = slice(q * 512, (q + 1) * 512)
                        g1, u1 = g1s[q % 2], u1s[q % 2]
                        k1, k3 = f'g1{q % 2}', f'u1{q % 2}'
                        t.dma('sp', lambda e: e.dma_start(out=g1[:], in_=x1_d[gt * 128:(gt + 1) * 128, qs]), r=['x1d'], w=[k1])
                        t.op('dve', lambda e: e.tensor_tensor(out=u1[:], in0=yacc[:, ti, qs], in1=G2[:, qs], op=ALU.mult),
                             r=['yacc', 'G2'], w=[k3])
                        t.op('dve', lambda e: e.tensor_tensor(out=g1[:], in0=g1[:], in1=u1[:], op=ALU.add), r=[k1, k3], w=[k1])
                        t.dma('sp', lambda e: e.dma_start(out=out_d[gt * 128:(gt + 1) * 128, qs], in_=g1[:]), r=[k1], w=['outd'])
                t.barrier()
    return nc


def _t5_bucket(dist):
    max_exact = 16
    d = np.maximum(dist, 0)
    lr = np.log(np.maximum(d, max_exact).astype(np.float32) / max_exact)
    large = max_exact + (lr / math.log(128 / max_exact) * (32 - max_exact)).astype(np.int32)
    large = np.minimum(large, 31)
    return np.where(d < max_exact, d, large)


def _prep(inputs):
    f = np.float32
    x = np.asarray(inputs["x"], f)[0]
    g = lambda k: np.asarray(inputs[k], f)[0]
    pp = lambda v: np.ascontiguousarray(v.reshape(-1, 128).T)
    perm = []
    for jj in range(8):
        perm += list(range(jj * 64, jj * 64 + 64)) + list(range((8 + jj) * 64, (8 + jj) * 64 + 64))
    perm += list(range(1024, 3328))
    perm = np.array(perm)
    win = np.ascontiguousarray(g("w_in")[:, perm])
    b_in = g("b_in")[perm]
    pv = np.zeros((128, NPV), f)
    pv[:, PV_C:PV_C + 16] = pp(np.asarray(inputs["c"], f)[0])
    pv[:, PV_G1:PV_G1 + 16] = pp(g("g_norm1"))
    pv[:, PV_BIN:PV_BIN + 26] = pp(b_in)
    pv[:, PV_GQ] = np.tile(g("g_q"), 2)
    pv[:, PV_GK] = np.tile(g("g_k"), 2)
    pv[:, PV_WDW:PV_WDW + 248] = g("w_dw").reshape(31, 8, 128).transpose(2, 1, 0).reshape(128, 248)
    pv[:, PV_BDW:PV_BDW + 8] = pp(g("b_dw"))
    pv[:, PV_LNG:PV_LNG + 8] = pp(g("ln_g"))
    pv[:, PV_LNB:PV_LNB + 8] = pp(g("ln_b"))
    pv[:, PV_GOC:PV_GOC + 8] = pp(g("g_out_conv"))
    pv[:, PV_BG:PV_BG + 512] = g("b_gate").reshape(32, 16, 128).transpose(2, 0, 1).reshape(128, 512)
    pv[:, PV_BU:PV_BU + 512] = g("b_up").reshape(32, 16, 128).transpose(2, 0, 1).reshape(128, 512)
    pv[:, PV_G2:PV_G2 + 16] = pp(g("g_norm2"))
    pv[:, PV_WR:PV_WR + 512] = g("w_router").reshape(16, 128, 32).transpose(1, 0, 2).reshape(128, 512)
    rv = np.zeros((1, NRV), f)
    rv[0, RV_GOA:RV_GOA + 1024] = g("g_out_attn")
    rv[0, RV_BOUT:RV_BOUT + D] = g("b_out")
    rv[0, RV_G2:RV_G2 + D] = g("g_norm2")
    rv[0, RV_BR:RV_BR + 32] = g("b_router")
    rv[0, RV_SINK:RV_SINK + 16] = g("sinks")
    rv = np.ascontiguousarray(np.broadcast_to(rv, (128, NRV)))
    rb = np.asarray(inputs["rel_bias"], f)
    kk = np.arange(128)[:, None]
    qq = np.arange(128)[None, :]
    bt = np.zeros((128, 2, 2, 8, 128), f)
    for kt in range(2):
        dist = qq + 128 - kk if kt == 0 else qq - kk
        valid = (dist >= 0) & (dist < 128)
        bk = _t5_bucket(dist)
        for gg in range(2):
            for jj in range(8):
                bt[:, kt, gg, jj, :] = np.where(valid, rb[bk, 8 * gg + jj], f(-30000.0))
    bt = bt.reshape(128, 4096)
    blk = np.zeros((128, 128), f)
    blk[:64, :64] = 1
    blk[64:, 64:] = 1
    common = dict(pv=pv, rv=rv, bada=g("b_ada")[None, :], wada=g("w_ada"), win=win, wout=g("w_out"), biast=bt,
                  wg=g("w_gate"), wu=g("w_up"), wd=g("w_down"), bd=g("b_down"),
                  identf=np.eye(128, dtype=f), blk=blk)
    in_maps = []
    for c in range(NCORE):
        xh = np.zeros((17 * 128, D), f)
        if c == 0:
            xh[128:] = x[0:TOK]
        else:
            xh[:] = x[c * TOK - 128:(c + 1) * TOK]
        hvv = np.full((128, 1), 0.0 if c == 0 else 1.0, f)
        m = dict(common)
        m["xh"] = xh
        m["hv"] = hvv
        in_maps.append(m)
    return in_maps


def kernel(**inputs):
    in_maps = _prep(inputs)
    nc = build_nc()
    res = run_bass_kernel_spmd(nc, in_maps, core_ids=list(range(NCORE)))
    out = np.concatenate([np.asarray(r["out"], np.float32) for r in res.results], axis=0)
    return out.reshape(1, NCORE * TOK, D)
```

```python
import math
from contextlib import ExitStack
import numpy as np
import concourse.bass as bass
import concourse.mybir as mybir
from concourse.bass_utils import run_bass_kernel_spmd

F32 = mybir.dt.float32
BF16 = mybir.dt.bfloat16
ALU = mybir.AluOpType
AF = mybir.ActivationFunctionType
AX = mybir.AxisListType

D = 2048
NCORE = 8
TOK = 2048
NT = 16
E = 32
EPS = 1e-6
PV_C, PV_G1, PV_BIN, PV_GQ, PV_GK = 0, 16, 32, 58, 59
PV_WDW, PV_BDW, PV_LNG, PV_LNB, PV_GOC = 60, 308, 316, 324, 332
PV_BG, PV_BU, PV_WR = 340, 852, 1364
PV_G2 = 1364 + 512
NPV = 1364 + 512 + 16
RV_GOA, RV_BOUT, RV_G2, RV_BR, RV_SINK = 0, 1024, 3072, 5120, 5152
NRV = 5168


class Trk:
    def __init__(s, nc):
        s.nc = nc
        s.eng = dict(pe=nc.tensor, act=nc.scalar, dve=nc.vector, pool=nc.gpsimd, sp=nc.sync)
        s.csem = {e: nc.alloc_semaphore("c_" + e) for e in s.eng}
        s.ND = 24
        s.dsem = [nc.alloc_semaphore(f"dq{i}") for i in range(s.ND)]
        s.reset_state()

    def reset_state(s):
        s.cnt = {e: 0 for e in s.eng}
        s.seen = {c: {p: 0 for p in s.eng} for c in s.eng}
        s.dval = [0] * s.ND
        s.dseen = {c: [0] * s.ND for c in s.eng}
        s.drr = 0
        s.lastw = {}
        s.readers = {}

    def _wait(s, c, tok):
        if tok[0] == 'e':
            _, p, seq = tok
            if p == c and p == 'pe':
                return
            if s.seen[c][p] >= seq:
                return
            s.eng[c].wait_ge(s.csem[p], seq)
            s.seen[c][p] = seq
        else:
            _, k, val = tok
            if s.dseen[c][k] >= val:
                return
            s.eng[c].wait_ge(s.dsem[k], val)
            s.dseen[c][k] = val

    def _deps(s, c, r, w):
        for b in r:
            t = s.lastw.get(b)
            if t:
                s._wait(c, t)
        for b in w:
            t = s.lastw.get(b)
            if t:
                s._wait(c, t)
            rd = s.readers.get(b)
            if rd:
                for p, seq in rd[0].items():
                    if p != c:
                        s._wait(c, ('e', p, seq))
                for t in rd[1]:
                    s._wait(c, t)

    def _record(s, tok, r, w):
        for b in r:
            rd = s.readers.setdefault(b, [{}, []])
            if tok[0] == 'e':
                rd[0][tok[1]] = tok[2]
            else:
                rd[1].append(tok)
        for b in w:
            s.lastw[b] = tok
            s.readers[b] = [{}, []]

    def op(s, c, fn, r=(), w=()):
        s._deps(c, r, w)
        ins = fn(s.eng[c])
        s.cnt[c] += 1
        ins.then_inc(s.csem[c], 1)
        s._record(('e', c, s.cnt[c]), r, w)

    def dma(s, c, fn, r=(), w=()):
        s._deps(c, r, w)
        k = s.drr
        s.drr = (s.drr + 1) % s.ND
        if s.dval[k] > 0:
            s._wait(c, ('d', k, s.dval[k]))
        ins = fn(s.eng[c])
        s.dval[k] += 16
        ins.then_inc(s.dsem[k], 16)
        s._record(('d', k, s.dval[k]), r, w)

    def barrier(s):
        for c in s.eng:
            for p in s.eng:
                if s.cnt[p] > 0:
                    s._wait(c, ('e', p, s.cnt[p]))
            for k in range(s.ND):
                if s.dval[k] > 0:
                    s._wait(c, ('d', k, s.dval[k]))
        s.lastw = {}
        s.readers = {}


def build_nc(dbg=None):
    dbg = dbg or {}
    STOP = dbg.get('stop')
    HALVES = dbg.get('halves', [0, 1])
    GROUPS = dbg.get('groups', [0, 1])
    NEXP = dbg.get('n_exp', E)
    TAPS = dbg.get('taps', False)
    nc = bass.Bass("TRN2", target_bir_lowering=False)

    def din(name, shape, dt=F32):
        return nc.dram_tensor(name, list(shape), dt, kind="ExternalInput").ap()

    xh_d = din("xh", [17 * 128, D])
    hv_d = din("hv", [128, 1])
    pv_d = din("pv", [128, NPV])
    rv_d = din("rv", [128, NRV])
    bada_d = din("bada", [1, 6 * D])
    wada_d = din("wada", [D, 6 * D])
    win_d = din("win", [D, 3328])
    wout_d = din("wout", [D, D])
    bias_d = din("biast", [128, 4096])
    wg_d = din("wg", [NEXP, 8, 128, 16, 256])
    wu_d = din("wu", [NEXP, 8, 128, 16, 256])
    wd_d = din("wd", [NEXP, 8, 128, 16, 256])
    bd_d = din("bd", [NEXP, D])
    identf_d = din("identf", [128, 128])
    blk_d = din("blk", [128, 128])
    out_d = nc.dram_tensor("out", [TOK, D], F32, kind="ExternalOutput").ap()
    x1_d = nc.dram_tensor("x1s", [TOK, D], F32, kind=("ExternalOutput" if TAPS else "Internal")).ap()
    g1s_d = nc.dram_tensor("g1s", [2, 128, D], F32, kind="Internal").ap()

    t = Trk(nc)

    def tap(name, tens, shape, dt, key):
        if not TAPS:
            return
        dd = nc.dram_tensor("tap_" + name, list(shape), dt, kind="ExternalOutput").ap()
        t.dma('sp', lambda e: e.dma_start(out=dd, in_=tens[:]), r=[key], w=['tap_' + name])
    _uid = [0]

    def SB(name, shape, dt):
        _uid[0] += 1
        return nc.alloc_sbuf_tensor(f"{name}_s{_uid[0]}", shape, dt)

    def SBT(name, shape, dt):
        _uid[0] += 1
        return nc.sbuf_tensor(f"{name}_s{_uid[0]}", shape, dt)

    pv = SB("pv", [128, NPV], F32)
    hv = SB("hv", [128, 1], F32)
    identf = SB("identf", [128, 128], F32)
    identb = SB("identb", [128, 128], BF16)
    blkb = SB("blkb", [128, 128], BF16)
    onesf = SB("onesf", [128, 128], F32)
    sT = SB("sT", [128, 16], BF16)
    modT = SB("modT", [128, 4, 16], F32)
    A2 = SB("A2", [128, 16], F32)
    A1 = SB("A1", [128, 16], F32)
    G2 = SB("G2", [128, D], F32)
    goa = SB("goa", [128, 1024], F32)
    brt = SB("brt", [128, 32], F32)
    esink = SB("esink", [128, 16], F32)
    ebt = SB("ebt", [128, 4096], BF16)
    ebt0 = SB("ebt0", [128, 2048], BF16)
    wrb = SB("wrb", [128, 512], BF16)
    ssq = SB("ssq", [128, 1], F32)
    rstd = SB("rstd", [128, 1], F32)

    ps = [nc.alloc_psum_tensor(f"ps{i}", [128, 512], F32) for i in range(6)]
    psT = [nc.alloc_psum_tensor(f"psT{i}", [128, 1024], BF16) for i in range(2)]

    def P(i):
        return f"ps{i}"

    t.dma('sp', lambda e: e.dma_start(out=pv[:], in_=pv_d), w=['pv'])
    t.dma('sp', lambda e: e.dma_start(out=hv[:], in_=hv_d), w=['hv'])
    t.dma('sp', lambda e: e.dma_start(out=identf[:], in_=identf_d), w=['identf'])
    t.dma('pool', lambda e: e.dma_start(out=identb[:], in_=identf_d), w=['identb'])
    t.dma('pool', lambda e: e.dma_start(out=blkb[:], in_=blk_d), w=['blkb'])
    t.op('dve', lambda e: e.memset(onesf[:], 1.0), w=['onesf'])
    t.op('dve', lambda e: e.tensor_copy(out=wrb[:], in_=pv[:, PV_WR:PV_WR + 512]), r=['pv'], w=['wrb'])
    t.op('act', lambda e: e.activation(out=sT[:], in_=pv[:, PV_C:PV_C + 16], func=AF.Silu), r=['pv'], w=['sT'])

    with ExitStack() as _es:
        rvt = _es.enter_context(SBT("rvt", [128, NRV], F32))
        biasf = _es.enter_context(SBT("biasf", [128, 4096], F32))
        wr0 = _es.enter_context(SBT("wr0", [128, 16, 512], BF16))
        wr1 = _es.enter_context(SBT("wr1", [128, 16, 512], BF16))
        brow0 = _es.enter_context(SBT("brow0", [1, 512], F32))
        brow1 = _es.enter_context(SBT("brow1", [1, 512], F32))
        mrow = _es.enter_context(SBT("mrow", [1, 512], F32))
        bcg = _es.enter_context(SBT("bcg", [128, D], F32))
        gb1 = _es.enter_context(SBT("gb1", [128, D], F32))
        wr = [wr0, wr1]
        brow = [brow0, brow1]
        t.dma('sp', lambda e: e.dma_start(out=rvt[:], in_=rv_d), w=['rvt'])
        t.dma('sp', lambda e: e.dma_start(out=biasf[:], in_=bias_d), w=['biasf'])
        t.op('act', lambda e: e.activation(out=ebt[:], in_=biasf[:], func=AF.Exp), r=['biasf'], w=['ebt'])
        t.op('dve', lambda e: e.tensor_scalar(out=ebt0[:], in0=ebt[:, 0:2048], scalar1=hv[:, 0:1], scalar2=None,
                                              op0=ALU.mult), r=['ebt', 'hv'], w=['ebt0'])
        t.op('act', lambda e: e.activation(out=esink[:], in_=rvt[:, RV_SINK:RV_SINK + 16], func=AF.Exp),
             r=['rvt'], w=['esink'])
        t.op('dve', lambda e: e.tensor_copy(out=goa[:], in_=rvt[:, RV_GOA:RV_GOA + 1024]), r=['rvt'], w=['goa'])
        t.op('dve', lambda e: e.tensor_copy(out=brt[:], in_=rvt[:, RV_BR:RV_BR + 32]), r=['rvt'], w=['brt'])

        wada_v = wada_d.rearrange("(kc p) f -> p kc f", p=128)
        for n in range(24):
            b = n % 2
            t.dma('pool', lambda e: e.dma_start(out=wr[b][:], in_=wada_v[:, :, n * 512:(n + 1) * 512]), w=[f'wr{b}'])
            t.dma('sp', lambda e: e.dma_start(out=brow[b][:], in_=bada_d[0:1, n * 512:(n + 1) * 512]), w=[f'brow{b}'])
            for kc in range(16):
                t.op('pe', lambda e: e.matmul(ps[0][0:1, :], lhsT=sT[:, kc:kc + 1], rhs=wr[b][:, kc, :],
                                              start=(kc == 0), stop=(kc == 15)), r=['sT', f'wr{b}'], w=[P(0)])
            t.op('dve', lambda e: e.tensor_tensor(out=mrow[:], in0=ps[0][0:1, :], in1=brow[b][:], op=ALU.add),
                 r=[P(0), f'brow{b}'], w=['mrow'])
            which, q = n // 4, n % 4
            if which in (0, 1, 3, 4):
                mi = {0: 0, 1: 1, 3: 2, 4: 3}[which]
                for j in range(4):
                    t.op('pe', lambda e: e.matmul(ps[1][:, j:j + 1], lhsT=mrow[0:1, j * 128:(j + 1) * 128],
                                                  rhs=onesf[0:1, 0:1], start=True, stop=True), r=['mrow', 'onesf'], w=[P(1)])
                t.op('dve', lambda e: e.tensor_copy(out=modT[:, mi, q * 4:(q + 1) * 4], in_=ps[1][:, 0:4]),
                     r=[P(1)], w=['modT'])
            else:
                dstt, dk = (bcg, 'bcg') if which == 2 else (G2, 'G2')
                t.op('pe', lambda e: e.matmul(ps[2][:, :], lhsT=onesf[0:1, 0:128], rhs=mrow[0:1, :],
                                              start=True, stop=True), r=['mrow', 'onesf'], w=[P(2)])
                t.op('act', lambda e: e.activation(out=dstt[:, q * 512:(q + 1) * 512], in_=ps[2][:, :], func=AF.Identity),
                     r=[P(2)], w=[dk])
        t.op('dve', lambda e: e.scalar_tensor_tensor(out=A1[:], in0=modT[:, 1, :], scalar=1.0, in1=pv[:, PV_G1:PV_G1 + 16],
                                                     op0=ALU.add, op1=ALU.mult), r=['modT', 'pv'], w=['A1'])
        t.op('dve', lambda e: e.scalar_tensor_tensor(out=A2[:], in0=modT[:, 3, :], scalar=1.0, in1=pv[:, PV_G2:PV_G2 + 16],
                                                     op0=ALU.add, op1=ALU.mult), r=['modT', 'pv'], w=['A2'])
        t.op('dve', lambda e: e.tensor_tensor(out=gb1[:], in0=bcg[:], in1=rvt[:, RV_BOUT:RV_BOUT + D], op=ALU.mult),
             r=['bcg', 'rvt'], w=['gb1'])
        t.dma('sp', lambda e: e.dma_start(out=g1s_d[0], in_=bcg[:]), r=['bcg'], w=['g1sd'])
        t.dma('sp', lambda e: e.dma_start(out=g1s_d[1], in_=gb1[:]), r=['gb1'], w=['g1sd'])
        tap('modT', modT, [128, 4, 16], F32, 'modT')
        tap('A1', A1, [128, 16], F32, 'A1')
        tap('bc0', bcg, [128, D], F32, 'bcg')
        tap('ebt', ebt, [128, 4096], BF16, 'ebt')
        t.barrier()
        if STOP == 'ada':
            return nc

    win_v = win_d.rearrange("(kc p) f -> p kc f", p=128)
    wout_v = wout_d.rearrange("(kc p) f -> p kc f", p=128)

    def rsqrt_into(dst, src_ap, scale, r, w):
        t.op('act', lambda e: e.activation(out=dst, in_=src_ap, func=AF.Sqrt, bias=EPS, scale=scale), r=r, w=w)
        t.op('dve', lambda e: e.reciprocal(out=dst, in_=dst), r=w, w=w)

    for hf in HALVES:
        with ExitStack() as _esh:
            h2T = _esh.enter_context(SBT("h2T", [128, 16, 1024], BF16))
            wt = _esh.enter_context(SBT("wt", [128, 8, 32], F32))
            with ExitStack() as _esm:
                mixedT = _esm.enter_context(SBT("mixedT", [128, 16, 1024], BF16))
                with ExitStack() as _es:
                    qT = _es.enter_context(SBT("qT", [128, 8, 1152], BF16))
                    kT = _es.enter_context(SBT("kT", [128, 1152], BF16))
                    v1 = _es.enter_context(SBT("v1", [128, 9, 2, 65], BF16))
                    hglu = _es.enter_context(SBT("hglu", [128, 8, 1152], BF16))
                    t.op('pool', lambda e: e.memset(v1[:], 1.0), w=['v1'])
                    with ExitStack() as _es:
                        hT = _es.enter_context(SBT("hT", [128, 16, 1152], BF16))
                        xb0 = _es.enter_context(SBT("xb0", [128, D], F32))
                        xs = _es.enter_context(SBT("xs", [128, D], BF16))
                        wi0 = _es.enter_context(SBT("wi0", [128, 16, 256], BF16))
                        wi1 = _es.enter_context(SBT("wi1", [128, 16, 256], BF16))
                        tA = xb0[:, 0:512]
                        tB = xs[:, 0:512]
                        tC = xb0[:, 512:1024]
                        xb = [xb0, xb0]; junk = xs
                        wi = [wi0, wi1]
                        for tt in range(9):
                            xt = xb[tt % 2]
                            xk = 'xb0'
                            r0 = (hf * 8 + tt) * 128
                            t.dma('sp', lambda e: e.dma_start(out=xt[:], in_=xh_d[r0:r0 + 128, :]), w=[xk])
                            t.op('dve', lambda e: e.memset(ssq[:], 0.0), w=['ssq'])
                            t.op('act', lambda e: e.activation(out=junk[:], in_=xt[:], func=AF.Square, accum_out=ssq[:, 0:1]),
                                 r=[xk, 'ssq'], w=['xs', 'ssq'])
                            rsqrt_into(rstd[:], ssq[:], 1.0 / D, ['ssq'], ['rstd'])
                            t.op('dve', lambda e: e.tensor_scalar(out=xs[:], in0=xt[:], scalar1=rstd[:, 0:1], scalar2=None, op0=ALU.mult),
                                 r=[xk, 'rstd'], w=['xs'])
                            for c in range(16):
                                t.op('pe', lambda e: e.transpose(out=psT[c // 8][:, (c % 8) * 128:(c % 8 + 1) * 128],
                                                                 in_=xs[:, c * 128:(c + 1) * 128], identity=identb[:]),
                                     r=['xs', 'identb'], w=[f'psT{c // 8}'])
                            for c in range(16):
                                t.op('dve', lambda e: e.tensor_scalar(out=hT[:, c, tt * 128:(tt + 1) * 128],
                                                                      in0=psT[c // 8][:, (c % 8) * 128:(c % 8 + 1) * 128],
                                                                      scalar1=A1[:, c:c + 1], scalar2=modT[:, 0, c:c + 1],
                                                                      op0=ALU.mult, op1=ALU.add),
                                     r=[f'psT{c // 8}', 'A1', 'modT'], w=['hT'])
                        if STOP == 'norm':
                            tap(f'hT{hf}', hT, [128, 16, 1152], BF16, 'hT')
                            t.barrier()
                            return nc
                        t.barrier()
                        chunks = [(0, 128), (128, 512), (640, 512)]
                        for j in dbg.get('jlist', range(26)):
                            wc = j // 2
                            b = wc % 2
                            if j % 2 == 0 or 'jlist' in dbg:
                                t.dma('pool', lambda e: e.dma_start(out=wi[b][:], in_=win_v[:, :, wc * 256:wc * 256 + 256]),
                                      w=[f'wi{b}'])
                            jo = (j % 2) * 128
                            bias = pv[:, PV_BIN + j:PV_BIN + j + 1]
                            for ci, (c0, cn) in enumerate(chunks):
                                pb = ps[ci % 2]
                                for kc in range(0 if dbg.get('nomm') else 16):
                                    t.op('pe', lambda e: e.matmul(pb[:, 0:cn], lhsT=wi[b][:, kc, jo:jo + 128], rhs=hT[:, kc, c0:c0 + cn],
                                                                  start=(kc == 0), stop=(kc == 15)), r=[f'wi{b}', 'hT'], w=[P(ci % 2)])
                                if dbg.get('noevac'):
                                    continue
                                if j < 9:
                                    gcol = PV_GQ if j < 8 else PV_GK
                                    dst = qT[:, j, c0:c0 + cn] if j < 8 else kT[:, c0:c0 + cn]
                                    dk = 'qT' if j < 8 else 'kT'
                                    t.op('act', lambda e: e.activation(out=tB[:, 0:cn], in_=pb[:, 0:cn], func=AF.Square, bias=bias),
                                         r=[P(ci % 2), 'pv'], w=['tB'])
                                    t.op('act', lambda e: e.activation(out=tA[:, 0:cn], in_=pb[:, 0:cn], func=AF.Identity, bias=bias),
                                         r=[P(ci % 2), 'pv'], w=['tA'])
                                    t.op('pe', lambda e: e.matmul(ps[2][:, 0:cn], lhsT=blkb[:], rhs=tB[:, 0:cn], start=True, stop=True),
                                         r=['blkb', 'tB'], w=[P(2)])
                                    rsqrt_into(tC[:, 0:cn], ps[2][:, 0:cn], 1.0 / 64, [P(2)], ['tC'])
                                    t.op('dve', lambda e: e.scalar_tensor_tensor(out=dst, in0=tA[:, 0:cn], scalar=pv[:, gcol:gcol + 1],
                                                                                 in1=tC[:, 0:cn], op0=ALU.mult, op1=ALU.mult),
                                         r=['tA', 'tC', 'pv'], w=[dk])
                                elif j == 9:
                                    t.op('act', lambda e: e.activation(out=tB[:, 0:cn], in_=pb[:, 0:cn], func=AF.Identity, bias=bias),
                                         r=[P(ci % 2), 'pv'], w=['tB'])
                                    for s_ in range(cn // 128):
                                        tt = c0 // 128 + s_
                                        t.op('pe', lambda e: e.transpose(out=psT[0][:, 0:128], in_=tB[:, s_ * 128:(s_ + 1) * 128],
                                                                         identity=identb[:]), r=['tB', 'identb'], w=['psT0'])
                                        t.op('dve', lambda e: e.tensor_copy(out=v1[:, tt, :, 0:64],
                                                                            in_=psT[0][:, 0:128].rearrange("p (g d) -> p g d", g=2)),
                                             r=['psT0'], w=['v1'])
                                elif j < 18:
                                    c = j - 10
                                    t.op('dve', lambda e: e.tensor_scalar(out=hglu[:, c, c0:c0 + cn], in0=pb[:, 0:cn], scalar1=bias,
                                                                          scalar2=None, op0=ALU.add), r=[P(ci % 2), 'pv'], w=['hglu'])
                                else:
                                    c = j - 18
                                    t.op('act', lambda e: e.activation(out=tB[:, 0:cn], in_=pb[:, 0:cn], func=AF.Sigmoid, bias=bias),
                                         r=[P(ci % 2), 'pv'], w=['tB'])
                                    t.op('pool', lambda e: e.tensor_tensor(out=hglu[:, c, c0:c0 + cn], in0=hglu[:, c, c0:c0 + cn],
                                                                           in1=tB[:, 0:cn], op=ALU.mult), r=['tB', 'hglu'], w=['hglu'])
                        if hf == 0 and not dbg.get('nohv'):
                            for c in range(8):
                                t.op('pool', lambda e: e.tensor_scalar(out=hglu[:, c, 0:128], in0=hglu[:, c, 0:128], scalar1=hv[:, 0:1],
                                                                       scalar2=None, op0=ALU.mult), r=['hglu', 'hv'], w=['hglu'])
                        tap(f'hT{hf}', hT, [128, 16, 1152], BF16, 'hT')
                        tap(f'qT{hf}', qT, [128, 8, 1152], BF16, 'qT')
                        tap(f'kT{hf}', kT, [128, 1152], BF16, 'kT')
                        tap(f'v1{hf}', v1, [128, 9, 2, 65], BF16, 'v1')
                        tap(f'hglu{hf}', hglu, [128, 8, 1152], BF16, 'hglu')
                        t.barrier()
                        if STOP == 'inproj':
                            return nc

                    with ExitStack() as _es:
                        acc = _es.enter_context(SBT("acc", [128, 8, 1024], F32))
                        sq = _es.enter_context(SBT("sq", [128, 512], F32))
                        mean = _es.enter_context(SBT("mean", [128, 512], F32))
                        var = _es.enter_context(SBT("var", [128, 512], F32))
                        tmp = _es.enter_context(SBT("tmp", [128, 512], F32))
                        yatt = _es.enter_context(SBT("yatt", [128, 1024], F32))
                        ynb = _es.enter_context(SBT("ynb", [128, 1024], BF16))
                        pe0 = _es.enter_context(SBT("pe0", [128, 512], F32))
                        pe1 = _es.enter_context(SBT("pe1", [128, 512], F32))
                        pT0 = _es.enter_context(SBT("pT0", [128, 1024], BF16))
                        pT1 = _es.enter_context(SBT("pT1", [128, 1024], BF16))
                        den = _es.enter_context(SBT("den", [128, 8], F32))
                        junk2 = _es.enter_context(SBT("junk2", [128, 1024], BF16))
                        for c in range(8):
                            en = 'dve'
                            ak = f'acc{c}'
                            wc0 = PV_WDW + c * 31
                            t.op(en, lambda e: e.tensor_scalar(out=acc[:, c, :], in0=hglu[:, c, 98:98 + 1024], scalar1=pv[:, wc0:wc0 + 1],
                                                               scalar2=pv[:, PV_BDW + c:PV_BDW + c + 1], op0=ALU.mult, op1=ALU.add),
                                 r=['hglu', 'pv'], w=[ak])
                            for j in range(1, 31):
                                if en == 'dve':
                                    t.op(en, lambda e: e.scalar_tensor_tensor(out=acc[:, c, :], in0=hglu[:, c, 98 + j:98 + j + 1024],
                                                                              scalar=pv[:, wc0 + j:wc0 + j + 1], in1=acc[:, c, :],
                                                                              op0=ALU.mult, op1=ALU.add), r=['hglu', 'pv', ak], w=[ak])
                                else:
                                    t.op(en, lambda e: e.tensor_scalar(out=ctmp[:], in0=hglu[:, c, 98 + j:98 + j + 1024],
                                                                       scalar1=pv[:, wc0 + j:wc0 + j + 1], scalar2=None, op0=ALU.mult),
                                         r=['hglu', 'pv'], w=['ctmp'])
                                    t.op(en, lambda e: e.tensor_tensor(out=acc[:, c, :], in0=acc[:, c, :], in1=ctmp[:], op=ALU.add),
                                         r=['ctmp', ak], w=[ak])
                        for ch in range(2):
                            cs = slice(ch * 512, (ch + 1) * 512)
                            for c in range(8):
                                t.op('act', lambda e: e.activation(out=sq[:], in_=acc[:, c, cs], func=AF.Square), r=[f'acc{c}'], w=['sq'])
                                t.op('pe', lambda e: e.matmul(ps[0][:], lhsT=onesf[:], rhs=acc[:, c, cs], start=(c == 0), stop=(c == 7)),
                                     r=['onesf', f'acc{c}'], w=[P(0)])
                                t.op('pe', lambda e: e.matmul(ps[1][:], lhsT=onesf[:], rhs=sq[:], start=(c == 0), stop=(c == 7)),
                                     r=['onesf', 'sq'], w=[P(1)])
                            t.op('dve', lambda e: e.tensor_scalar(out=mean[:], in0=ps[0][:], scalar1=1.0 / 1024, scalar2=None, op0=ALU.mult),
                                 r=[P(0)], w=['mean'])
                            t.op('dve', lambda e: e.tensor_tensor(out=tmp[:], in0=mean[:], in1=mean[:], op=ALU.mult), r=['mean'], w=['tmp'])
                            t.op('dve', lambda e: e.scalar_tensor_tensor(out=var[:], in0=ps[1][:], scalar=1.0 / 1024, in1=tmp[:],
                                                                         op0=ALU.mult, op1=ALU.subtract), r=[P(1), 'tmp'], w=['var'])
                            rsqrt_into(var[:], var[:], 1.0, ['var'], ['var'])
                            for c in range(8):
                                ak = f'acc{c}'
                                t.op('dve', lambda e: e.tensor_tensor(out=tmp[:], in0=acc[:, c, cs], in1=mean[:], op=ALU.subtract),
                                     r=[ak, 'mean'], w=['tmp'])
                                t.op('dve', lambda e: e.tensor_tensor(out=tmp[:], in0=tmp[:], in1=var[:], op=ALU.mult), r=['tmp', 'var'], w=['tmp'])
                                t.op('act', lambda e: e.activation(out=acc[:, c, cs], in_=tmp[:], func=AF.Silu,
                                                                   bias=pv[:, PV_LNB + c:PV_LNB + c + 1], scale=pv[:, PV_LNG + c:PV_LNG + c + 1]),
                                     r=['tmp', 'pv'], w=[ak])
                                t.op('act', lambda e: e.activation(out=sq[:], in_=acc[:, c, cs], func=AF.Square), r=[ak], w=['sq'])
                                t.op('pe', lambda e: e.matmul(ps[2][:], lhsT=onesf[:], rhs=sq[:], start=(c == 0), stop=(c == 7)),
                                     r=['onesf', 'sq'], w=[P(2)])
                            rsqrt_into(var[:], ps[2][:], 1.0 / 1024, [P(2)], ['var'])
                            for c in range(8):
                                t.op('dve', lambda e: e.scalar_tensor_tensor(out=mixedT[:, 8 + c, cs], in0=acc[:, c, cs],
                                                                             scalar=pv[:, PV_GOC + c:PV_GOC + c + 1], in1=var[:],
                                                                             op0=ALU.mult, op1=ALU.mult), r=[f'acc{c}', 'var', 'pv'], w=['mixedT'])

                        pes = [pe0, pe1]
                        pTs = [pT0, pT1]
                        for n in range(8):
                            tt = n + 1
                            for g in range(2):
                                gp = slice(g * 64, (g + 1) * 64)
                                for kt in range(2):
                                    kc0 = (tt - 1 + kt) * 128
                                    for hh in range(2):
                                        pi = 2 + hh
                                        t.op('pe', lambda e: e.matmul(ps[pi][:], lhsT=kT[gp, kc0:kc0 + 128],
                                                                      rhs=qT[gp, 4 * hh:4 * hh + 4, tt * 128:(tt + 1) * 128],
                                                                      start=True, stop=True), r=['kT', 'qT'], w=[P(pi)])
                                        t.op('act', lambda e: e.activation(out=pes[hh][:], in_=ps[pi][:], func=AF.Exp, scale=0.125),
                                             r=[P(pi)], w=[f'pe{hh}'])
                                        if kt == 0 and n == 0 and hf == 0:
                                            eb = ebt0[:, g * 1024 + hh * 512:g * 1024 + (hh + 1) * 512]
                                        else:
                                            o = kt * 2048 + g * 1024 + hh * 512
                                            eb = ebt[:, o:o + 512]
                                        t.op('dve', lambda e: e.tensor_tensor(out=pTs[kt][:, hh * 512:(hh + 1) * 512], in0=pes[hh][:], in1=eb,
                                                                              op=ALU.mult), r=[f'pe{hh}', 'ebt', 'ebt0'], w=[f'pT{kt}'])
                                for jj in range(8):
                                    pi = 4 + jj // 4
                                    oc = (jj % 4) * 65
                                    for kt in range(2):
                                        t.op('pe', lambda e: e.matmul(ps[pi][:, oc:oc + 65], lhsT=pTs[kt][:, jj * 128:(jj + 1) * 128],
                                                                      rhs=v1[:, tt - 1 + kt, g, :], start=(kt == 0), stop=(kt == 1)),
                                             r=[f'pT{kt}', 'v1'], w=[P(pi)])
                                for half in range(2):
                                    pi = 4 + half
                                    pv3 = ps[pi][:, 0:260].rearrange("p (h d) -> p h d", d=65)
                                    hs = 8 * g + 4 * half
                                    t.op('dve', lambda e: e.tensor_tensor(out=den[:, 0:4], in0=pv3[:, :, 64], in1=esink[:, hs:hs + 4], op=ALU.add),
                                         r=[P(pi), 'esink'], w=['den'])
                                    t.op('dve', lambda e: e.reciprocal(out=den[:, 0:4], in_=den[:, 0:4]), r=['den'], w=['den'])
                                    for j4 in range(4):
                                        h = hs + j4
                                        t.op('dve', lambda e: e.tensor_scalar(out=yatt[:, h * 64:(h + 1) * 64], in0=pv3[:, j4, 0:64],
                                                                              scalar1=den[:, j4:j4 + 1], scalar2=None, op0=ALU.mult),
                                             r=[P(pi), 'den'], w=['yatt'])
                            t.op('dve', lambda e: e.memset(ssq[:], 0.0), w=['ssq'])
                            t.op('act', lambda e: e.activation(out=junk2[:], in_=yatt[:], func=AF.Square, accum_out=ssq[:, 0:1]),
                                 r=['yatt', 'ssq'], w=['junk2', 'ssq'])
                            rsqrt_into(rstd[:], ssq[:], 1.0 / 1024, ['ssq'], ['rstd'])
                            t.op('dve', lambda e: e.scalar_tensor_tensor(out=ynb[:], in0=yatt[:], scalar=rstd[:, 0:1], in1=goa[:],
                                                                         op0=ALU.mult, op1=ALU.mult), r=['yatt', 'rstd', 'goa'], w=['ynb'])
                            for c in range(8):
                                t.op('pe', lambda e: e.transpose(out=psT[1][:, c * 128:(c + 1) * 128], in_=ynb[:, c * 128:(c + 1) * 128],
                                                                 identity=identb[:]), r=['ynb', 'identb'], w=['psT1'])
                            t.op('dve', lambda e: e.tensor_copy(out=mixedT[:, 0:8, n * 128:(n + 1) * 128],
                                                                in_=psT[1][:, :].rearrange("p (c q) -> p c q", c=8)), r=['psT1'], w=['mixedT'])
                        tap(f'mixedT{hf}', mixedT, [128, 16, 1024], BF16, 'mixedT')
                        t.barrier()
                        if STOP == 'mixer':
                            return nc


                with ExitStack() as _es:
                    wo = _es.enter_context(SBT("wo", [128, 16, D], BF16))
                    G1 = _es.enter_context(SBT("G1", [128, D], F32))
                    GB1 = _es.enter_context(SBT("GB1", [128, D], F32))
                    xt2 = _es.enter_context(SBT("xt2", [128, D], F32))
                    x1 = _es.enter_context(SBT("x1", [128, D], F32))
                    h2b = _es.enter_context(SBT("h2b", [128, D], BF16))
                    lg = _es.enter_context(SBT("lg", [128, 32], F32))
                    top8 = _es.enter_context(SBT("top8", [128, 8], F32))
                    msk = _es.enter_context(SBT("msk", [128, 32], F32))
                    sm = _es.enter_context(SBT("sm", [128, 2], F32))
                    t.dma('sp', lambda e: e.dma_start(out=G1[:], in_=g1s_d[0]), r=['g1sd'], w=['G1'])
                    t.dma('sp', lambda e: e.dma_start(out=GB1[:], in_=g1s_d[1]), r=['g1sd'], w=['GB1'])
                    for q in range(4):
                        t.dma('pool', lambda e: e.dma_start(out=wo[:, :, q * 512:(q + 1) * 512], in_=wout_v[:, :, q * 512:(q + 1) * 512]),
                              w=['wo'])
                    for lt in range(8):
                        gt = hf * 8 + lt
                        t.dma('sp', lambda e: e.dma_start(out=xt2[:], in_=xh_d[(gt + 1) * 128:(gt + 2) * 128, :]), w=['xt2'])
                        t.op('pool', lambda e: e.tensor_tensor(out=xt2[:], in0=xt2[:], in1=GB1[:], op=ALU.add), r=['xt2', 'GB1'], w=['xt2'])
                        for nq in range(4):
                            for cc in range(16):
                                t.op('pe', lambda e: e.matmul(ps[nq][:], lhsT=mixedT[:, cc, lt * 128:(lt + 1) * 128],
                                                              rhs=wo[:, cc, nq * 512:(nq + 1) * 512], start=(cc == 0), stop=(cc == 15)),
                                     r=['mixedT', 'wo'], w=[P(nq)])
                            ns = slice(nq * 512, (nq + 1) * 512)
                            t.op('dve', lambda e: e.tensor_tensor(out=x1[:, ns], in0=ps[nq][:], in1=G1[:, ns], op=ALU.mult),
                                 r=[P(nq), 'G1'], w=['x1'])
                        t.op('dve', lambda e: e.tensor_tensor(out=x1[:], in0=x1[:], in1=xt2[:], op=ALU.add), r=['x1', 'xt2'], w=['x1'])
                        t.dma('sp', lambda e: e.dma_start(out=x1_d[gt * 128:(gt + 1) * 128, :], in_=x1[:]), r=['x1'], w=['x1d'])
                        t.op('dve', lambda e: e.memset(ssq[:], 0.0), w=['ssq'])
                        t.op('act', lambda e: e.activation(out=h2b[:], in_=x1[:], func=AF.Square, accum_out=ssq[:, 0:1]),
                             r=['x1', 'ssq'], w=['h2b', 'ssq'])
                        rsqrt_into(rstd[:], ssq[:], 1.0 / D, ['ssq'], ['rstd'])
                        t.op('dve', lambda e: e.tensor_scalar(out=h2b[:], in0=x1[:], scalar1=rstd[:, 0:1], scalar2=None, op0=ALU.mult),
                             r=['x1', 'rstd'], w=['h2b'])
                        for cc in range(16):
                            t.op('pe', lambda e: e.transpose(out=psT[cc // 8][:, (cc % 8) * 128:(cc % 8 + 1) * 128],
                                                             in_=h2b[:, cc * 128:(cc + 1) * 128], identity=identb[:]),
                                 r=['h2b', 'identb'], w=[f'psT{cc // 8}'])
                        for cc in range(16):
                            pin = psT[cc // 8][:, (cc % 8) * 128:(cc % 8 + 1) * 128]
                            if cc % 2 == 0:
                                t.op('dve', lambda e: e.tensor_scalar(out=h2T[:, cc, lt * 128:(lt + 1) * 128], in0=pin,
                                                                      scalar1=A2[:, cc:cc + 1], scalar2=modT[:, 2, cc:cc + 1],
                                                                      op0=ALU.mult, op1=ALU.add),
                                     r=[f'psT{cc // 8}', 'A2', 'modT'], w=['h2T'])
                            else:
                                t.op('act', lambda e: e.activation(out=h2T[:, cc, lt * 128:(lt + 1) * 128], in_=pin, func=AF.Identity,
                                                                   bias=modT[:, 2, cc:cc + 1], scale=A2[:, cc:cc + 1]),
                                     r=[f'psT{cc // 8}', 'A2', 'modT'], w=['h2T'])
                        for cc in range(16):
                            t.op('pe', lambda e: e.matmul(ps[5][:, 0:32], lhsT=h2T[:, cc, lt * 128:(lt + 1) * 128],
                                                          rhs=wrb[:, cc * 32:(cc + 1) * 32],
                                                          start=(cc == 0), stop=(cc == 15)), r=['h2T', 'wrb'], w=[P(5)])
                        t.op('dve', lambda e: e.tensor_tensor(out=lg[:], in0=ps[5][:, 0:32], in1=brt[:], op=ALU.add), r=[P(5), 'brt'], w=['lg'])
                        t.op('dve', lambda e: e.max(out=top8[:], in_=lg[:]), r=['lg'], w=['top8'])
                        t.op('dve', lambda e: e.tensor_scalar(out=msk[:], in0=lg[:], scalar1=top8[:, 3:4], scalar2=None, op0=ALU.is_ge),
                             r=['lg', 'top8'], w=['msk'])
                        t.op('dve', lambda e: e.tensor_scalar(out=sm[:, 0:1], in0=top8[:, 0:1], scalar1=-1.0, scalar2=None, op0=ALU.mult),
                             r=['top8'], w=['sm'])
                        t.op('act', lambda e: e.activation(out=lg[:], in_=lg[:], func=AF.Exp, bias=sm[:, 0:1]), r=['lg', 'sm'], w=['lg'])
                        t.op('dve', lambda e: e.tensor_tensor(out=lg[:], in0=lg[:], in1=msk[:], op=ALU.mult), r=['lg', 'msk'], w=['lg'])
                        t.op('dve', lambda e: e.reduce_sum(out=sm[:, 1:2], in_=lg[:], axis=AX.X), r=['lg'], w=['sm'])
                        t.op('dve', lambda e: e.reciprocal(out=sm[:, 1:2], in_=sm[:, 1:2]), r=['sm'], w=['sm'])
                        t.op('dve', lambda e: e.tensor_scalar(out=wt[:, lt, :], in0=lg[:], scalar1=sm[:, 1:2], scalar2=None, op0=ALU.mult),
                             r=['lg', 'sm'], w=['wt'])
                    t.barrier()
                    if STOP == 'outproj':
                        return nc

            with ExitStack() as _es:
                yacc = _es.enter_context(SBT("yacc", [128, 8, D], F32))
                actT = _es.enter_context(SBT("actT", [128, 16, 1024], BF16))
                wm0 = _es.enter_context(SBT("wm0", [128, 16, 256], BF16))
                wm1 = _es.enter_context(SBT("wm1", [128, 16, 256], BF16))
                wm2 = _es.enter_context(SBT("wm2", [128, 16, 256], BF16))
                wm3 = _es.enter_context(SBT("wm3", [128, 16, 256], BF16))
                bdb0 = _es.enter_context(SBT("bdb0", [128, 256], F32))
                bdb1 = _es.enter_context(SBT("bdb1", [128, 256], F32))
                g1 = _es.enter_context(SBT("g1", [128, 512], F32))
                sg = _es.enter_context(SBT("sg", [128, 512], F32))
                u1 = _es.enter_context(SBT("u1", [128, 512], F32))
                wm = [wm0, wm1, wm2, wm3]
                bdb = [bdb0, bdb1]
                wmi = 0
                t.op('dve', lambda e: e.memset(yacc[:], 0.0), w=['yacc'])
                for ex in range(NEXP):
                    for qf in range(8):
                        bg_, bu_ = wmi % 4, (wmi + 1) % 4
                        wmi += 2
                        t.dma('pool', lambda e: e.dma_start(out=wm[bg_][:], in_=wg_d[ex, qf]), w=[f'wm{bg_}'])
                        t.dma('pool', lambda e: e.dma_start(out=wm[bu_][:], in_=wu_d[ex, qf]), w=[f'wm{bu_}'])
                        for ft in range(2):
                            f = qf * 2 + ft
                            bgc = pv[:, PV_BG + ex * 16 + f:PV_BG + ex * 16 + f + 1]
                            buc = pv[:, PV_BU + ex * 16 + f:PV_BU + ex * 16 + f + 1]
                            for ch in range(2):
                                cs = slice(ch * 512, (ch + 1) * 512)
                                pg, pu = ps[2 * ch], ps[2 * ch + 1]
                                kg, ku = P(2 * ch), P(2 * ch + 1)
                                for kc in range(16):
                                    t.op('pe', lambda e: e.matmul(pg[:], lhsT=wm[bg_][:, kc, ft * 128:(ft + 1) * 128], rhs=h2T[:, kc, cs],
                                                                  start=(kc == 0), stop=(kc == 15)), r=[f'wm{bg_}', 'h2T'], w=[kg])
                                for kc in range(16):
                                    t.op('pe', lambda e: e.matmul(pu[:], lhsT=wm[bu_][:, kc, ft * 128:(ft + 1) * 128], rhs=h2T[:, kc, cs],
                                                                  start=(kc == 0), stop=(kc == 15)), r=[f'wm{bu_}', 'h2T'], w=[ku])
                                t.op('dve', lambda e: e.tensor_scalar(out=g1[:], in0=pg[:], scalar1=bgc, scalar2=7.0, op0=ALU.add, op1=ALU.min),
                                     r=[kg, 'pv'], w=['g1'])
                                t.op('act', lambda e: e.activation(out=sg[:], in_=g1[:], func=AF.Sigmoid, scale=1.702), r=['g1'], w=['sg'])
                                t.op('dve', lambda e: e.tensor_scalar(out=u1[:], in0=pu[:], scalar1=buc, scalar2=7.0, op0=ALU.add, op1=ALU.min),
                                     r=[ku, 'pv'], w=['u1'])
                                t.op('dve', lambda e: e.tensor_scalar(out=u1[:], in0=u1[:], scalar1=-7.0, scalar2=1.0, op0=ALU.max, op1=ALU.add),
                                     r=['u1'], w=['u1'])
                                t.op('pool', lambda e: e.tensor_tensor(out=g1[:], in0=g1[:], in1=sg[:], op=ALU.mult), r=['g1', 'sg'], w=['g1'])
                                t.op('dve', lambda e: e.tensor_tensor(out=actT[:, f, cs], in0=g1[:], in1=u1[:], op=ALU.mult),
                                     r=['g1', 'u1'], w=['actT'])
                    for dq in range(8):
                        bd_ = wmi % 4
                        wmi += 1
                        bb = dq % 2
                        ds_ = slice(dq * 256, (dq + 1) * 256)
                        t.dma('pool', lambda e: e.dma_start(out=wm[bd_][:], in_=wd_d[ex, dq]), w=[f'wm{bd_}'])
                        t.dma('sp', lambda e: e.dma_start(out=bdb[bb][:], in_=bd_d[ex:ex + 1, ds_].partition_broadcast(128)),
                              w=[f'bdb{bb}'])
                        for ti in range(8):
                            pd, kd = ps[4 + ti % 2], P(4 + ti % 2)
                            for fc in range(16):
                                t.op('pe', lambda e: e.matmul(pd[:, 0:256], lhsT=actT[:, fc, ti * 128:(ti + 1) * 128], rhs=wm[bd_][:, fc, :],
                                                              start=(fc == 0), stop=(fc == 15)), r=['actT', f'wm{bd_}'], w=[kd])
                            t.op('dve', lambda e: e.tensor_tensor(out=sg[:, 0:256], in0=pd[:, 0:256], in1=bdb[bb][:], op=ALU.add),
                                 r=[kd, f'bdb{bb}'], w=['sg'])
                            t.op('dve', lambda e: e.scalar_tensor_tensor(out=yacc[:, ti, ds_], in0=sg[:, 0:256], scalar=wt[:, ti, ex:ex + 1],
                                                                         in1=yacc[:, ti, ds_], op0=ALU.mult, op1=ALU.add),
                                 r=['sg', 'wt', 'yacc'], w=['yacc'])
                for ti in range(8):
                    gt = hf * 8 + ti
                    for q in range(4):
                        qs = slice(q * 512, (q + 1) * 512)
                        t.dma('sp', lambda e: e.dma_start(out=g1[:], in_=x1_d[gt * 128:(gt + 1) * 128, qs]), r=['x1d'], w=['g1'])
                        t.op('dve', lambda e: e.tensor_tensor(out=u1[:], in0=yacc[:, ti, qs], in1=G2[:, qs], op=ALU.mult),
                             r=['yacc', 'G2'], w=['u1'])
                        t.op('dve', lambda e: e.tensor_tensor(out=g1[:], in0=g1[:], in1=u1[:], op=ALU.add), r=['g1', 'u1'], w=['g1'])
                        t.dma('sp', lambda e: e.dma_start(out=out_d[gt * 128:(gt + 1) * 128, qs], in_=g1[:]), r=['g1'], w=['outd'])
                t.barrier()
    return nc


def _t5_bucket(dist):
    max_exact = 16
    d = np.maximum(dist, 0)
    lr = np.log(np.maximum(d, max_exact).astype(np.float32) / max_exact)
    large = max_exact + (lr / math.log(128 / max_exact) * (32 - max_exact)).astype(np.int32)
    large = np.minimum(large, 31)
    return np.where(d < max_exact, d, large)


def _prep(inputs):
    f = np.float32
    x = np.asarray(inputs["x"], f)[0]
    g = lambda k: np.asarray(inputs[k], f)[0]
    pp = lambda v: np.ascontiguousarray(v.reshape(-1, 128).T)
    perm = []
    for jj in range(8):
        perm += list(range(jj * 64, jj * 64 + 64)) + list(range((8 + jj) * 64, (8 + jj) * 64 + 64))
    perm += list(range(1024, 3328))
    perm = np.array(perm)
    win = np.ascontiguousarray(g("w_in")[:, perm])
    b_in = g("b_in")[perm]
    pv = np.zeros((128, NPV), f)
    pv[:, PV_C:PV_C + 16] = pp(np.asarray(inputs["c"], f)[0])
    pv[:, PV_G1:PV_G1 + 16] = pp(g("g_norm1"))
    pv[:, PV_BIN:PV_BIN + 26] = pp(b_in)
    pv[:, PV_GQ] = np.tile(g("g_q"), 2)
    pv[:, PV_GK] = np.tile(g("g_k"), 2)
    pv[:, PV_WDW:PV_WDW + 248] = g("w_dw").reshape(31, 8, 128).transpose(2, 1, 0).reshape(128, 248)
    pv[:, PV_BDW:PV_BDW + 8] = pp(g("b_dw"))
    pv[:, PV_LNG:PV_LNG + 8] = pp(g("ln_g"))
    pv[:, PV_LNB:PV_LNB + 8] = pp(g("ln_b"))
    pv[:, PV_GOC:PV_GOC + 8] = pp(g("g_out_conv"))
    pv[:, PV_BG:PV_BG + 512] = g("b_gate").reshape(32, 16, 128).transpose(2, 0, 1).reshape(128, 512)
    pv[:, PV_BU:PV_BU + 512] = g("b_up").reshape(32, 16, 128).transpose(2, 0, 1).reshape(128, 512)
    pv[:, PV_G2:PV_G2 + 16] = pp(g("g_norm2"))
    pv[:, PV_WR:PV_WR + 512] = g("w_router").reshape(16, 128, 32).transpose(1, 0, 2).reshape(128, 512)
    rv = np.zeros((1, NRV), f)
    rv[0, RV_GOA:RV_GOA + 1024] = g("g_out_attn")
    rv[0, RV_BOUT:RV_BOUT + D] = g("b_out")
    rv[0, RV_G2:RV_G2 + D] = g("g_norm2")
    rv[0, RV_BR:RV_BR + 32] = g("b_router")
    rv[0, RV_SINK:RV_SINK + 16] = g("sinks")
    rv = np.ascontiguousarray(np.broadcast_to(rv, (128, NRV)))
    rb = np.asarray(inputs["rel_bias"], f)
    kk = np.arange(128)[:, None]
    qq = np.arange(128)[None, :]
    bt = np.zeros((128, 2, 2, 8, 128), f)
    for kt in range(2):
        dist = qq + 128 - kk if kt == 0 else qq - kk
        valid = (dist >= 0) & (dist < 128)
        bk = _t5_bucket(dist)
        for gg in range(2):
            for jj in range(8):
                bt[:, kt, gg, jj, :] = np.where(valid, rb[bk, 8 * gg + jj], f(-30000.0))
    bt = bt.reshape(128, 4096)
    blk = np.zeros((128, 128), f)
    blk[:64, :64] = 1
    blk[64:, 64:] = 1
    def tile_w(w):
        return np.ascontiguousarray(w.reshape(E, 16, 128, 8, 256).transpose(0, 3, 2, 1, 4))

    common = dict(pv=pv, rv=rv, bada=g("b_ada")[None, :], wada=g("w_ada"), win=win, wout=g("w_out"), biast=bt,
                  wg=tile_w(g("w_gate")), wu=tile_w(g("w_up")), wd=tile_w(g("w_down")), bd=g("b_down"),
                  identf=np.eye(128, dtype=f), blk=blk)
    in_maps = []
    for c in range(NCORE):
        xh = np.zeros((17 * 128, D), f)
        if c == 0:
            xh[128:] = x[0:TOK]
        else:
            xh[:] = x[c * TOK - 128:(c + 1) * TOK]
        hvv = np.full((128, 1), 0.0 if c == 0 else 1.0, f)
        m = dict(common)
        m["xh"] = xh
        m["hv"] = hvv
        in_maps.append(m)
    return in_maps


def kernel(**inputs):
    in_maps = _prep(inputs)
    nc = build_nc()
    res = run_bass_kernel_spmd(nc, in_maps, core_ids=list(range(NCORE)))
    out = np.concatenate([np.asarray(r["out"], np.float32) for r in res.results], axis=0)
    return out.reshape(1, NCORE * TOK, D)
```

```python
import math
from contextlib import ExitStack
import numpy as np
import concourse.bass as bass
import concourse.mybir as mybir
from concourse.bass_utils import run_bass_kernel_spmd

F32 = mybir.dt.float32
BF16 = mybir.dt.bfloat16
ALU = mybir.AluOpType
AF = mybir.ActivationFunctionType
AX = mybir.AxisListType

D = 2048
NCORE = 8
TOK = 2048
NT = 16
E = 32
EPS = 1e-6
PV_C, PV_G1, PV_BIN, PV_GQ, PV_GK = 0, 16, 32, 58, 59
PV_WDW, PV_BDW, PV_LNG, PV_LNB, PV_GOC = 60, 308, 316, 324, 332
PV_BG, PV_BU, PV_WR = 340, 852, 1364
PV_G2 = 1364 + 512
NPV = 1364 + 512 + 16
RV_GOA, RV_BOUT, RV_G2, RV_BR, RV_SINK = 0, 1024, 3072, 5120, 5152
NRV = 5168


class Trk:
    def __init__(s, nc):
        s.nc = nc
        s.eng = dict(pe=nc.tensor, act=nc.scalar, dve=nc.vector, pool=nc.gpsimd, sp=nc.sync)
        s.csem = {e: nc.alloc_semaphore("c_" + e) for e in s.eng}
        s.ND = 24
        s.dsem = [nc.alloc_semaphore(f"dq{i}") for i in range(s.ND)]
        s.reset_state()

    def reset_state(s):
        s.cnt = {e: 0 for e in s.eng}
        s.seen = {c: {p: 0 for p in s.eng} for c in s.eng}
        s.dval = [0] * s.ND
        s.dseen = {c: [0] * s.ND for c in s.eng}
        s.drr = 0
        s.lastw = {}
        s.readers = {}

    def _wait(s, c, tok):
        if tok[0] == 'e':
            _, p, seq = tok
            if p == c and p == 'pe':
                return
            if s.seen[c][p] >= seq:
                return
            s.eng[c].wait_ge(s.csem[p], seq)
            s.seen[c][p] = seq
        else:
            _, k, val = tok
            if s.dseen[c][k] >= val:
                return
            s.eng[c].wait_ge(s.dsem[k], val)
            s.dseen[c][k] = val

    def _deps(s, c, r, w):
        for b in r:
            t = s.lastw.get(b)
            if t:
                s._wait(c, t)
        for b in w:
            t = s.lastw.get(b)
            if t:
                s._wait(c, t)
            rd = s.readers.get(b)
            if rd:
                for p, seq in rd[0].items():
                    if p != c:
                        s._wait(c, ('e', p, seq))
                for t in rd[1]:
                    s._wait(c, t)

    def _record(s, tok, r, w):
        for b in r:
            rd = s.readers.setdefault(b, [{}, []])
            if tok[0] == 'e':
                rd[0][tok[1]] = tok[2]
            else:
                rd[1].append(tok)
        for b in w:
            s.lastw[b] = tok
            s.readers[b] = [{}, []]

    def op(s, c, fn, r=(), w=()):
        s._deps(c, r, w)
        ins = fn(s.eng[c])
        s.cnt[c] += 1
        ins.then_inc(s.csem[c], 1)
        s._record(('e', c, s.cnt[c]), r, w)

    def dma(s, c, fn, r=(), w=()):
        s._deps(c, r, w)
        k = s.drr
        s.drr = (s.drr + 1) % s.ND
        if s.dval[k] > 0:
            s._wait(c, ('d', k, s.dval[k]))
        ins = fn(s.eng[c])
        s.dval[k] += 16
        ins.then_inc(s.dsem[k], 16)
        s._record(('d', k, s.dval[k]), r, w)

    def barrier(s):
        for c in s.eng:
            for p in s.eng:
                if s.cnt[p] > 0:
                    s._wait(c, ('e', p, s.cnt[p]))
            for k in range(s.ND):
                if s.dval[k] > 0:
                    s._wait(c, ('d', k, s.dval[k]))
        s.lastw = {}
        s.readers = {}


def build_nc(dbg=None):
    dbg = dbg or {}
    STOP = dbg.get('stop')
    HALVES = dbg.get('halves', [0, 1])
    GROUPS = dbg.get('groups', [0, 1])
    NEXP = dbg.get('n_exp', E)
    TAPS = dbg.get('taps', False)
    nc = bass.Bass("TRN2", target_bir_lowering=False)

    def din(name, shape, dt=F32):
        return nc.dram_tensor(name, list(shape), dt, kind="ExternalInput").ap()

    xh_d = din("xh", [17 * 128, D])
    hv_d = din("hv", [128, 1])
    pv_d = din("pv", [128, NPV])
    rv_d = din("rv", [128, NRV])
    bada_d = din("bada", [1, 6 * D])
    wada_d = din("wada", [D, 6 * D])
    win_d = din("win", [D, 3328])
    wout_d = din("wout", [D, D])
    bias_d = din("biast", [128, 4096])
    wg_d = din("wg", [NEXP, 8, 128, 16, 256])
    wu_d = din("wu", [NEXP, 8, 128, 16, 256])
    wd_d = din("wd", [NEXP, 8, 128, 16, 256])
    bd_d = din("bd", [NEXP, D])
    identf_d = din("identf", [128, 128])
    blk_d = din("blk", [128, 128])
    out_d = nc.dram_tensor("out", [TOK, D], F32, kind="ExternalOutput").ap()
    x1_d = nc.dram_tensor("x1s", [TOK, D], F32, kind=("ExternalOutput" if TAPS else "Internal")).ap()
    g1s_d = nc.dram_tensor("g1s", [2, 128, D], F32, kind="Internal").ap()

    t = Trk(nc)

    def tap(name, tens, shape, dt, key):
        if not TAPS:
            return
        dd = nc.dram_tensor("tap_" + name, list(shape), dt, kind="ExternalOutput").ap()
        t.dma('sp', lambda e: e.dma_start(out=dd, in_=tens[:]), r=[key], w=['tap_' + name])
    _uid = [0]

    def SB(name, shape, dt):
        _uid[0] += 1
        return nc.alloc_sbuf_tensor(f"{name}_s{_uid[0]}", shape, dt)

    def SBT(name, shape, dt):
        _uid[0] += 1
        return nc.sbuf_tensor(f"{name}_s{_uid[0]}", shape, dt)

    pv = SB("pv", [128, NPV], F32)
    hv = SB("hv", [128, 1], F32)
    identf = SB("identf", [128, 128], F32)
    identb = SB("identb", [128, 128], BF16)
    blkb = SB("blkb", [128, 128], BF16)
    onesf = SB("onesf", [128, 128], F32)
    sT = SB("sT", [128, 16], BF16)
    modT = SB("modT", [128, 4, 16], F32)
    A2 = SB("A2", [128, 16], F32)
    A1 = SB("A1", [128, 16], F32)
    G2 = SB("G2", [128, D], F32)
    goa = SB("goa", [128, 1024], F32)
    brt = SB("brt", [128, 32], F32)
    esink = SB("esink", [128, 16], F32)
    ebt = SB("ebt", [128, 4096], BF16)
    ebt0 = SB("ebt0", [128, 2048], BF16)
    wrb = SB("wrb", [128, 512], BF16)
    ssq = SB("ssq", [128, 1], F32)
    rstd = SB("rstd", [128, 1], F32)

    ps = [nc.alloc_psum_tensor(f"ps{i}", [128, 512], F32) for i in range(6)]
    psT = [nc.alloc_psum_tensor(f"psT{i}", [128, 1024], BF16) for i in range(2)]

    def P(i):
        return f"ps{i}"

    t.dma('sp', lambda e: e.dma_start(out=pv[:], in_=pv_d), w=['pv'])
    t.dma('sp', lambda e: e.dma_start(out=hv[:], in_=hv_d), w=['hv'])
    t.dma('sp', lambda e: e.dma_start(out=identf[:], in_=identf_d), w=['identf'])
    t.dma('pool', lambda e: e.dma_start(out=identb[:], in_=identf_d), w=['identb'])
    t.dma('pool', lambda e: e.dma_start(out=blkb[:], in_=blk_d), w=['blkb'])
    t.op('dve', lambda e: e.memset(onesf[:], 1.0), w=['onesf'])
    t.op('dve', lambda e: e.tensor_copy(out=wrb[:], in_=pv[:, PV_WR:PV_WR + 512]), r=['pv'], w=['wrb'])
    t.op('act', lambda e: e.activation(out=sT[:], in_=pv[:, PV_C:PV_C + 16], func=AF.Silu), r=['pv'], w=['sT'])

    with ExitStack() as _es:
        rvt = _es.enter_context(SBT("rvt", [128, NRV], F32))
        biasf = _es.enter_context(SBT("biasf", [128, 4096], F32))
        wr0 = _es.enter_context(SBT("wr0", [128, 16, 512], BF16))
        wr1 = _es.enter_context(SBT("wr1", [128, 16, 512], BF16))
        brow0 = _es.enter_context(SBT("brow0", [1, 512], F32))
        brow1 = _es.enter_context(SBT("brow1", [1, 512], F32))
        mrow = _es.enter_context(SBT("mrow", [1, 512], F32))
        bcg = _es.enter_context(SBT("bcg", [128, D], F32))
        gb1 = _es.enter_context(SBT("gb1", [128, D], F32))
        wr = [wr0, wr1]
        brow = [brow0, brow1]
        t.dma('sp', lambda e: e.dma_start(out=rvt[:], in_=rv_d), w=['rvt'])
        t.dma('sp', lambda e: e.dma_start(out=biasf[:], in_=bias_d), w=['biasf'])
        t.op('act', lambda e: e.activation(out=ebt[:], in_=biasf[:], func=AF.Exp), r=['biasf'], w=['ebt'])
        t.op('dve', lambda e: e.tensor_scalar(out=ebt0[:], in0=ebt[:, 0:2048], scalar1=hv[:, 0:1], scalar2=None,
                                              op0=ALU.mult), r=['ebt', 'hv'], w=['ebt0'])
        t.op('act', lambda e: e.activation(out=esink[:], in_=rvt[:, RV_SINK:RV_SINK + 16], func=AF.Exp),
             r=['rvt'], w=['esink'])
        t.op('dve', lambda e: e.tensor_copy(out=goa[:], in_=rvt[:, RV_GOA:RV_GOA + 1024]), r=['rvt'], w=['goa'])
        t.op('dve', lambda e: e.tensor_copy(out=brt[:], in_=rvt[:, RV_BR:RV_BR + 32]), r=['rvt'], w=['brt'])

        wada_v = wada_d.rearrange("(kc p) f -> p kc f", p=128)
        for n in range(24):
            b = n % 2
            t.dma('pool', lambda e: e.dma_start(out=wr[b][:], in_=wada_v[:, :, n * 512:(n + 1) * 512]), w=[f'wr{b}'])
            t.dma('sp', lambda e: e.dma_start(out=brow[b][:], in_=bada_d[0:1, n * 512:(n + 1) * 512]), w=[f'brow{b}'])
            for kc in range(16):
                t.op('pe', lambda e: e.matmul(ps[0][0:1, :], lhsT=sT[:, kc:kc + 1], rhs=wr[b][:, kc, :],
                                              start=(kc == 0), stop=(kc == 15)), r=['sT', f'wr{b}'], w=[P(0)])
            t.op('dve', lambda e: e.tensor_tensor(out=mrow[:], in0=ps[0][0:1, :], in1=brow[b][:], op=ALU.add),
                 r=[P(0), f'brow{b}'], w=['mrow'])
            which, q = n // 4, n % 4
            if which in (0, 1, 3, 4):
                mi = {0: 0, 1: 1, 3: 2, 4: 3}[which]
                for j in range(4):
                    t.op('pe', lambda e: e.matmul(ps[1][:, j:j + 1], lhsT=mrow[0:1, j * 128:(j + 1) * 128],
                                                  rhs=onesf[0:1, 0:1], start=True, stop=True), r=['mrow', 'onesf'], w=[P(1)])
                t.op('dve', lambda e: e.tensor_copy(out=modT[:, mi, q * 4:(q + 1) * 4], in_=ps[1][:, 0:4]),
                     r=[P(1)], w=['modT'])
            else:
                dstt, dk = (bcg, 'bcg') if which == 2 else (G2, 'G2')
                t.op('pe', lambda e: e.matmul(ps[2][:, :], lhsT=onesf[0:1, 0:128], rhs=mrow[0:1, :],
                                              start=True, stop=True), r=['mrow', 'onesf'], w=[P(2)])
                t.op('act', lambda e: e.activation(out=dstt[:, q * 512:(q + 1) * 512], in_=ps[2][:, :], func=AF.Identity),
                     r=[P(2)], w=[dk])
        t.op('dve', lambda e: e.scalar_tensor_tensor(out=A1[:], in0=modT[:, 1, :], scalar=1.0, in1=pv[:, PV_G1:PV_G1 + 16],
                                                     op0=ALU.add, op1=ALU.mult), r=['modT', 'pv'], w=['A1'])
        t.op('dve', lambda e: e.scalar_tensor_tensor(out=A2[:], in0=modT[:, 3, :], scalar=1.0, in1=pv[:, PV_G2:PV_G2 + 16],
                                                     op0=ALU.add, op1=ALU.mult), r=['modT', 'pv'], w=['A2'])
        t.op('dve', lambda e: e.tensor_tensor(out=gb1[:], in0=bcg[:], in1=rvt[:, RV_BOUT:RV_BOUT + D], op=ALU.mult),
             r=['bcg', 'rvt'], w=['gb1'])
        t.dma('sp', lambda e: e.dma_start(out=g1s_d[0], in_=bcg[:]), r=['bcg'], w=['g1sd'])
        t.dma('sp', lambda e: e.dma_start(out=g1s_d[1], in_=gb1[:]), r=['gb1'], w=['g1sd'])
        tap('modT', modT, [128, 4, 16], F32, 'modT')
        tap('A1', A1, [128, 16], F32, 'A1')
        tap('bc0', bcg, [128, D], F32, 'bcg')
        tap('ebt', ebt, [128, 4096], BF16, 'ebt')
        t.barrier()
        if STOP == 'ada':
            return nc

    win_v = win_d.rearrange("(kc p) f -> p kc f", p=128)
    wout_v = wout_d.rearrange("(kc p) f -> p kc f", p=128)

    def rsqrt_into(dst, src_ap, scale, r, w):
        t.op('act', lambda e: e.activation(out=dst, in_=src_ap, func=AF.Sqrt, bias=EPS, scale=scale), r=r, w=w)
        t.op('dve', lambda e: e.reciprocal(out=dst, in_=dst), r=w, w=w)

    for hf in HALVES:
        with ExitStack() as _esh:
            h2T = _esh.enter_context(SBT("h2T", [128, 16, 1024], BF16))
            wt = _esh.enter_context(SBT("wt", [128, 8, 32], F32))
            with ExitStack() as _esm:
                mixedT = _esm.enter_context(SBT("mixedT", [128, 16, 1024], BF16))
                with ExitStack() as _es:
                    qT = _es.enter_context(SBT("qT", [128, 8, 1152], BF16))
                    kT = _es.enter_context(SBT("kT", [128, 1152], BF16))
                    v1 = _es.enter_context(SBT("v1", [128, 9, 2, 65], BF16))
                    hglu = _es.enter_context(SBT("hglu", [128, 8, 1152], BF16))
                    t.op('pool', lambda e: e.memset(v1[:], 1.0), w=['v1'])
                    with ExitStack() as _es:
                        hT = _es.enter_context(SBT("hT", [128, 16, 1152], BF16))
                        xb0 = _es.enter_context(SBT("xb0", [128, D], F32))
                        xs = _es.enter_context(SBT("xs", [128, D], BF16))
                        wi0 = _es.enter_context(SBT("wi0", [128, 16, 256], BF16))
                        wi1 = _es.enter_context(SBT("wi1", [128, 16, 256], BF16))
                        tA = xb0[:, 0:512]
                        tB = xs[:, 0:512]
                        tC = xb0[:, 512:1024]
                        xb = [xb0, xb0]; junk = xs
                        wi = [wi0, wi1]
                        for tt in range(9):
                            xt = xb[tt % 2]
                            xk = 'xb0'
                            r0 = (hf * 8 + tt) * 128
                            t.dma('sp', lambda e: e.dma_start(out=xt[:], in_=xh_d[r0:r0 + 128, :]), w=[xk])
                            t.op('dve', lambda e: e.memset(ssq[:], 0.0), w=['ssq'])
                            t.op('act', lambda e: e.activation(out=junk[:], in_=xt[:], func=AF.Square, accum_out=ssq[:, 0:1]),
                                 r=[xk, 'ssq'], w=['xs', 'ssq'])
                            rsqrt_into(rstd[:], ssq[:], 1.0 / D, ['ssq'], ['rstd'])
                            t.op('dve', lambda e: e.tensor_scalar(out=xs[:], in0=xt[:], scalar1=rstd[:, 0:1], scalar2=None, op0=ALU.mult),
                                 r=[xk, 'rstd'], w=['xs'])
                            for c in range(16):
                                t.op('pe', lambda e: e.transpose(out=psT[c // 8][:, (c % 8) * 128:(c % 8 + 1) * 128],
                                                                 in_=xs[:, c * 128:(c + 1) * 128], identity=identb[:]),
                                     r=['xs', 'identb'], w=[f'psT{c // 8}'])
                            for c in range(16):
                                t.op('dve', lambda e: e.tensor_scalar(out=hT[:, c, tt * 128:(tt + 1) * 128],
                                                                      in0=psT[c // 8][:, (c % 8) * 128:(c % 8 + 1) * 128],
                                                                      scalar1=A1[:, c:c + 1], scalar2=modT[:, 0, c:c + 1],
                                                                      op0=ALU.mult, op1=ALU.add),
                                     r=[f'psT{c // 8}', 'A1', 'modT'], w=['hT'])
                        if STOP == 'norm':
                            tap(f'hT{hf}', hT, [128, 16, 1152], BF16, 'hT')
                            t.barrier()
                            return nc
                        t.barrier()
                        chunks = [(0, 128), (128, 512), (640, 512)]
                        for j in dbg.get('jlist', range(26)):
                            wc = j // 2
                            b = wc % 2
                            if j % 2 == 0 or 'jlist' in dbg:
                                t.dma('pool', lambda e: e.dma_start(out=wi[b][:], in_=win_v[:, :, wc * 256:wc * 256 + 256]),
                                      w=[f'wi{b}'])
                            jo = (j % 2) * 128
                            bias = pv[:, PV_BIN + j:PV_BIN + j + 1]
                            for ci, (c0, cn) in enumerate(chunks):
                                pb = ps[ci % 2]
                                for kc in range(0 if dbg.get('nomm') else 16):
                                    t.op('pe', lambda e: e.matmul(pb[:, 0:cn], lhsT=wi[b][:, kc, jo:jo + 128], rhs=hT[:, kc, c0:c0 + cn],
                                                                  start=(kc == 0), stop=(kc == 15)), r=[f'wi{b}', 'hT'], w=[P(ci % 2)])
                                if dbg.get('noevac'):
                                    continue
                                if j < 9:
                                    gcol = PV_GQ if j < 8 else PV_GK
                                    dst = qT[:, j, c0:c0 + cn] if j < 8 else kT[:, c0:c0 + cn]
                                    dk = 'qT' if j < 8 else 'kT'
                                    t.op('act', lambda e: e.activation(out=tB[:, 0:cn], in_=pb[:, 0:cn], func=AF.Square, bias=bias),
                                         r=[P(ci % 2), 'pv'], w=['tB'])
                                    t.op('act', lambda e: e.activation(out=tA[:, 0:cn], in_=pb[:, 0:cn], func=AF.Identity, bias=bias),
                                         r=[P(ci % 2), 'pv'], w=['tA'])
                                    t.op('pe', lambda e: e.matmul(ps[2][:, 0:cn], lhsT=blkb[:], rhs=tB[:, 0:cn], start=True, stop=True),
                                         r=['blkb', 'tB'], w=[P(2)])
                                    rsqrt_into(tC[:, 0:cn], ps[2][:, 0:cn], 1.0 / 64, [P(2)], ['tC'])
                                    t.op('dve', lambda e: e.scalar_tensor_tensor(out=dst, in0=tA[:, 0:cn], scalar=pv[:, gcol:gcol + 1],
                                                                                 in1=tC[:, 0:cn], op0=ALU.mult, op1=ALU.mult),
                                         r=['tA', 'tC', 'pv'], w=[dk])
                                elif j == 9:
                                    t.op('act', lambda e: e.activation(out=tB[:, 0:cn], in_=pb[:, 0:cn], func=AF.Identity, bias=bias),
                                         r=[P(ci % 2), 'pv'], w=['tB'])
                                    for s_ in range(cn // 128):
                                        tt = c0 // 128 + s_
                                        t.op('pe', lambda e: e.transpose(out=psT[0][:, 0:128], in_=tB[:, s_ * 128:(s_ + 1) * 128],
                                                                         identity=identb[:]), r=['tB', 'identb'], w=['psT0'])
                                        t.op('dve', lambda e: e.tensor_copy(out=v1[:, tt, :, 0:64],
                                                                            in_=psT[0][:, 0:128].rearrange("p (g d) -> p g d", g=2)),
                                             r=['psT0'], w=['v1'])
                                elif j < 18:
                                    c = j - 10
                                    t.op('dve', lambda e: e.tensor_scalar(out=hglu[:, c, c0:c0 + cn], in0=pb[:, 0:cn], scalar1=bias,
                                                                          scalar2=None, op0=ALU.add), r=[P(ci % 2), 'pv'], w=['hglu'])
                                else:
                                    c = j - 18
                                    t.op('act', lambda e: e.activation(out=tB[:, 0:cn], in_=pb[:, 0:cn], func=AF.Sigmoid, bias=bias),
                                         r=[P(ci % 2), 'pv'], w=['tB'])
                                    t.op('pool', lambda e: e.tensor_tensor(out=hglu[:, c, c0:c0 + cn], in0=hglu[:, c, c0:c0 + cn],
                                                                           in1=tB[:, 0:cn], op=ALU.mult), r=['tB', 'hglu'], w=['hglu'])
                        if hf == 0 and not dbg.get('nohv'):
                            for c in range(8):
                                t.op('pool', lambda e: e.tensor_scalar(out=hglu[:, c, 0:128], in0=hglu[:, c, 0:128], scalar1=hv[:, 0:1],
                                                                       scalar2=None, op0=ALU.mult), r=['hglu', 'hv'], w=['hglu'])
                        tap(f'hT{hf}', hT, [128, 16, 1152], BF16, 'hT')
                        tap(f'qT{hf}', qT, [128, 8, 1152], BF16, 'qT')
                        tap(f'kT{hf}', kT, [128, 1152], BF16, 'kT')
                        tap(f'v1{hf}', v1, [128, 9, 2, 65], BF16, 'v1')
                        tap(f'hglu{hf}', hglu, [128, 8, 1152], BF16, 'hglu')
                        t.barrier()
                        if STOP == 'inproj':
                            return nc

                    with ExitStack() as _es:
                        acc = _es.enter_context(SBT("acc", [128, 8, 1024], F32))
                        sq = _es.enter_context(SBT("sq", [128, 512], F32))
                        mean = _es.enter_context(SBT("mean", [128, 512], F32))
                        var = _es.enter_context(SBT("var", [128, 512], F32))
                        tmp = _es.enter_context(SBT("tmp", [128, 512], F32))
                        yatt = _es.enter_context(SBT("yatt", [128, 1024], F32))
                        ynb = _es.enter_context(SBT("ynb", [128, 1024], BF16))
                        pe0 = _es.enter_context(SBT("pe0", [128, 512], F32))
                        pe1 = _es.enter_context(SBT("pe1", [128, 512], F32))
                        pT0 = _es.enter_context(SBT("pT0", [128, 1024], BF16))
                        pT1 = _es.enter_context(SBT("pT1", [128, 1024], BF16))
                        den = _es.enter_context(SBT("den", [128, 8], F32))
                        junk2 = _es.enter_context(SBT("junk2", [128, 1024], BF16))
                        for c in range(8):
                            en = 'dve'
                            ak = f'acc{c}'
                            wc0 = PV_WDW + c * 31
                            t.op(en, lambda e: e.tensor_scalar(out=acc[:, c, :], in0=hglu[:, c, 98:98 + 1024], scalar1=pv[:, wc0:wc0 + 1],
                                                               scalar2=pv[:, PV_BDW + c:PV_BDW + c + 1], op0=ALU.mult, op1=ALU.add),
                                 r=['hglu', 'pv'], w=[ak])
                            for j in range(1, 31):
                                if en == 'dve':
                                    t.op(en, lambda e: e.scalar_tensor_tensor(out=acc[:, c, :], in0=hglu[:, c, 98 + j:98 + j + 1024],
                                                                              scalar=pv[:, wc0 + j:wc0 + j + 1], in1=acc[:, c, :],
                                                                              op0=ALU.mult, op1=ALU.add), r=['hglu', 'pv', ak], w=[ak])
                                else:
                                    t.op(en, lambda e: e.tensor_scalar(out=ctmp[:], in0=hglu[:, c, 98 + j:98 + j + 1024],
                                                                       scalar1=pv[:, wc0 + j:wc0 + j + 1], scalar2=None, op0=ALU.mult),
                                         r=['hglu', 'pv'], w=['ctmp'])
                                    t.op(en, lambda e: e.tensor_tensor(out=acc[:, c, :], in0=acc[:, c, :], in1=ctmp[:], op=ALU.add),
                                         r=['ctmp', ak], w=[ak])
                        for ch in range(2):
                            cs = slice(ch * 512, (ch + 1) * 512)
                            for c in range(8):
                                t.op('act', lambda e: e.activation(out=sq[:], in_=acc[:, c, cs], func=AF.Square), r=[f'acc{c}'], w=['sq'])
                                t.op('pe', lambda e: e.matmul(ps[0][:], lhsT=onesf[:], rhs=acc[:, c, cs], start=(c == 0), stop=(c == 7)),
                                     r=['onesf', f'acc{c}'], w=[P(0)])
                                t.op('pe', lambda e: e.matmul(ps[1][:], lhsT=onesf[:], rhs=sq[:], start=(c == 0), stop=(c == 7)),
                                     r=['onesf', 'sq'], w=[P(1)])
                            t.op('dve', lambda e: e.tensor_scalar(out=mean[:], in0=ps[0][:], scalar1=1.0 / 1024, scalar2=None, op0=ALU.mult),
                                 r=[P(0)], w=['mean'])
                            t.op('dve', lambda e: e.tensor_tensor(out=tmp[:], in0=mean[:], in1=mean[:], op=ALU.mult), r=['mean'], w=['tmp'])
                            t.op('dve', lambda e: e.scalar_tensor_tensor(out=var[:], in0=ps[1][:], scalar=1.0 / 1024, in1=tmp[:],
                                                                         op0=ALU.mult, op1=ALU.subtract), r=[P(1), 'tmp'], w=['var'])
                            rsqrt_into(var[:], var[:], 1.0, ['var'], ['var'])
                            for c in range(8):
                                ak = f'acc{c}'
                                t.op('dve', lambda e: e.tensor_tensor(out=tmp[:], in0=acc[:, c, cs], in1=mean[:], op=ALU.subtract),
                                     r=[ak, 'mean'], w=['tmp'])
                                t.op('dve', lambda e: e.tensor_tensor(out=tmp[:], in0=tmp[:], in1=var[:], op=ALU.mult), r=['tmp', 'var'], w=['tmp'])
                                t.op('act', lambda e: e.activation(out=acc[:, c, cs], in_=tmp[:], func=AF.Silu,
                                                                   bias=pv[:, PV_LNB + c:PV_LNB + c + 1], scale=pv[:, PV_LNG + c:PV_LNG + c + 1]),
                                     r=['tmp', 'pv'], w=[ak])
                                t.op('act', lambda e: e.activation(out=sq[:], in_=acc[:, c, cs], func=AF.Square), r=[ak], w=['sq'])
                                t.op('pe', lambda e: e.matmul(ps[2][:], lhsT=onesf[:], rhs=sq[:], start=(c == 0), stop=(c == 7)),
                                     r=['onesf', 'sq'], w=[P(2)])
                            rsqrt_into(var[:], ps[2][:], 1.0 / 1024, [P(2)], ['var'])
                            for c in range(8):
                                t.op('dve', lambda e: e.scalar_tensor_tensor(out=mixedT[:, 8 + c, cs], in0=acc[:, c, cs],
                                                                             scalar=pv[:, PV_GOC + c:PV_GOC + c + 1], in1=var[:],
                                                                             op0=ALU.mult, op1=ALU.mult), r=[f'acc{c}', 'var', 'pv'], w=['mixedT'])

                        pes = [pe0, pe1]
                        pTs = [pT0, pT1]
                        for n in range(8):
                            tt = n + 1
                            for g in range(2):
                                gp = slice(g * 64, (g + 1) * 64)
                                for kt in range(2):
                                    kc0 = (tt - 1 + kt) * 128
                                    for hh in range(2):
                                        pi = 2 + hh
                                        t.op('pe', lambda e: e.matmul(ps[pi][:], lhsT=kT[gp, kc0:kc0 + 128],
                                                                      rhs=qT[gp, 4 * hh:4 * hh + 4, tt * 128:(tt + 1) * 128],
                                                                      start=True, stop=True), r=['kT', 'qT'], w=[P(pi)])
                                        t.op('act', lambda e: e.activation(out=pes[hh][:], in_=ps[pi][:], func=AF.Exp, scale=0.125),
                                             r=[P(pi)], w=[f'pe{hh}'])
                                        if kt == 0 and n == 0 and hf == 0:
                                            eb = ebt0[:, g * 1024 + hh * 512:g * 1024 + (hh + 1) * 512]
                                        else:
                                            o = kt * 2048 + g * 1024 + hh * 512
                                            eb = ebt[:, o:o + 512]
                                        t.op('dve', lambda e: e.tensor_tensor(out=pTs[kt][:, hh * 512:(hh + 1) * 512], in0=pes[hh][:], in1=eb,
                                                                              op=ALU.mult), r=[f'pe{hh}', 'ebt', 'ebt0'], w=[f'pT{kt}'])
                                for jj in range(8):
                                    pi = 4 + jj // 4
                                    oc = (jj % 4) * 65
                                    for kt in range(2):
                                        t.op('pe', lambda e: e.matmul(ps[pi][:, oc:oc + 65], lhsT=pTs[kt][:, jj * 128:(jj + 1) * 128],
                                                                      rhs=v1[:, tt - 1 + kt, g, :], start=(kt == 0), stop=(kt == 1)),
                                             r=[f'pT{kt}', 'v1'], w=[P(pi)])
                                for half in range(2):
                                    pi = 4 + half
                                    pv3 = ps[pi][:, 0:260].rearrange("p (h d) -> p h d", d=65)
                                    hs = 8 * g + 4 * half
                                    t.op('dve', lambda e: e.tensor_tensor(out=den[:, 0:4], in0=pv3[:, :, 64], in1=esink[:, hs:hs + 4], op=ALU.add),
                                         r=[P(pi), 'esink'], w=['den'])
                                    t.op('dve', lambda e: e.reciprocal(out=den[:, 0:4], in_=den[:, 0:4]), r=['den'], w=['den'])
                                    for j4 in range(4):
                                        h = hs + j4
                                        t.op('dve', lambda e: e.tensor_scalar(out=yatt[:, h * 64:(h + 1) * 64], in0=pv3[:, j4, 0:64],
                                                                              scalar1=den[:, j4:j4 + 1], scalar2=None, op0=ALU.mult),
                                             r=[P(pi), 'den'], w=['yatt'])
                            t.op('dve', lambda e: e.memset(ssq[:], 0.0), w=['ssq'])
                            t.op('act', lambda e: e.activation(out=junk2[:], in_=yatt[:], func=AF.Square, accum_out=ssq[:, 0:1]),
                                 r=['yatt', 'ssq'], w=['junk2', 'ssq'])
                            rsqrt_into(rstd[:], ssq[:], 1.0 / 1024, ['ssq'], ['rstd'])
                            t.op('dve', lambda e: e.scalar_tensor_tensor(out=ynb[:], in0=yatt[:], scalar=rstd[:, 0:1], in1=goa[:],
                                                                         op0=ALU.mult, op1=ALU.mult), r=['yatt', 'rstd', 'goa'], w=['ynb'])
                            for c in range(8):
                                t.op('pe', lambda e: e.transpose(out=psT[1][:, c * 128:(c + 1) * 128], in_=ynb[:, c * 128:(c + 1) * 128],
                                                                 identity=identb[:]), r=['ynb', 'identb'], w=['psT1'])
                            t.op('dve', lambda e: e.tensor_copy(out=mixedT[:, 0:8, n * 128:(n + 1) * 128],
                                                                in_=psT[1][:, :].rearrange("p (c q) -> p c q", c=8)), r=['psT1'], w=['mixedT'])
                        tap(f'mixedT{hf}', mixedT, [128, 16, 1024], BF16, 'mixedT')
                        t.barrier()
                        if STOP == 'mixer':
                            return nc


                with ExitStack() as _es:
                    wo = _es.enter_context(SBT("wo", [128, 16, D], BF16))
                    G1 = _es.enter_context(SBT("G1", [128, D], F32))
                    GB1 = _es.enter_context(SBT("GB1", [128, D], F32))
                    xt2 = _es.enter_context(SBT("xt2", [128, D], F32))
                    x1 = _es.enter_context(SBT("x1", [128, D], F32))
                    h2b = _es.enter_context(SBT("h2b", [128, D], BF16))
                    lg = _es.enter_context(SBT("lg", [128, 32], F32))
                    top8 = _es.enter_context(SBT("top8", [128, 8], F32))
                    msk = _es.enter_context(SBT("msk", [128, 32], F32))
                    sm = _es.enter_context(SBT("sm", [128, 2], F32))
                    t.dma('sp', lambda e: e.dma_start(out=G1[:], in_=g1s_d[0]), r=['g1sd'], w=['G1'])
                    t.dma('sp', lambda e: e.dma_start(out=GB1[:], in_=g1s_d[1]), r=['g1sd'], w=['GB1'])
                    for q in range(4):
                        t.dma('pool', lambda e: e.dma_start(out=wo[:, :, q * 512:(q + 1) * 512], in_=wout_v[:, :, q * 512:(q + 1) * 512]),
                              w=['wo'])
                    for lt in range(8):
                        gt = hf * 8 + lt
                        t.dma('sp', lambda e: e.dma_start(out=xt2[:], in_=xh_d[(gt + 1) * 128:(gt + 2) * 128, :]), w=['xt2'])
                        t.op('pool', lambda e: e.tensor_tensor(out=xt2[:], in0=xt2[:], in1=GB1[:], op=ALU.add), r=['xt2', 'GB1'], w=['xt2'])
                        for nq in range(4):
                            for cc in range(16):
                                t.op('pe', lambda e: e.matmul(ps[nq][:], lhsT=mixedT[:, cc, lt * 128:(lt + 1) * 128],
                                                              rhs=wo[:, cc, nq * 512:(nq + 1) * 512], start=(cc == 0), stop=(cc == 15)),
                                     r=['mixedT', 'wo'], w=[P(nq)])
                            ns = slice(nq * 512, (nq + 1) * 512)
                            t.op('dve', lambda e: e.tensor_tensor(out=x1[:, ns], in0=ps[nq][:], in1=G1[:, ns], op=ALU.mult),
                                 r=[P(nq), 'G1'], w=['x1'])
                        t.op('dve', lambda e: e.tensor_tensor(out=x1[:], in0=x1[:], in1=xt2[:], op=ALU.add), r=['x1', 'xt2'], w=['x1'])
                        t.dma('sp', lambda e: e.dma_start(out=x1_d[gt * 128:(gt + 1) * 128, :], in_=x1[:]), r=['x1'], w=['x1d'])
                        t.op('dve', lambda e: e.memset(ssq[:], 0.0), w=['ssq'])
                        t.op('act', lambda e: e.activation(out=h2b[:], in_=x1[:], func=AF.Square, accum_out=ssq[:, 0:1]),
                             r=['x1', 'ssq'], w=['h2b', 'ssq'])
                        rsqrt_into(rstd[:], ssq[:], 1.0 / D, ['ssq'], ['rstd'])
                        t.op('dve', lambda e: e.tensor_scalar(out=h2b[:], in0=x1[:], scalar1=rstd[:, 0:1], scalar2=None, op0=ALU.mult),
                             r=['x1', 'rstd'], w=['h2b'])
                        for cc in range(16):
                            t.op('pe', lambda e: e.transpose(out=psT[cc // 8][:, (cc % 8) * 128:(cc % 8 + 1) * 128],
                                                             in_=h2b[:, cc * 128:(cc + 1) * 128], identity=identb[:]),
                                 r=['h2b', 'identb'], w=[f'psT{cc // 8}'])
                        for cc in range(16):
                            pin = psT[cc // 8][:, (cc % 8) * 128:(cc % 8 + 1) * 128]
                            if cc % 2 == 0:
                                t.op('dve', lambda e: e.tensor_scalar(out=h2T[:, cc, lt * 128:(lt + 1) * 128], in0=pin,
                                                                      scalar1=A2[:, cc:cc + 1], scalar2=modT[:, 2, cc:cc + 1],
                                                                      op0=ALU.mult, op1=ALU.add),
                                     r=[f'psT{cc // 8}', 'A2', 'modT'], w=['h2T'])
                            else:
                                t.op('act', lambda e: e.activation(out=h2T[:, cc, lt * 128:(lt + 1) * 128], in_=pin, func=AF.Identity,
                                                                   bias=modT[:, 2, cc:cc + 1], scale=A2[:, cc:cc + 1]),
                                     r=[f'psT{cc // 8}', 'A2', 'modT'], w=['h2T'])
                        for cc in range(16):
                            t.op('pe', lambda e: e.matmul(ps[5][:, 0:32], lhsT=h2T[:, cc, lt * 128:(lt + 1) * 128],
                                                          rhs=wrb[:, cc * 32:(cc + 1) * 32],
                                                          start=(cc == 0), stop=(cc == 15)), r=['h2T', 'wrb'], w=[P(5)])
                        t.op('dve', lambda e: e.tensor_tensor(out=lg[:], in0=ps[5][:, 0:32], in1=brt[:], op=ALU.add), r=[P(5), 'brt'], w=['lg'])
                        t.op('dve', lambda e: e.max(out=top8[:], in_=lg[:]), r=['lg'], w=['top8'])
                        t.op('dve', lambda e: e.tensor_scalar(out=msk[:], in0=lg[:], scalar1=top8[:, 3:4], scalar2=None, op0=ALU.is_ge),
                             r=['lg', 'top8'], w=['msk'])
                        t.op('dve', lambda e: e.tensor_scalar(out=sm[:, 0:1], in0=top8[:, 0:1], scalar1=-1.0, scalar2=None, op0=ALU.mult),
                             r=['top8'], w=['sm'])
                        t.op('act', lambda e: e.activation(out=lg[:], in_=lg[:], func=AF.Exp, bias=sm[:, 0:1]), r=['lg', 'sm'], w=['lg'])
                        t.op('dve', lambda e: e.tensor_tensor(out=lg[:], in0=lg[:], in1=msk[:], op=ALU.mult), r=['lg', 'msk'], w=['lg'])
                        t.op('dve', lambda e: e.reduce_sum(out=sm[:, 1:2], in_=lg[:], axis=AX.X), r=['lg'], w=['sm'])
                        t.op('dve', lambda e: e.reciprocal(out=sm[:, 1:2], in_=sm[:, 1:2]), r=['sm'], w=['sm'])
                        t.op('dve', lambda e: e.tensor_scalar(out=wt[:, lt, :], in0=lg[:], scalar1=sm[:, 1:2], scalar2=None, op0=ALU.mult),
                             r=['lg', 'sm'], w=['wt'])
                    t.barrier()
                    if STOP == 'outproj':
                        return nc

            with ExitStack() as _es:
                yacc = _es.enter_context(SBT("yacc", [128, 8, D], F32))
                actT = _es.enter_context(SBT("actT", [128, 16, 1024], BF16))
                wm0 = _es.enter_context(SBT("wm0", [128, 16, 256], BF16))
                wm1 = _es.enter_context(SBT("wm1", [128, 16, 256], BF16))
                wm2 = _es.enter_context(SBT("wm2", [128, 16, 256], BF16))
                wm3 = _es.enter_context(SBT("wm3", [128, 16, 256], BF16))
                bdb0 = _es.enter_context(SBT("bdb0", [128, 256], F32))
                bdb1 = _es.enter_context(SBT("bdb1", [128, 256], F32))
                g1 = _es.enter_context(SBT("g1", [128, 512], F32))
                sg = _es.enter_context(SBT("sg", [128, 512], F32))
                u1 = _es.enter_context(SBT("u1", [128, 512], F32))
                wm = [wm0, wm1, wm2, wm3]
                bdb = [bdb0, bdb1]
                wmi = 0
                t.op('dve', lambda e: e.memset(yacc[:], 0.0), w=['yacc'])
                units = []
                for ex in range(NEXP):
                    for qf in range(8):
                        units.append(('A', ex, qf, wmi % 4, (wmi + 1) % 4))
                        wmi += 2
                    for dq in range(8):
                        units.append(('B', ex, dq, wmi % 4, dq % 2))
                        wmi += 1

                def issue(u):
                    kind, ex, idx, a, b = u
                    if kind == 'A':
                        t.dma('pool', lambda e: e.dma_start(out=wm[a][:], in_=wg_d[ex, idx]), w=[f'wm{a}'])
                        t.dma('pool', lambda e: e.dma_start(out=wm[b][:], in_=wu_d[ex, idx]), w=[f'wm{b}'])
                    else:
                        ds_ = slice(idx * 256, (idx + 1) * 256)
                        t.dma('pool', lambda e: e.dma_start(out=wm[a][:], in_=wd_d[ex, idx]), w=[f'wm{a}'])
                        t.dma('sp', lambda e: e.dma_start(out=bdb[b][:], in_=bd_d[ex:ex + 1, ds_].partition_broadcast(128)),
                              w=[f'bdb{b}'])

                def compute(u):
                    kind, ex, idx, a, b = u
                    if kind == 'A':
                        qf, bg_, bu_ = idx, a, b
                        for ft in range(2):
                            f = qf * 2 + ft
                            bgc = pv[:, PV_BG + ex * 16 + f:PV_BG + ex * 16 + f + 1]
                            buc = pv[:, PV_BU + ex * 16 + f:PV_BU + ex * 16 + f + 1]
                            for ch in range(2):
                                cs = slice(ch * 512, (ch + 1) * 512)
                                pg, pu = ps[2 * ch], ps[2 * ch + 1]
                                kg, ku = P(2 * ch), P(2 * ch + 1)
                                for kc in range(16):
                                    t.op('pe', lambda e: e.matmul(pg[:], lhsT=wm[bg_][:, kc, ft * 128:(ft + 1) * 128], rhs=h2T[:, kc, cs],
                                                                  start=(kc == 0), stop=(kc == 15)), r=[f'wm{bg_}', 'h2T'], w=[kg])
                                for kc in range(16):
                                    t.op('pe', lambda e: e.matmul(pu[:], lhsT=wm[bu_][:, kc, ft * 128:(ft + 1) * 128], rhs=h2T[:, kc, cs],
                                                                  start=(kc == 0), stop=(kc == 15)), r=[f'wm{bu_}', 'h2T'], w=[ku])
                                t.op('dve', lambda e: e.tensor_scalar(out=g1[:], in0=pg[:], scalar1=bgc, scalar2=7.0, op0=ALU.add, op1=ALU.min),
                                     r=[kg, 'pv'], w=['g1'])
                                t.op('act', lambda e: e.activation(out=sg[:], in_=g1[:], func=AF.Sigmoid, scale=1.702), r=['g1'], w=['sg'])
                                t.op('dve', lambda e: e.tensor_scalar(out=u1[:], in0=pu[:], scalar1=buc, scalar2=7.0, op0=ALU.add, op1=ALU.min),
                                     r=[ku, 'pv'], w=['u1'])
                                t.op('dve', lambda e: e.tensor_scalar(out=u1[:], in0=u1[:], scalar1=-7.0, scalar2=1.0, op0=ALU.max, op1=ALU.add),
                                     r=['u1'], w=['u1'])
                                t.op('pool', lambda e: e.tensor_tensor(out=g1[:], in0=g1[:], in1=sg[:], op=ALU.mult), r=['g1', 'sg'], w=['g1'])
                                t.op('dve', lambda e: e.tensor_tensor(out=actT[:, f, cs], in0=g1[:], in1=u1[:], op=ALU.mult),
                                     r=['g1', 'u1'], w=['actT'])
                    else:
                        dq, bd_, bb = idx, a, b
                        ds_ = slice(dq * 256, (dq + 1) * 256)
                        for ti in range(8):
                            pd, kd = ps[4 + ti % 2], P(4 + ti % 2)
                            for fc in range(16):
                                t.op('pe', lambda e: e.matmul(pd[:, 0:256], lhsT=actT[:, fc, ti * 128:(ti + 1) * 128], rhs=wm[bd_][:, fc, :],
                                                              start=(fc == 0), stop=(fc == 15)), r=['actT', f'wm{bd_}'], w=[kd])
                            t.op('dve', lambda e: e.tensor_tensor(out=sg[:, 0:256], in0=pd[:, 0:256], in1=bdb[bb][:], op=ALU.add),
                                 r=[kd, f'bdb{bb}'], w=['sg'])
                            t.op('dve', lambda e: e.scalar_tensor_tensor(out=yacc[:, ti, ds_], in0=sg[:, 0:256], scalar=wt[:, ti, ex:ex + 1],
                                                                         in1=yacc[:, ti, ds_], op0=ALU.mult, op1=ALU.add),
                                 r=['sg', 'wt', 'yacc'], w=['yacc'])

                issue(units[0])
                for ui, u in enumerate(units):
                    if ui + 1 < len(units):
                        issue(units[ui + 1])
                    compute(u)
                for ti in range(8):
                    gt = hf * 8 + ti
                    for q in range(4):
                        qs = slice(q * 512, (q + 1) * 512)
                        t.dma('sp', lambda e: e.dma_start(out=g1[:], in_=x1_d[gt * 128:(gt + 1) * 128, qs]), r=['x1d'], w=['g1'])
                        t.op('dve', lambda e: e.tensor_tensor(out=u1[:], in0=yacc[:, ti, qs], in1=G2[:, qs], op=ALU.mult),
                             r=['yacc', 'G2'], w=['u1'])
                        t.op('dve', lambda e: e.tensor_tensor(out=g1[:], in0=g1[:], in1=u1[:], op=ALU.add), r=['g1', 'u1'], w=['g1'])
                        t.dma('sp', lambda e: e.dma_start(out=out_d[gt * 128:(gt + 1) * 128, qs], in_=g1[:]), r=['g1'], w=['outd'])
                t.barrier()
    return nc


def _t5_bucket(dist):
    max_exact = 16
    d = np.maximum(dist, 0)
    lr = np.log(np.maximum(d, max_exact).astype(np.float32) / max_exact)
    large = max_exact + (lr / math.log(128 / max_exact) * (32 - max_exact)).astype(np.int32)
    large = np.minimum(large, 31)
    return np.where(d < max_exact, d, large)


def _prep(inputs):
    f = np.float32
    x = np.asarray(inputs["x"], f)[0]
    g = lambda k: np.asarray(inputs[k], f)[0]
    pp = lambda v: np.ascontiguousarray(v.reshape(-1, 128).T)
    perm = []
    for jj in range(8):
        perm += list(range(jj * 64, jj * 64 + 64)) + list(range((8 + jj) * 64, (8 + jj) * 64 + 64))
    perm += list(range(1024, 3328))
    perm = np.array(perm)
    win = np.ascontiguousarray(g("w_in")[:, perm])
    b_in = g("b_in")[perm]
    pv = np.zeros((128, NPV), f)
    pv[:, PV_C:PV_C + 16] = pp(np.asarray(inputs["c"], f)[0])
    pv[:, PV_G1:PV_G1 + 16] = pp(g("g_norm1"))
    pv[:, PV_BIN:PV_BIN + 26] = pp(b_in)
    pv[:, PV_GQ] = np.tile(g("g_q"), 2)
    pv[:, PV_GK] = np.tile(g("g_k"), 2)
    pv[:, PV_WDW:PV_WDW + 248] = g("w_dw").reshape(31, 8, 128).transpose(2, 1, 0).reshape(128, 248)
    pv[:, PV_BDW:PV_BDW + 8] = pp(g("b_dw"))
    pv[:, PV_LNG:PV_LNG + 8] = pp(g("ln_g"))
    pv[:, PV_LNB:PV_LNB + 8] = pp(g("ln_b"))
    pv[:, PV_GOC:PV_GOC + 8] = pp(g("g_out_conv"))
    pv[:, PV_BG:PV_BG + 512] = g("b_gate").reshape(32, 16, 128).transpose(2, 0, 1).reshape(128, 512)
    pv[:, PV_BU:PV_BU + 512] = g("b_up").reshape(32, 16, 128).transpose(2, 0, 1).reshape(128, 512)
    pv[:, PV_G2:PV_G2 + 16] = pp(g("g_norm2"))
    pv[:, PV_WR:PV_WR + 512] = g("w_router").reshape(16, 128, 32).transpose(1, 0, 2).reshape(128, 512)
    rv = np.zeros((1, NRV), f)
    rv[0, RV_GOA:RV_GOA + 1024] = g("g_out_attn")
    rv[0, RV_BOUT:RV_BOUT + D] = g("b_out")
    rv[0, RV_G2:RV_G2 + D] = g("g_norm2")
    rv[0, RV_BR:RV_BR + 32] = g("b_router")
    rv[0, RV_SINK:RV_SINK + 16] = g("sinks")
    rv = np.ascontiguousarray(np.broadcast_to(rv, (128, NRV)))
    rb = np.asarray(inputs["rel_bias"], f)
    kk = np.arange(128)[:, None]
    qq = np.arange(128)[None, :]
    bt = np.zeros((128, 2, 2, 8, 128), f)
    for kt in range(2):
        dist = qq + 128 - kk if kt == 0 else qq - kk
        valid = (dist >= 0) & (dist < 128)
        bk = _t5_bucket(dist)
        for gg in range(2):
            for jj in range(8):
                bt[:, kt, gg, jj, :] = np.where(valid, rb[bk, 8 * gg + jj], f(-30000.0))
    bt = bt.reshape(128, 4096)
    blk = np.zeros((128, 128), f)
    blk[:64, :64] = 1
    blk[64:, 64:] = 1
    def tile_w(w):
        return np.ascontiguousarray(w.reshape(E, 16, 128, 8, 256).transpose(0, 3, 2, 1, 4))

    common = dict(pv=pv, rv=rv, bada=g("b_ada")[None, :], wada=g("w_ada"), win=win, wout=g("w_out"), biast=bt,
                  wg=tile_w(g("w_gate")), wu=tile_w(g("w_up")), wd=tile_w(g("w_down")), bd=g("b_down"),
                  identf=np.eye(128, dtype=f), blk=blk)
    in_maps = []
    for c in range(NCORE):
        xh = np.zeros((17 * 128, D), f)
        if c == 0:
            xh[128:] = x[0:TOK]
        else:
            xh[:] = x[c * TOK - 128:(c + 1) * TOK]
        hvv = np.full((128, 1), 0.0 if c == 0 else 1.0, f)
        m = dict(common)
        m["xh"] = xh
        m["hv"] = hvv
        in_maps.append(m)
    return in_maps


def kernel(**inputs):
    in_maps = _prep(inputs)
    nc = build_nc()
    res = run_bass_kernel_spmd(nc, in_maps, core_ids=list(range(NCORE)))
    out = np.concatenate([np.asarray(r["out"], np.float32) for r in res.results], axis=0)
    return out.reshape(1, NCORE * TOK, D)
```

```python
import math
from contextlib import ExitStack
import numpy as np
import concourse.bass as bass
import concourse.mybir as mybir
from concourse.bass_utils import run_bass_kernel_spmd

F32 = mybir.dt.float32
BF16 = mybir.dt.bfloat16
ALU = mybir.AluOpType
AF = mybir.ActivationFunctionType
AX = mybir.AxisListType

D = 2048
NCORE = 8
TOK = 2048
NT = 16
E = 32
EPS = 1e-6
PV_C, PV_G1, PV_BIN, PV_GQ, PV_GK = 0, 16, 32, 58, 59
PV_WDW, PV_BDW, PV_LNG, PV_LNB, PV_GOC = 60, 308, 316, 324, 332
PV_BG, PV_BU, PV_WR = 340, 852, 1364
PV_G2 = 1364 + 512
NPV = 1364 + 512 + 16
RV_GOA, RV_BOUT, RV_G2, RV_BR, RV_SINK = 0, 1024, 3072, 5120, 5152
NRV = 5168


class Trk:
    def __init__(s, nc):
        s.nc = nc
        s.eng = dict(pe=nc.tensor, act=nc.scalar, dve=nc.vector, pool=nc.gpsimd, sp=nc.sync)
        s.csem = {e: nc.alloc_semaphore("c_" + e) for e in s.eng}
        s.ND = 24
        s.dsem = [nc.alloc_semaphore(f"dq{i}") for i in range(s.ND)]
        s.reset_state()

    def reset_state(s):
        s.cnt = {e: 0 for e in s.eng}
        s.seen = {c: {p: 0 for p in s.eng} for c in s.eng}
        s.dval = [0] * s.ND
        s.dseen = {c: [0] * s.ND for c in s.eng}
        s.drr = 0
        s.lastw = {}
        s.readers = {}

    def _wait(s, c, tok):
        if tok[0] == 'e':
            _, p, seq = tok
            if p == c and p == 'pe':
                return
            if s.seen[c][p] >= seq:
                return
            s.eng[c].wait_ge(s.csem[p], seq)
            s.seen[c][p] = seq
        else:
            _, k, val = tok
            if s.dseen[c][k] >= val:
                return
            s.eng[c].wait_ge(s.dsem[k], val)
            s.dseen[c][k] = val

    def _deps(s, c, r, w):
        for b in r:
            t = s.lastw.get(b)
            if t:
                s._wait(c, t)
        for b in w:
            t = s.lastw.get(b)
            if t:
                s._wait(c, t)
            rd = s.readers.get(b)
            if rd:
                for p, seq in rd[0].items():
                    if p != c:
                        s._wait(c, ('e', p, seq))
                for t in rd[1]:
                    s._wait(c, t)

    def _record(s, tok, r, w):
        for b in r:
            rd = s.readers.setdefault(b, [{}, []])
            if tok[0] == 'e':
                rd[0][tok[1]] = tok[2]
            else:
                rd[1].append(tok)
        for b in w:
            s.lastw[b] = tok
            s.readers[b] = [{}, []]

    def op(s, c, fn, r=(), w=()):
        s._deps(c, r, w)
        ins = fn(s.eng[c])
        s.cnt[c] += 1
        ins.then_inc(s.csem[c], 1)
        s._record(('e', c, s.cnt[c]), r, w)

    def dma(s, c, fn, r=(), w=()):
        s._deps(c, r, w)
        k = s.drr
        s.drr = (s.drr + 1) % s.ND
        if s.dval[k] > 0:
            s._wait(c, ('d', k, s.dval[k]))
        ins = fn(s.eng[c])
        s.dval[k] += 16
        ins.then_inc(s.dsem[k], 16)
        s._record(('d', k, s.dval[k]), r, w)

    def barrier(s):
        for c in s.eng:
            for p in s.eng:
                if s.cnt[p] > 0:
                    s._wait(c, ('e', p, s.cnt[p]))
            for k in range(s.ND):
                if s.dval[k] > 0:
                    s._wait(c, ('d', k, s.dval[k]))
        s.lastw = {}
        s.readers = {}


def build_nc(dbg=None):
    dbg = dbg or {}
    STOP = dbg.get('stop')
    HALVES = dbg.get('halves', [0, 1])
    GROUPS = dbg.get('groups', [0, 1])
    NEXP = dbg.get('n_exp', E)
    TAPS = dbg.get('taps', False)
    nc = bass.Bass("TRN2", target_bir_lowering=False)

    def din(name, shape, dt=F32):
        return nc.dram_tensor(name, list(shape), dt, kind="ExternalInput").ap()

    xh_d = din("xh", [17 * 128, D])
    hv_d = din("hv", [128, 1])
    pv_d = din("pv", [128, NPV])
    rv_d = din("rv", [128, NRV])
    bada_d = din("bada", [1, 6 * D])
    wada_d = din("wada", [D, 6 * D])
    win_d = din("win", [D, 3328])
    wout_d = din("wout", [D, D])
    bias_d = din("biast", [128, 4096])
    wg_d = din("wg", [NEXP, 8, 128, 16, 256])
    wu_d = din("wu", [NEXP, 8, 128, 16, 256])
    wd_d = din("wd", [NEXP, 4, 128, 16, 512])
    bd_d = din("bd", [NEXP, D])
    identf_d = din("identf", [128, 128])
    blk_d = din("blk", [128, 128])
    out_d = nc.dram_tensor("out", [TOK, D], F32, kind="ExternalOutput").ap()
    x1_d = nc.dram_tensor("x1s", [TOK, D], F32, kind=("ExternalOutput" if TAPS else "Internal")).ap()
    g1s_d = nc.dram_tensor("g1s", [2, 128, D], F32, kind="Internal").ap()

    t = Trk(nc)

    def tap(name, tens, shape, dt, key):
        if not TAPS:
            return
        dd = nc.dram_tensor("tap_" + name, list(shape), dt, kind="ExternalOutput").ap()
        t.dma('sp', lambda e: e.dma_start(out=dd, in_=tens[:]), r=[key], w=['tap_' + name])
    _uid = [0]

    def SB(name, shape, dt):
        _uid[0] += 1
        return nc.alloc_sbuf_tensor(f"{name}_s{_uid[0]}", shape, dt)

    def SBT(name, shape, dt):
        _uid[0] += 1
        return nc.sbuf_tensor(f"{name}_s{_uid[0]}", shape, dt)

    pv = SB("pv", [128, NPV], F32)
    hv = SB("hv", [128, 1], F32)
    identf = SB("identf", [128, 128], F32)
    identb = SB("identb", [128, 128], BF16)
    blkb = SB("blkb", [128, 128], BF16)
    onesf = SB("onesf", [128, 128], F32)
    sT = SB("sT", [128, 16], BF16)
    modT = SB("modT", [128, 4, 16], F32)
    A2 = SB("A2", [128, 16], F32)
    A1 = SB("A1", [128, 16], F32)
    G2 = SB("G2", [128, D], F32)
    goa = SB("goa", [128, 1024], F32)
    brt = SB("brt", [128, 32], F32)
    esink = SB("esink", [128, 16], F32)
    ebt = SB("ebt", [128, 4096], BF16)
    ebt0 = SB("ebt0", [128, 2048], BF16)
    wrb = SB("wrb", [128, 512], BF16)
    ssq = SB("ssq", [128, 1], F32)
    rstd = SB("rstd", [128, 1], F32)

    ps = [nc.alloc_psum_tensor(f"ps{i}", [128, 512], F32) for i in range(6)]
    psT = [nc.alloc_psum_tensor(f"psT{i}", [128, 1024], BF16) for i in range(2)]

    def P(i):
        return f"ps{i}"

    t.dma('sp', lambda e: e.dma_start(out=pv[:], in_=pv_d), w=['pv'])
    t.dma('sp', lambda e: e.dma_start(out=hv[:], in_=hv_d), w=['hv'])
    t.dma('sp', lambda e: e.dma_start(out=identf[:], in_=identf_d), w=['identf'])
    t.dma('pool', lambda e: e.dma_start(out=identb[:], in_=identf_d), w=['identb'])
    t.dma('pool', lambda e: e.dma_start(out=blkb[:], in_=blk_d), w=['blkb'])
    t.op('dve', lambda e: e.memset(onesf[:], 1.0), w=['onesf'])
    t.op('dve', lambda e: e.tensor_copy(out=wrb[:], in_=pv[:, PV_WR:PV_WR + 512]), r=['pv'], w=['wrb'])
    t.op('act', lambda e: e.activation(out=sT[:], in_=pv[:, PV_C:PV_C + 16], func=AF.Silu), r=['pv'], w=['sT'])

    with ExitStack() as _es:
        rvt = _es.enter_context(SBT("rvt", [128, NRV], F32))
        biasf = _es.enter_context(SBT("biasf", [128, 4096], F32))
        wr0 = _es.enter_context(SBT("wr0", [128, 16, 512], BF16))
        wr1 = _es.enter_context(SBT("wr1", [128, 16, 512], BF16))
        brow0 = _es.enter_context(SBT("brow0", [1, 512], F32))
        brow1 = _es.enter_context(SBT("brow1", [1, 512], F32))
        mrow = _es.enter_context(SBT("mrow", [1, 512], F32))
        bcg = _es.enter_context(SBT("bcg", [128, D], F32))
        gb1 = _es.enter_context(SBT("gb1", [128, D], F32))
        wr = [wr0, wr1]
        brow = [brow0, brow1]
        t.dma('sp', lambda e: e.dma_start(out=rvt[:], in_=rv_d), w=['rvt'])
        t.dma('sp', lambda e: e.dma_start(out=biasf[:], in_=bias_d), w=['biasf'])
        t.op('act', lambda e: e.activation(out=ebt[:], in_=biasf[:], func=AF.Exp), r=['biasf'], w=['ebt'])
        t.op('dve', lambda e: e.tensor_scalar(out=ebt0[:], in0=ebt[:, 0:2048], scalar1=hv[:, 0:1], scalar2=None,
                                              op0=ALU.mult), r=['ebt', 'hv'], w=['ebt0'])
        t.op('act', lambda e: e.activation(out=esink[:], in_=rvt[:, RV_SINK:RV_SINK + 16], func=AF.Exp),
             r=['rvt'], w=['esink'])
        t.op('dve', lambda e: e.tensor_copy(out=goa[:], in_=rvt[:, RV_GOA:RV_GOA + 1024]), r=['rvt'], w=['goa'])
        t.op('dve', lambda e: e.tensor_copy(out=brt[:], in_=rvt[:, RV_BR:RV_BR + 32]), r=['rvt'], w=['brt'])

        wada_v = wada_d.rearrange("(kc p) f -> p kc f", p=128)
        for n in range(24):
            b = n % 2
            t.dma('pool', lambda e: e.dma_start(out=wr[b][:], in_=wada_v[:, :, n * 512:(n + 1) * 512]), w=[f'wr{b}'])
            t.dma('sp', lambda e: e.dma_start(out=brow[b][:], in_=bada_d[0:1, n * 512:(n + 1) * 512]), w=[f'brow{b}'])
            for kc in range(16):
                t.op('pe', lambda e: e.matmul(ps[0][0:1, :], lhsT=sT[:, kc:kc + 1], rhs=wr[b][:, kc, :],
                                              start=(kc == 0), stop=(kc == 15)), r=['sT', f'wr{b}'], w=[P(0)])
            t.op('dve', lambda e: e.tensor_tensor(out=mrow[:], in0=ps[0][0:1, :], in1=brow[b][:], op=ALU.add),
                 r=[P(0), f'brow{b}'], w=['mrow'])
            which, q = n // 4, n % 4
            if which in (0, 1, 3, 4):
                mi = {0: 0, 1: 1, 3: 2, 4: 3}[which]
                for j in range(4):
                    t.op('pe', lambda e: e.matmul(ps[1][:, j:j + 1], lhsT=mrow[0:1, j * 128:(j + 1) * 128],
                                                  rhs=onesf[0:1, 0:1], start=True, stop=True), r=['mrow', 'onesf'], w=[P(1)])
                t.op('dve', lambda e: e.tensor_copy(out=modT[:, mi, q * 4:(q + 1) * 4], in_=ps[1][:, 0:4]),
                     r=[P(1)], w=['modT'])
            else:
                dstt, dk = (bcg, 'bcg') if which == 2 else (G2, 'G2')
                t.op('pe', lambda e: e.matmul(ps[2][:, :], lhsT=onesf[0:1, 0:128], rhs=mrow[0:1, :],
                                              start=True, stop=True), r=['mrow', 'onesf'], w=[P(2)])
                t.op('act', lambda e: e.activation(out=dstt[:, q * 512:(q + 1) * 512], in_=ps[2][:, :], func=AF.Identity),
                     r=[P(2)], w=[dk])
        t.op('dve', lambda e: e.scalar_tensor_tensor(out=A1[:], in0=modT[:, 1, :], scalar=1.0, in1=pv[:, PV_G1:PV_G1 + 16],
                                                     op0=ALU.add, op1=ALU.mult), r=['modT', 'pv'], w=['A1'])
        t.op('dve', lambda e: e.scalar_tensor_tensor(out=A2[:], in0=modT[:, 3, :], scalar=1.0, in1=pv[:, PV_G2:PV_G2 + 16],
                                                     op0=ALU.add, op1=ALU.mult), r=['modT', 'pv'], w=['A2'])
        t.op('dve', lambda e: e.tensor_tensor(out=gb1[:], in0=bcg[:], in1=rvt[:, RV_BOUT:RV_BOUT + D], op=ALU.mult),
             r=['bcg', 'rvt'], w=['gb1'])
        t.dma('sp', lambda e: e.dma_start(out=g1s_d[0], in_=bcg[:]), r=['bcg'], w=['g1sd'])
        t.dma('sp', lambda e: e.dma_start(out=g1s_d[1], in_=gb1[:]), r=['gb1'], w=['g1sd'])
        tap('modT', modT, [128, 4, 16], F32, 'modT')
        tap('A1', A1, [128, 16], F32, 'A1')
        tap('bc0', bcg, [128, D], F32, 'bcg')
        tap('ebt', ebt, [128, 4096], BF16, 'ebt')
        t.barrier()
        if STOP == 'ada':
            return nc

    win_v = win_d.rearrange("(kc p) f -> p kc f", p=128)
    wout_v = wout_d.rearrange("(kc p) f -> p kc f", p=128)

    def rsqrt_into(dst, src_ap, scale, r, w):
        t.op('act', lambda e: e.activation(out=dst, in_=src_ap, func=AF.Sqrt, bias=EPS, scale=scale), r=r, w=w)
        t.op('dve', lambda e: e.reciprocal(out=dst, in_=dst), r=w, w=w)

    for hf in HALVES:
        with ExitStack() as _esh:
            h2T = _esh.enter_context(SBT("h2T", [128, 16, 1024], BF16))
            wt = _esh.enter_context(SBT("wt", [128, 8, 32], F32))
            with ExitStack() as _esm:
                mixedT = _esm.enter_context(SBT("mixedT", [128, 16, 1024], BF16))
                with ExitStack() as _es:
                    qT = _es.enter_context(SBT("qT", [128, 8, 1152], BF16))
                    kT = _es.enter_context(SBT("kT", [128, 1152], BF16))
                    v1 = _es.enter_context(SBT("v1", [128, 9, 2, 65], BF16))
                    hglu = _es.enter_context(SBT("hglu", [128, 8, 1152], BF16))
                    t.op('pool', lambda e: e.memset(v1[:], 1.0), w=['v1'])
                    with ExitStack() as _es:
                        hT = _es.enter_context(SBT("hT", [128, 16, 1152], BF16))
                        xb0 = _es.enter_context(SBT("xb0", [128, D], F32))
                        xs = _es.enter_context(SBT("xs", [128, D], BF16))
                        wi0 = _es.enter_context(SBT("wi0", [128, 16, 256], BF16))
                        wi1 = _es.enter_context(SBT("wi1", [128, 16, 256], BF16))
                        tA = xb0[:, 0:512]
                        tB = xs[:, 0:512]
                        tC = xb0[:, 512:1024]
                        xb = [xb0, xb0]; junk = xs
                        wi = [wi0, wi1]
                        for tt in range(9):
                            xt = xb[tt % 2]
                            xk = 'xb0'
                            r0 = (hf * 8 + tt) * 128
                            t.dma('sp', lambda e: e.dma_start(out=xt[:], in_=xh_d[r0:r0 + 128, :]), w=[xk])
                            t.op('dve', lambda e: e.memset(ssq[:], 0.0), w=['ssq'])
                            t.op('act', lambda e: e.activation(out=junk[:], in_=xt[:], func=AF.Square, accum_out=ssq[:, 0:1]),
                                 r=[xk, 'ssq'], w=['xs', 'ssq'])
                            rsqrt_into(rstd[:], ssq[:], 1.0 / D, ['ssq'], ['rstd'])
                            t.op('dve', lambda e: e.tensor_scalar(out=xs[:], in0=xt[:], scalar1=rstd[:, 0:1], scalar2=None, op0=ALU.mult),
                                 r=[xk, 'rstd'], w=['xs'])
                            for c in range(16):
                                t.op('pe', lambda e: e.transpose(out=psT[c // 8][:, (c % 8) * 128:(c % 8 + 1) * 128],
                                                                 in_=xs[:, c * 128:(c + 1) * 128], identity=identb[:]),
                                     r=['xs', 'identb'], w=[f'psT{c // 8}'])
                            for c in range(16):
                                t.op('dve', lambda e: e.tensor_scalar(out=hT[:, c, tt * 128:(tt + 1) * 128],
                                                                      in0=psT[c // 8][:, (c % 8) * 128:(c % 8 + 1) * 128],
                                                                      scalar1=A1[:, c:c + 1], scalar2=modT[:, 0, c:c + 1],
                                                                      op0=ALU.mult, op1=ALU.add),
                                     r=[f'psT{c // 8}', 'A1', 'modT'], w=['hT'])
                        if STOP == 'norm':
                            tap(f'hT{hf}', hT, [128, 16, 1152], BF16, 'hT')
                            t.barrier()
                            return nc
                        t.barrier()
                        chunks = [(0, 128), (128, 512), (640, 512)]
                        for j in dbg.get('jlist', range(26)):
                            wc = j // 2
                            b = wc % 2
                            if j % 2 == 0 or 'jlist' in dbg:
                                t.dma('pool', lambda e: e.dma_start(out=wi[b][:], in_=win_v[:, :, wc * 256:wc * 256 + 256]),
                                      w=[f'wi{b}'])
                            jo = (j % 2) * 128
                            bias = pv[:, PV_BIN + j:PV_BIN + j + 1]
                            for ci, (c0, cn) in enumerate(chunks):
                                pb = ps[ci % 2]
                                for kc in range(0 if dbg.get('nomm') else 16):
                                    t.op('pe', lambda e: e.matmul(pb[:, 0:cn], lhsT=wi[b][:, kc, jo:jo + 128], rhs=hT[:, kc, c0:c0 + cn],
                                                                  start=(kc == 0), stop=(kc == 15)), r=[f'wi{b}', 'hT'], w=[P(ci % 2)])
                                if dbg.get('noevac'):
                                    continue
                                if j < 9:
                                    gcol = PV_GQ if j < 8 else PV_GK
                                    dst = qT[:, j, c0:c0 + cn] if j < 8 else kT[:, c0:c0 + cn]
                                    dk = 'qT' if j < 8 else 'kT'
                                    t.op('act', lambda e: e.activation(out=tB[:, 0:cn], in_=pb[:, 0:cn], func=AF.Square, bias=bias),
                                         r=[P(ci % 2), 'pv'], w=['tB'])
                                    t.op('act', lambda e: e.activation(out=tA[:, 0:cn], in_=pb[:, 0:cn], func=AF.Identity, bias=bias),
                                         r=[P(ci % 2), 'pv'], w=['tA'])
                                    t.op('pe', lambda e: e.matmul(ps[2][:, 0:cn], lhsT=blkb[:], rhs=tB[:, 0:cn], start=True, stop=True),
                                         r=['blkb', 'tB'], w=[P(2)])
                                    rsqrt_into(tC[:, 0:cn], ps[2][:, 0:cn], 1.0 / 64, [P(2)], ['tC'])
                                    t.op('dve', lambda e: e.scalar_tensor_tensor(out=dst, in0=tA[:, 0:cn], scalar=pv[:, gcol:gcol + 1],
                                                                                 in1=tC[:, 0:cn], op0=ALU.mult, op1=ALU.mult),
                                         r=['tA', 'tC', 'pv'], w=[dk])
                                elif j == 9:
                                    t.op('act', lambda e: e.activation(out=tB[:, 0:cn], in_=pb[:, 0:cn], func=AF.Identity, bias=bias),
                                         r=[P(ci % 2), 'pv'], w=['tB'])
                                    for s_ in range(cn // 128):
                                        tt = c0 // 128 + s_
                                        t.op('pe', lambda e: e.transpose(out=psT[0][:, 0:128], in_=tB[:, s_ * 128:(s_ + 1) * 128],
                                                                         identity=identb[:]), r=['tB', 'identb'], w=['psT0'])
                                        t.op('dve', lambda e: e.tensor_copy(out=v1[:, tt, :, 0:64],
                                                                            in_=psT[0][:, 0:128].rearrange("p (g d) -> p g d", g=2)),
                                             r=['psT0'], w=['v1'])
                                elif j < 18:
                                    c = j - 10
                                    t.op('dve', lambda e: e.tensor_scalar(out=hglu[:, c, c0:c0 + cn], in0=pb[:, 0:cn], scalar1=bias,
                                                                          scalar2=None, op0=ALU.add), r=[P(ci % 2), 'pv'], w=['hglu'])
                                else:
                                    c = j - 18
                                    t.op('act', lambda e: e.activation(out=tB[:, 0:cn], in_=pb[:, 0:cn], func=AF.Sigmoid, bias=bias),
                                         r=[P(ci % 2), 'pv'], w=['tB'])
                                    t.op('pool', lambda e: e.tensor_tensor(out=hglu[:, c, c0:c0 + cn], in0=hglu[:, c, c0:c0 + cn],
                                                                           in1=tB[:, 0:cn], op=ALU.mult), r=['tB', 'hglu'], w=['hglu'])
                        if hf == 0 and not dbg.get('nohv'):
                            for c in range(8):
                                t.op('pool', lambda e: e.tensor_scalar(out=hglu[:, c, 0:128], in0=hglu[:, c, 0:128], scalar1=hv[:, 0:1],
                                                                       scalar2=None, op0=ALU.mult), r=['hglu', 'hv'], w=['hglu'])
                        tap(f'hT{hf}', hT, [128, 16, 1152], BF16, 'hT')
                        tap(f'qT{hf}', qT, [128, 8, 1152], BF16, 'qT')
                        tap(f'kT{hf}', kT, [128, 1152], BF16, 'kT')
                        tap(f'v1{hf}', v1, [128, 9, 2, 65], BF16, 'v1')
                        tap(f'hglu{hf}', hglu, [128, 8, 1152], BF16, 'hglu')
                        t.barrier()
                        if STOP == 'inproj':
                            return nc

                    with ExitStack() as _es:
                        acc = _es.enter_context(SBT("acc", [128, 8, 1024], F32))
                        sq = _es.enter_context(SBT("sq", [128, 512], F32))
                        mean = _es.enter_context(SBT("mean", [128, 512], F32))
                        var = _es.enter_context(SBT("var", [128, 512], F32))
                        tmp = _es.enter_context(SBT("tmp", [128, 512], F32))
                        yatt = _es.enter_context(SBT("yatt", [128, 1024], F32))
                        ynb = _es.enter_context(SBT("ynb", [128, 1024], BF16))
                        pe0 = _es.enter_context(SBT("pe0", [128, 512], F32))
                        pe1 = _es.enter_context(SBT("pe1", [128, 512], F32))
                        pT0 = _es.enter_context(SBT("pT0", [128, 1024], BF16))
                        pT1 = _es.enter_context(SBT("pT1", [128, 1024], BF16))
                        den = _es.enter_context(SBT("den", [128, 8], F32))
                        junk2 = _es.enter_context(SBT("junk2", [128, 1024], BF16))
                        for c in range(8):
                            en = 'dve'
                            ak = f'acc{c}'
                            wc0 = PV_WDW + c * 31
                            t.op(en, lambda e: e.tensor_scalar(out=acc[:, c, :], in0=hglu[:, c, 98:98 + 1024], scalar1=pv[:, wc0:wc0 + 1],
                                                               scalar2=pv[:, PV_BDW + c:PV_BDW + c + 1], op0=ALU.mult, op1=ALU.add),
                                 r=['hglu', 'pv'], w=[ak])
                            for j in range(1, 31):
                                if en == 'dve':
                                    t.op(en, lambda e: e.scalar_tensor_tensor(out=acc[:, c, :], in0=hglu[:, c, 98 + j:98 + j + 1024],
                                                                              scalar=pv[:, wc0 + j:wc0 + j + 1], in1=acc[:, c, :],
                                                                              op0=ALU.mult, op1=ALU.add), r=['hglu', 'pv', ak], w=[ak])
                                else:
                                    t.op(en, lambda e: e.tensor_scalar(out=ctmp[:], in0=hglu[:, c, 98 + j:98 + j + 1024],
                                                                       scalar1=pv[:, wc0 + j:wc0 + j + 1], scalar2=None, op0=ALU.mult),
                                         r=['hglu', 'pv'], w=['ctmp'])
                                    t.op(en, lambda e: e.tensor_tensor(out=acc[:, c, :], in0=acc[:, c, :], in1=ctmp[:], op=ALU.add),
                                         r=['ctmp', ak], w=[ak])
                        for ch in range(2):
                            cs = slice(ch * 512, (ch + 1) * 512)
                            for c in range(8):
                                t.op('act', lambda e: e.activation(out=sq[:], in_=acc[:, c, cs], func=AF.Square), r=[f'acc{c}'], w=['sq'])
                                t.op('pe', lambda e: e.matmul(ps[0][:], lhsT=onesf[:], rhs=acc[:, c, cs], start=(c == 0), stop=(c == 7)),
                                     r=['onesf', f'acc{c}'], w=[P(0)])
                                t.op('pe', lambda e: e.matmul(ps[1][:], lhsT=onesf[:], rhs=sq[:], start=(c == 0), stop=(c == 7)),
                                     r=['onesf', 'sq'], w=[P(1)])
                            t.op('dve', lambda e: e.tensor_scalar(out=mean[:], in0=ps[0][:], scalar1=1.0 / 1024, scalar2=None, op0=ALU.mult),
                                 r=[P(0)], w=['mean'])
                            t.op('dve', lambda e: e.tensor_tensor(out=tmp[:], in0=mean[:], in1=mean[:], op=ALU.mult), r=['mean'], w=['tmp'])
                            t.op('dve', lambda e: e.scalar_tensor_tensor(out=var[:], in0=ps[1][:], scalar=1.0 / 1024, in1=tmp[:],
                                                                         op0=ALU.mult, op1=ALU.subtract), r=[P(1), 'tmp'], w=['var'])
                            rsqrt_into(var[:], var[:], 1.0, ['var'], ['var'])
                            for c in range(8):
                                ak = f'acc{c}'
                                t.op('dve', lambda e: e.tensor_tensor(out=tmp[:], in0=acc[:, c, cs], in1=mean[:], op=ALU.subtract),
                                     r=[ak, 'mean'], w=['tmp'])
                                t.op('dve', lambda e: e.tensor_tensor(out=tmp[:], in0=tmp[:], in1=var[:], op=ALU.mult), r=['tmp', 'var'], w=['tmp'])
                                t.op('act', lambda e: e.activation(out=acc[:, c, cs], in_=tmp[:], func=AF.Silu,
                                                                   bias=pv[:, PV_LNB + c:PV_LNB + c + 1], scale=pv[:, PV_LNG + c:PV_LNG + c + 1]),
                                     r=['tmp', 'pv'], w=[ak])
                                t.op('act', lambda e: e.activation(out=sq[:], in_=acc[:, c, cs], func=AF.Square), r=[ak], w=['sq'])
                                t.op('pe', lambda e: e.matmul(ps[2][:], lhsT=onesf[:], rhs=sq[:], start=(c == 0), stop=(c == 7)),
                                     r=['onesf', 'sq'], w=[P(2)])
                            rsqrt_into(var[:], ps[2][:], 1.0 / 1024, [P(2)], ['var'])
                            for c in range(8):
                                t.op('dve', lambda e: e.scalar_tensor_tensor(out=mixedT[:, 8 + c, cs], in0=acc[:, c, cs],
                                                                             scalar=pv[:, PV_GOC + c:PV_GOC + c + 1], in1=var[:],
                                                                             op0=ALU.mult, op1=ALU.mult), r=[f'acc{c}', 'var', 'pv'], w=['mixedT'])

                        pes = [pe0, pe1]
                        pTs = [pT0, pT1]
                        for n in range(8):
                            tt = n + 1
                            for g in range(2):
                                gp = slice(g * 64, (g + 1) * 64)
                                for kt in range(2):
                                    kc0 = (tt - 1 + kt) * 128
                                    for hh in range(2):
                                        pi = 2 + hh
                                        t.op('pe', lambda e: e.matmul(ps[pi][:], lhsT=kT[gp, kc0:kc0 + 128],
                                                                      rhs=qT[gp, 4 * hh:4 * hh + 4, tt * 128:(tt + 1) * 128],
                                                                      start=True, stop=True), r=['kT', 'qT'], w=[P(pi)])
                                        t.op('act', lambda e: e.activation(out=pes[hh][:], in_=ps[pi][:], func=AF.Exp, scale=0.125),
                                             r=[P(pi)], w=[f'pe{hh}'])
                                        if kt == 0 and n == 0 and hf == 0:
                                            eb = ebt0[:, g * 1024 + hh * 512:g * 1024 + (hh + 1) * 512]
                                        else:
                                            o = kt * 2048 + g * 1024 + hh * 512
                                            eb = ebt[:, o:o + 512]
                                        t.op('dve', lambda e: e.tensor_tensor(out=pTs[kt][:, hh * 512:(hh + 1) * 512], in0=pes[hh][:], in1=eb,
                                                                              op=ALU.mult), r=[f'pe{hh}', 'ebt', 'ebt0'], w=[f'pT{kt}'])
                                for jj in range(8):
                                    pi = 4 + jj // 4
                                    oc = (jj % 4) * 65
                                    for kt in range(2):
                                        t.op('pe', lambda e: e.matmul(ps[pi][:, oc:oc + 65], lhsT=pTs[kt][:, jj * 128:(jj + 1) * 128],
                                                                      rhs=v1[:, tt - 1 + kt, g, :], start=(kt == 0), stop=(kt == 1)),
                                             r=[f'pT{kt}', 'v1'], w=[P(pi)])
                                for half in range(2):
                                    pi = 4 + half
                                    pv3 = ps[pi][:, 0:260].rearrange("p (h d) -> p h d", d=65)
                                    hs = 8 * g + 4 * half
                                    t.op('dve', lambda e: e.tensor_tensor(out=den[:, 0:4], in0=pv3[:, :, 64], in1=esink[:, hs:hs + 4], op=ALU.add),
                                         r=[P(pi), 'esink'], w=['den'])
                                    t.op('dve', lambda e: e.reciprocal(out=den[:, 0:4], in_=den[:, 0:4]), r=['den'], w=['den'])
                                    for j4 in range(4):
                                        h = hs + j4
                                        t.op('dve', lambda e: e.tensor_scalar(out=yatt[:, h * 64:(h + 1) * 64], in0=pv3[:, j4, 0:64],
                                                                              scalar1=den[:, j4:j4 + 1], scalar2=None, op0=ALU.mult),
                                             r=[P(pi), 'den'], w=['yatt'])
                            t.op('dve', lambda e: e.memset(ssq[:], 0.0), w=['ssq'])
                            t.op('act', lambda e: e.activation(out=junk2[:], in_=yatt[:], func=AF.Square, accum_out=ssq[:, 0:1]),
                                 r=['yatt', 'ssq'], w=['junk2', 'ssq'])
                            rsqrt_into(rstd[:], ssq[:], 1.0 / 1024, ['ssq'], ['rstd'])
                            t.op('dve', lambda e: e.scalar_tensor_tensor(out=ynb[:], in0=yatt[:], scalar=rstd[:, 0:1], in1=goa[:],
                                                                         op0=ALU.mult, op1=ALU.mult), r=['yatt', 'rstd', 'goa'], w=['ynb'])
                            for c in range(8):
                                t.op('pe', lambda e: e.transpose(out=psT[1][:, c * 128:(c + 1) * 128], in_=ynb[:, c * 128:(c + 1) * 128],
                                                                 identity=identb[:]), r=['ynb', 'identb'], w=['psT1'])
                            t.op('dve', lambda e: e.tensor_copy(out=mixedT[:, 0:8, n * 128:(n + 1) * 128],
                                                                in_=psT[1][:, :].rearrange("p (c q) -> p c q", c=8)), r=['psT1'], w=['mixedT'])
                        tap(f'mixedT{hf}', mixedT, [128, 16, 1024], BF16, 'mixedT')
                        t.barrier()
                        if STOP == 'mixer':
                            return nc


                with ExitStack() as _es:
                    wo = _es.enter_context(SBT("wo", [128, 16, D], BF16))
                    G1 = _es.enter_context(SBT("G1", [128, D], F32))
                    GB1 = _es.enter_context(SBT("GB1", [128, D], F32))
                    xt2 = _es.enter_context(SBT("xt2", [128, D], F32))
                    x1 = _es.enter_context(SBT("x1", [128, D], F32))
                    h2b = _es.enter_context(SBT("h2b", [128, D], BF16))
                    lg = _es.enter_context(SBT("lg", [128, 32], F32))
                    top8 = _es.enter_context(SBT("top8", [128, 8], F32))
                    msk = _es.enter_context(SBT("msk", [128, 32], F32))
                    sm = _es.enter_context(SBT("sm", [128, 2], F32))
                    t.dma('sp', lambda e: e.dma_start(out=G1[:], in_=g1s_d[0]), r=['g1sd'], w=['G1'])
                    t.dma('sp', lambda e: e.dma_start(out=GB1[:], in_=g1s_d[1]), r=['g1sd'], w=['GB1'])
                    for q in range(4):
                        t.dma('pool', lambda e: e.dma_start(out=wo[:, :, q * 512:(q + 1) * 512], in_=wout_v[:, :, q * 512:(q + 1) * 512]),
                              w=['wo'])
                    for lt in range(8):
                        gt = hf * 8 + lt
                        t.dma('sp', lambda e: e.dma_start(out=xt2[:], in_=xh_d[(gt + 1) * 128:(gt + 2) * 128, :]), w=['xt2'])
                        t.op('pool', lambda e: e.tensor_tensor(out=xt2[:], in0=xt2[:], in1=GB1[:], op=ALU.add), r=['xt2', 'GB1'], w=['xt2'])
                        for nq in range(4):
                            for cc in range(16):
                                t.op('pe', lambda e: e.matmul(ps[nq][:], lhsT=mixedT[:, cc, lt * 128:(lt + 1) * 128],
                                                              rhs=wo[:, cc, nq * 512:(nq + 1) * 512], start=(cc == 0), stop=(cc == 15)),
                                     r=['mixedT', 'wo'], w=[P(nq)])
                            ns = slice(nq * 512, (nq + 1) * 512)
                            t.op('dve', lambda e: e.tensor_tensor(out=x1[:, ns], in0=ps[nq][:], in1=G1[:, ns], op=ALU.mult),
                                 r=[P(nq), 'G1'], w=['x1'])
                        t.op('dve', lambda e: e.tensor_tensor(out=x1[:], in0=x1[:], in1=xt2[:], op=ALU.add), r=['x1', 'xt2'], w=['x1'])
                        t.dma('sp', lambda e: e.dma_start(out=x1_d[gt * 128:(gt + 1) * 128, :], in_=x1[:]), r=['x1'], w=['x1d'])
                        t.op('dve', lambda e: e.memset(ssq[:], 0.0), w=['ssq'])
                        t.op('act', lambda e: e.activation(out=h2b[:], in_=x1[:], func=AF.Square, accum_out=ssq[:, 0:1]),
                             r=['x1', 'ssq'], w=['h2b', 'ssq'])
                        rsqrt_into(rstd[:], ssq[:], 1.0 / D, ['ssq'], ['rstd'])
                        t.op('dve', lambda e: e.tensor_scalar(out=h2b[:], in0=x1[:], scalar1=rstd[:, 0:1], scalar2=None, op0=ALU.mult),
                             r=['x1', 'rstd'], w=['h2b'])
                        for cc in range(16):
                            t.op('pe', lambda e: e.transpose(out=psT[cc // 8][:, (cc % 8) * 128:(cc % 8 + 1) * 128],
                                                             in_=h2b[:, cc * 128:(cc + 1) * 128], identity=identb[:]),
                                 r=['h2b', 'identb'], w=[f'psT{cc // 8}'])
                        for cc in range(16):
                            pin = psT[cc // 8][:, (cc % 8) * 128:(cc % 8 + 1) * 128]
                            if cc % 2 == 0:
                                t.op('dve', lambda e: e.tensor_scalar(out=h2T[:, cc, lt * 128:(lt + 1) * 128], in0=pin,
                                                                      scalar1=A2[:, cc:cc + 1], scalar2=modT[:, 2, cc:cc + 1],
                                                                      op0=ALU.mult, op1=ALU.add),
                                     r=[f'psT{cc // 8}', 'A2', 'modT'], w=['h2T'])
                            else:
                                t.op('act', lambda e: e.activation(out=h2T[:, cc, lt * 128:(lt + 1) * 128], in_=pin, func=AF.Identity,
                                                                   bias=modT[:, 2, cc:cc + 1], scale=A2[:, cc:cc + 1]),
                                     r=[f'psT{cc // 8}', 'A2', 'modT'], w=['h2T'])
                        for cc in range(16):
                            t.op('pe', lambda e: e.matmul(ps[5][:, 0:32], lhsT=h2T[:, cc, lt * 128:(lt + 1) * 128],
                                                          rhs=wrb[:, cc * 32:(cc + 1) * 32],
                                                          start=(cc == 0), stop=(cc == 15)), r=['h2T', 'wrb'], w=[P(5)])
                        t.op('dve', lambda e: e.tensor_tensor(out=lg[:], in0=ps[5][:, 0:32], in1=brt[:], op=ALU.add), r=[P(5), 'brt'], w=['lg'])
                        t.op('dve', lambda e: e.max(out=top8[:], in_=lg[:]), r=['lg'], w=['top8'])
                        t.op('dve', lambda e: e.tensor_scalar(out=msk[:], in0=lg[:], scalar1=top8[:, 3:4], scalar2=None, op0=ALU.is_ge),
                             r=['lg', 'top8'], w=['msk'])
                        t.op('dve', lambda e: e.tensor_scalar(out=sm[:, 0:1], in0=top8[:, 0:1], scalar1=-1.0, scalar2=None, op0=ALU.mult),
                             r=['top8'], w=['sm'])
                        t.op('act', lambda e: e.activation(out=lg[:], in_=lg[:], func=AF.Exp, bias=sm[:, 0:1]), r=['lg', 'sm'], w=['lg'])
                        t.op('dve', lambda e: e.tensor_tensor(out=lg[:], in0=lg[:], in1=msk[:], op=ALU.mult), r=['lg', 'msk'], w=['lg'])
                        t.op('dve', lambda e: e.reduce_sum(out=sm[:, 1:2], in_=lg[:], axis=AX.X), r=['lg'], w=['sm'])
                        t.op('dve', lambda e: e.reciprocal(out=sm[:, 1:2], in_=sm[:, 1:2]), r=['sm'], w=['sm'])
                        t.op('dve', lambda e: e.tensor_scalar(out=wt[:, lt, :], in0=lg[:], scalar1=sm[:, 1:2], scalar2=None, op0=ALU.mult),
                             r=['lg', 'sm'], w=['wt'])
                    t.barrier()
                    if STOP == 'outproj':
                        return nc

            with ExitStack() as _es:
                yacc = _es.enter_context(SBT("yacc", [128, 8, D], F32))
                actT = _es.enter_context(SBT("actT", [128, 16, 1024], BF16))
                ring = _es.enter_context(SBT("ring", [128, 16384], BF16))
                wm = [ring[:, i * 4096:(i + 1) * 4096].rearrange("p (k f) -> p k f", f=256) for i in range(4)]
                wbig = [ring[:, j * 8192:(j + 1) * 8192].rearrange("p (k f) -> p k f", f=512) for j in range(2)]
                bdb0 = _es.enter_context(SBT("bdb0", [128, 512], F32))
                bdb1 = _es.enter_context(SBT("bdb1", [128, 512], F32))
                g1 = _es.enter_context(SBT("g1", [128, 512], F32))
                sg = _es.enter_context(SBT("sg", [128, 512], F32))
                u1 = _es.enter_context(SBT("u1", [128, 512], F32))
                bdb = [bdb0, bdb1]
                wmi = 0
                t.op('dve', lambda e: e.memset(yacc[:], 0.0), w=['yacc'])
                units = []
                for ex in range(NEXP):
                    for qf in range(8):
                        units.append(('A', ex, qf, wmi % 4, (wmi + 1) % 4))
                        wmi += 2
                    for dq in range(4):
                        units.append(('B', ex, dq, wmi % 4, dq % 2))
                        wmi += 2

                def issue(u):
                    kind, ex, idx, a, b = u
                    if kind == 'A':
                        t.dma('pool', lambda e: e.dma_start(out=wm[a], in_=wg_d[ex, idx]), w=[f'wm{a}'])
                        t.dma('pool', lambda e: e.dma_start(out=wm[b], in_=wu_d[ex, idx]), w=[f'wm{b}'])
                    else:
                        ds_ = slice(idx * 512, (idx + 1) * 512)
                        t.dma('pool', lambda e: e.dma_start(out=wbig[a // 2], in_=wd_d[ex, idx]), w=[f'wm{a}', f'wm{a + 1}'])
                        t.dma('sp', lambda e: e.dma_start(out=bdb[b][:], in_=bd_d[ex:ex + 1, ds_].partition_broadcast(128)),
                              w=[f'bdb{b}'])

                def compute(u):
                    kind, ex, idx, a, b = u
                    if kind == 'A':
                        qf, bg_, bu_ = idx, a, b
                        for ft in range(2):
                            f = qf * 2 + ft
                            bgc = pv[:, PV_BG + ex * 16 + f:PV_BG + ex * 16 + f + 1]
                            buc = pv[:, PV_BU + ex * 16 + f:PV_BU + ex * 16 + f + 1]
                            for ch in range(2):
                                cs = slice(ch * 512, (ch + 1) * 512)
                                pg, pu = ps[2 * ch], ps[2 * ch + 1]
                                kg, ku = P(2 * ch), P(2 * ch + 1)
                                for kc in range(16):
                                    t.op('pe', lambda e: e.matmul(pg[:], lhsT=wm[bg_][:, kc, ft * 128:(ft + 1) * 128], rhs=h2T[:, kc, cs],
                                                                  start=(kc == 0), stop=(kc == 15)), r=[f'wm{bg_}', 'h2T'], w=[kg])
                                for kc in range(16):
                                    t.op('pe', lambda e: e.matmul(pu[:], lhsT=wm[bu_][:, kc, ft * 128:(ft + 1) * 128], rhs=h2T[:, kc, cs],
                                                                  start=(kc == 0), stop=(kc == 15)), r=[f'wm{bu_}', 'h2T'], w=[ku])
                                t.op('dve', lambda e: e.tensor_scalar(out=g1[:], in0=pg[:], scalar1=bgc, scalar2=7.0, op0=ALU.add, op1=ALU.min),
                                     r=[kg, 'pv'], w=['g1'])
                                t.op('act', lambda e: e.activation(out=sg[:], in_=g1[:], func=AF.Sigmoid, scale=1.702), r=['g1'], w=['sg'])
                                t.op('dve', lambda e: e.tensor_scalar(out=u1[:], in0=pu[:], scalar1=buc, scalar2=7.0, op0=ALU.add, op1=ALU.min),
                                     r=[ku, 'pv'], w=['u1'])
                                t.op('dve', lambda e: e.tensor_scalar(out=u1[:], in0=u1[:], scalar1=-7.0, scalar2=1.0, op0=ALU.max, op1=ALU.add),
                                     r=['u1'], w=['u1'])
                                t.op('pool', lambda e: e.tensor_tensor(out=g1[:], in0=g1[:], in1=sg[:], op=ALU.mult), r=['g1', 'sg'], w=['g1'])
                                t.op('dve', lambda e: e.tensor_tensor(out=actT[:, f, cs], in0=g1[:], in1=u1[:], op=ALU.mult),
                                     r=['g1', 'u1'], w=['actT'])
                    else:
                        dq, bd_, bb = idx, a, b
                        ds_ = slice(dq * 512, (dq + 1) * 512)
                        wb_ = wbig[bd_ // 2]
                        for ti in range(8):
                            pd, kd = ps[4 + ti % 2], P(4 + ti % 2)
                            for fc in range(16):
                                t.op('pe', lambda e: e.matmul(pd[:], lhsT=actT[:, fc, ti * 128:(ti + 1) * 128], rhs=wb_[:, fc, :],
                                                              start=(fc == 0), stop=(fc == 15)), r=['actT', f'wm{bd_}', f'wm{bd_ + 1}'], w=[kd])
                            t.op('dve', lambda e: e.tensor_tensor(out=sg[:], in0=pd[:], in1=bdb[bb][:], op=ALU.add),
                                 r=[kd, f'bdb{bb}'], w=['sg'])
                            t.op('dve', lambda e: e.scalar_tensor_tensor(out=yacc[:, ti, ds_], in0=sg[:], scalar=wt[:, ti, ex:ex + 1],
                                                                         in1=yacc[:, ti, ds_], op0=ALU.mult, op1=ALU.add),
                                 r=['sg', 'wt', 'yacc'], w=['yacc'])

                issue(units[0])
                for ui, u in enumerate(units):
                    if ui + 1 < len(units):
                        issue(units[ui + 1])
                    compute(u)
                for ti in range(8):
                    gt = hf * 8 + ti
                    for q in range(4):
                        qs = slice(q * 512, (q + 1) * 512)
                        t.dma('sp', lambda e: e.dma_start(out=g1[:], in_=x1_d[gt * 128:(gt + 1) * 128, qs]), r=['x1d'], w=['g1'])
                        t.op('dve', lambda e: e.tensor_tensor(out=u1[:], in0=yacc[:, ti, qs], in1=G2[:, qs], op=ALU.mult),
                             r=['yacc', 'G2'], w=['u1'])
                        t.op('dve', lambda e: e.tensor_tensor(out=g1[:], in0=g1[:], in1=u1[:], op=ALU.add), r=['g1', 'u1'], w=['g1'])
                        t.dma('sp', lambda e: e.dma_start(out=out_d[gt * 128:(gt + 1) * 128, qs], in_=g1[:]), r=['g1'], w=['outd'])
                t.barrier()
    return nc


def _t5_bucket(dist):
    max_exact = 16
    d = np.maximum(dist, 0)
    lr = np.log(np.maximum(d, max_exact).astype(np.float32) / max_exact)
    large = max_exact + (lr / math.log(128 / max_exact) * (32 - max_exact)).astype(np.int32)
    large = np.minimum(large, 31)
    return np.where(d < max_exact, d, large)


def _prep(inputs):
    f = np.float32
    x = np.asarray(inputs["x"], f)[0]
    g = lambda k: np.asarray(inputs[k], f)[0]
    pp = lambda v: np.ascontiguousarray(v.reshape(-1, 128).T)
    perm = []
    for jj in range(8):
        perm += list(range(jj * 64, jj * 64 + 64)) + list(range((8 + jj) * 64, (8 + jj) * 64 + 64))
    perm += list(range(1024, 3328))
    perm = np.array(perm)
    win = np.ascontiguousarray(g("w_in")[:, perm])
    b_in = g("b_in")[perm]
    pv = np.zeros((128, NPV), f)
    pv[:, PV_C:PV_C + 16] = pp(np.asarray(inputs["c"], f)[0])
    pv[:, PV_G1:PV_G1 + 16] = pp(g("g_norm1"))
    pv[:, PV_BIN:PV_BIN + 26] = pp(b_in)
    pv[:, PV_GQ] = np.tile(g("g_q"), 2)
    pv[:, PV_GK] = np.tile(g("g_k"), 2)
    pv[:, PV_WDW:PV_WDW + 248] = g("w_dw").reshape(31, 8, 128).transpose(2, 1, 0).reshape(128, 248)
    pv[:, PV_BDW:PV_BDW + 8] = pp(g("b_dw"))
    pv[:, PV_LNG:PV_LNG + 8] = pp(g("ln_g"))
    pv[:, PV_LNB:PV_LNB + 8] = pp(g("ln_b"))
    pv[:, PV_GOC:PV_GOC + 8] = pp(g("g_out_conv"))
    pv[:, PV_BG:PV_BG + 512] = g("b_gate").reshape(32, 16, 128).transpose(2, 0, 1).reshape(128, 512)
    pv[:, PV_BU:PV_BU + 512] = g("b_up").reshape(32, 16, 128).transpose(2, 0, 1).reshape(128, 512)
    pv[:, PV_G2:PV_G2 + 16] = pp(g("g_norm2"))
    pv[:, PV_WR:PV_WR + 512] = g("w_router").reshape(16, 128, 32).transpose(1, 0, 2).reshape(128, 512)
    rv = np.zeros((1, NRV), f)
    rv[0, RV_GOA:RV_GOA + 1024] = g("g_out_attn")
    rv[0, RV_BOUT:RV_BOUT + D] = g("b_out")
    rv[0, RV_G2:RV_G2 + D] = g("g_norm2")
    rv[0, RV_BR:RV_BR + 32] = g("b_router")
    rv[0, RV_SINK:RV_SINK + 16] = g("sinks")
    rv = np.ascontiguousarray(np.broadcast_to(rv, (128, NRV)))
    rb = np.asarray(inputs["rel_bias"], f)
    kk = np.arange(128)[:, None]
    qq = np.arange(128)[None, :]
    bt = np.zeros((128, 2, 2, 8, 128), f)
    for kt in range(2):
        dist = qq + 128 - kk if kt == 0 else qq - kk
        valid = (dist >= 0) & (dist < 128)
        bk = _t5_bucket(dist)
        for gg in range(2):
            for jj in range(8):
                bt[:, kt, gg, jj, :] = np.where(valid, rb[bk, 8 * gg + jj], f(-30000.0))
    bt = bt.reshape(128, 4096)
    blk = np.zeros((128, 128), f)
    blk[:64, :64] = 1
    blk[64:, 64:] = 1
    def tile_w(w):
        return np.ascontiguousarray(w.reshape(E, 16, 128, 8, 256).transpose(0, 3, 2, 1, 4))

    common = dict(pv=pv, rv=rv, bada=g("b_ada")[None, :], wada=g("w_ada"), win=win, wout=g("w_out"), biast=bt,
                  wg=tile_w(g("w_gate")), wu=tile_w(g("w_up")), wd=np.ascontiguousarray(g("w_down").reshape(E, 16, 128, 4, 512).transpose(0, 3, 2, 1, 4)), bd=g("b_down"),
                  identf=np.eye(128, dtype=f), blk=blk)
    in_maps = []
    for c in range(NCORE):
        xh = np.zeros((17 * 128, D), f)
        if c == 0:
            xh[128:] = x[0:TOK]
        else:
            xh[:] = x[c * TOK - 128:(c + 1) * TOK]
        hvv = np.full((128, 1), 0.0 if c == 0 else 1.0, f)
        m = dict(common)
        m["xh"] = xh
        m["hv"] = hvv
        in_maps.append(m)
    return in_maps


def kernel(**inputs):
    in_maps = _prep(inputs)
    nc = build_nc()
    res = run_bass_kernel_spmd(nc, in_maps, core_ids=list(range(NCORE)))
    out = np.concatenate([np.asarray(r["out"], np.float32) for r in res.results], axis=0)
    return out.reshape(1, NCORE * TOK, D)
```

```python
import math
from contextlib import ExitStack
import numpy as np
import concourse.bass as bass
import concourse.mybir as mybir
from concourse.bass_utils import run_bass_kernel_spmd

F32 = mybir.dt.float32
BF16 = mybir.dt.bfloat16
ALU = mybir.AluOpType
AF = mybir.ActivationFunctionType
AX = mybir.AxisListType

D = 2048
NCORE = 8
TOK = 2048
NT = 16
E = 32
EPS = 1e-6
PV_C, PV_G1, PV_BIN, PV_GQ, PV_GK = 0, 16, 32, 58, 59
PV_WDW, PV_BDW, PV_LNG, PV_LNB, PV_GOC = 60, 308, 316, 324, 332
PV_BG, PV_BU, PV_WR = 340, 852, 1364
PV_G2 = 1364 + 512
NPV = 1364 + 512 + 16
RV_GOA, RV_BOUT, RV_G2, RV_BR, RV_SINK = 0, 1024, 3072, 5120, 5152
NRV = 5168


class Trk:
    def __init__(s, nc):
        s.nc = nc
        s.eng = dict(pe=nc.tensor, act=nc.scalar, dve=nc.vector, pool=nc.gpsimd, sp=nc.sync)
        s.csem = {e: nc.alloc_semaphore("c_" + e) for e in s.eng}
        s.ND = 24
        s.dsem = [nc.alloc_semaphore(f"dq{i}") for i in range(s.ND)]
        s.reset_state()

    def reset_state(s):
        s.cnt = {e: 0 for e in s.eng}
        s.seen = {c: {p: 0 for p in s.eng} for c in s.eng}
        s.dval = [0] * s.ND
        s.dseen = {c: [0] * s.ND for c in s.eng}
        s.drr = 0
        s.lastw = {}
        s.readers = {}

    def _wait(s, c, tok):
        if tok[0] == 'e':
            _, p, seq = tok
            if p == c and p == 'pe':
                return
            if s.seen[c][p] >= seq:
                return
            s.eng[c].wait_ge(s.csem[p], seq)
            s.seen[c][p] = seq
        else:
            _, k, val = tok
            if s.dseen[c][k] >= val:
                return
            s.eng[c].wait_ge(s.dsem[k], val)
            s.dseen[c][k] = val

    def _deps(s, c, r, w):
        for b in r:
            t = s.lastw.get(b)
            if t:
                s._wait(c, t)
        for b in w:
            t = s.lastw.get(b)
            if t:
                s._wait(c, t)
            rd = s.readers.get(b)
            if rd:
                for p, seq in rd[0].items():
                    if p != c:
                        s._wait(c, ('e', p, seq))
                for t in rd[1]:
                    s._wait(c, t)

    def _record(s, tok, r, w):
        for b in r:
            rd = s.readers.setdefault(b, [{}, []])
            if tok[0] == 'e':
                rd[0][tok[1]] = tok[2]
            else:
                rd[1].append(tok)
        for b in w:
            s.lastw[b] = tok
            s.readers[b] = [{}, []]

    def op(s, c, fn, r=(), w=()):
        s._deps(c, r, w)
        ins = fn(s.eng[c])
        s.cnt[c] += 1
        ins.then_inc(s.csem[c], 1)
        s._record(('e', c, s.cnt[c]), r, w)

    def dma(s, c, fn, r=(), w=()):
        s._deps(c, r, w)
        k = s.drr
        s.drr = (s.drr + 1) % s.ND
        if s.dval[k] > 0:
            s._wait(c, ('d', k, s.dval[k]))
        ins = fn(s.eng[c])
        s.dval[k] += 16
        ins.then_inc(s.dsem[k], 16)
        s._record(('d', k, s.dval[k]), r, w)

    def barrier(s):
        for c in s.eng:
            for p in s.eng:
                if s.cnt[p] > 0:
                    s._wait(c, ('e', p, s.cnt[p]))
            for k in range(s.ND):
                if s.dval[k] > 0:
                    s._wait(c, ('d', k, s.dval[k]))
        s.lastw = {}
        s.readers = {}


def build_nc(dbg=None):
    dbg = dbg or {}
    STOP = dbg.get('stop')
    HALVES = dbg.get('halves', [0, 1])
    GROUPS = dbg.get('groups', [0, 1])
    NEXP = dbg.get('n_exp', E)
    TAPS = dbg.get('taps', False)
    nc = bass.Bass("TRN2", target_bir_lowering=False)

    def din(name, shape, dt=F32):
        return nc.dram_tensor(name, list(shape), dt, kind="ExternalInput").ap()

    xh_d = din("xh", [17 * 128, D])
    hv_d = din("hv", [128, 1])
    pv_d = din("pv", [128, NPV])
    rv_d = din("rv", [128, NRV])
    bada_d = din("bada", [1, 6 * D])
    wada_d = din("wada", [D, 6 * D])
    win_d = din("win", [D, 3328])
    wout_d = din("wout", [D, D])
    bias_d = din("biast", [128, 4096])
    wg_d = din("wg", [NEXP, 8, 128, 16, 256])
    wu_d = din("wu", [NEXP, 8, 128, 16, 256])
    wd_d = din("wd", [NEXP, 4, 128, 16, 512])
    bd_d = din("bd", [NEXP, D])
    identf_d = din("identf", [128, 128])
    blk_d = din("blk", [128, 128])
    out_d = nc.dram_tensor("out", [TOK, D], F32, kind="ExternalOutput").ap()
    x1_d = nc.dram_tensor("x1s", [TOK, D], F32, kind=("ExternalOutput" if TAPS else "Internal")).ap()
    g1s_d = nc.dram_tensor("g1s", [2, 128, D], F32, kind="Internal").ap()

    t = Trk(nc)

    def tap(name, tens, shape, dt, key):
        if not TAPS:
            return
        dd = nc.dram_tensor("tap_" + name, list(shape), dt, kind="ExternalOutput").ap()
        t.dma('sp', lambda e: e.dma_start(out=dd, in_=tens[:]), r=[key], w=['tap_' + name])
    _uid = [0]

    def SB(name, shape, dt):
        _uid[0] += 1
        return nc.alloc_sbuf_tensor(f"{name}_s{_uid[0]}", shape, dt)

    def SBT(name, shape, dt):
        _uid[0] += 1
        return nc.sbuf_tensor(f"{name}_s{_uid[0]}", shape, dt)

    pv = SB("pv", [128, NPV], F32)
    hv = SB("hv", [128, 1], F32)
    identf = SB("identf", [128, 128], F32)
    identb = SB("identb", [128, 128], BF16)
    blkb = SB("blkb", [128, 128], BF16)
    onesf = SB("onesf", [128, 128], F32)
    sT = SB("sT", [128, 16], BF16)
    modT = SB("modT", [128, 4, 16], F32)
    A2 = SB("A2", [128, 16], F32)
    A1 = SB("A1", [128, 16], F32)
    G2 = SB("G2", [128, D], F32)
    goa = SB("goa", [128, 1024], F32)
    brt = SB("brt", [128, 32], F32)
    esink = SB("esink", [128, 16], F32)
    ebt = SB("ebt", [128, 4096], BF16)
    ebt0 = SB("ebt0", [128, 2048], BF16)
    wrb = SB("wrb", [128, 512], BF16)
    ssq = SB("ssq", [128, 1], F32)
    rstd = SB("rstd", [128, 1], F32)

    ps = [nc.alloc_psum_tensor(f"ps{i}", [128, 512], F32) for i in range(6)]
    psT = [nc.alloc_psum_tensor(f"psT{i}", [128, 1024], BF16) for i in range(2)]

    def P(i):
        return f"ps{i}"

    t.dma('sp', lambda e: e.dma_start(out=pv[:], in_=pv_d), w=['pv'])
    t.dma('sp', lambda e: e.dma_start(out=hv[:], in_=hv_d), w=['hv'])
    t.dma('sp', lambda e: e.dma_start(out=identf[:], in_=identf_d), w=['identf'])
    t.dma('pool', lambda e: e.dma_start(out=identb[:], in_=identf_d), w=['identb'])
    t.dma('pool', lambda e: e.dma_start(out=blkb[:], in_=blk_d), w=['blkb'])
    t.op('dve', lambda e: e.memset(onesf[:], 1.0), w=['onesf'])
    t.op('dve', lambda e: e.tensor_copy(out=wrb[:], in_=pv[:, PV_WR:PV_WR + 512]), r=['pv'], w=['wrb'])
    t.op('act', lambda e: e.activation(out=sT[:], in_=pv[:, PV_C:PV_C + 16], func=AF.Silu), r=['pv'], w=['sT'])

    with ExitStack() as _es:
        rvt = _es.enter_context(SBT("rvt", [128, NRV], F32))
        biasf = _es.enter_context(SBT("biasf", [128, 4096], F32))
        wr0 = _es.enter_context(SBT("wr0", [128, 16, 512], BF16))
        wr1 = _es.enter_context(SBT("wr1", [128, 16, 512], BF16))
        brow0 = _es.enter_context(SBT("brow0", [1, 512], F32))
        brow1 = _es.enter_context(SBT("brow1", [1, 512], F32))
        mrow = _es.enter_context(SBT("mrow", [1, 512], F32))
        bcg = _es.enter_context(SBT("bcg", [128, D], F32))
        gb1 = _es.enter_context(SBT("gb1", [128, D], F32))
        wr = [wr0, wr1]
        brow = [brow0, brow1]
        t.dma('sp', lambda e: e.dma_start(out=rvt[:], in_=rv_d), w=['rvt'])
        t.dma('sp', lambda e: e.dma_start(out=biasf[:], in_=bias_d), w=['biasf'])
        t.op('act', lambda e: e.activation(out=ebt[:], in_=biasf[:], func=AF.Exp), r=['biasf'], w=['ebt'])
        t.op('dve', lambda e: e.tensor_scalar(out=ebt0[:], in0=ebt[:, 0:2048], scalar1=hv[:, 0:1], scalar2=None,
                                              op0=ALU.mult), r=['ebt', 'hv'], w=['ebt0'])
        t.op('act', lambda e: e.activation(out=esink[:], in_=rvt[:, RV_SINK:RV_SINK + 16], func=AF.Exp),
             r=['rvt'], w=['esink'])
        t.op('dve', lambda e: e.tensor_copy(out=goa[:], in_=rvt[:, RV_GOA:RV_GOA + 1024]), r=['rvt'], w=['goa'])
        t.op('dve', lambda e: e.tensor_copy(out=brt[:], in_=rvt[:, RV_BR:RV_BR + 32]), r=['rvt'], w=['brt'])

        wada_v = wada_d.rearrange("(kc p) f -> p kc f", p=128)
        for n in range(24):
            b = n % 2
            t.dma('pool', lambda e: e.dma_start(out=wr[b][:], in_=wada_v[:, :, n * 512:(n + 1) * 512]), w=[f'wr{b}'])
            t.dma('sp', lambda e: e.dma_start(out=brow[b][:], in_=bada_d[0:1, n * 512:(n + 1) * 512]), w=[f'brow{b}'])
            for kc in range(16):
                t.op('pe', lambda e: e.matmul(ps[0][0:1, :], lhsT=sT[:, kc:kc + 1], rhs=wr[b][:, kc, :],
                                              start=(kc == 0), stop=(kc == 15)), r=['sT', f'wr{b}'], w=[P(0)])
            t.op('dve', lambda e: e.tensor_tensor(out=mrow[:], in0=ps[0][0:1, :], in1=brow[b][:], op=ALU.add),
                 r=[P(0), f'brow{b}'], w=['mrow'])
            which, q = n // 4, n % 4
            if which in (0, 1, 3, 4):
                mi = {0: 0, 1: 1, 3: 2, 4: 3}[which]
                for j in range(4):
                    t.op('pe', lambda e: e.matmul(ps[1][:, j:j + 1], lhsT=mrow[0:1, j * 128:(j + 1) * 128],
                                                  rhs=onesf[0:1, 0:1], start=True, stop=True), r=['mrow', 'onesf'], w=[P(1)])
                t.op('dve', lambda e: e.tensor_copy(out=modT[:, mi, q * 4:(q + 1) * 4], in_=ps[1][:, 0:4]),
                     r=[P(1)], w=['modT'])
            else:
                dstt, dk = (bcg, 'bcg') if which == 2 else (G2, 'G2')
                t.op('pe', lambda e: e.matmul(ps[2][:, :], lhsT=onesf[0:1, 0:128], rhs=mrow[0:1, :],
                                              start=True, stop=True), r=['mrow', 'onesf'], w=[P(2)])
                t.op('act', lambda e: e.activation(out=dstt[:, q * 512:(q + 1) * 512], in_=ps[2][:, :], func=AF.Identity),
                     r=[P(2)], w=[dk])
        t.op('dve', lambda e: e.scalar_tensor_tensor(out=A1[:], in0=modT[:, 1, :], scalar=1.0, in1=pv[:, PV_G1:PV_G1 + 16],
                                                     op0=ALU.add, op1=ALU.mult), r=['modT', 'pv'], w=['A1'])
        t.op('dve', lambda e: e.scalar_tensor_tensor(out=A2[:], in0=modT[:, 3, :], scalar=1.0, in1=pv[:, PV_G2:PV_G2 + 16],
                                                     op0=ALU.add, op1=ALU.mult), r=['modT', 'pv'], w=['A2'])
        t.op('dve', lambda e: e.tensor_tensor(out=gb1[:], in0=bcg[:], in1=rvt[:, RV_BOUT:RV_BOUT + D], op=ALU.mult),
             r=['bcg', 'rvt'], w=['gb1'])
        t.dma('sp', lambda e: e.dma_start(out=g1s_d[0], in_=bcg[:]), r=['bcg'], w=['g1sd'])
        t.dma('sp', lambda e: e.dma_start(out=g1s_d[1], in_=gb1[:]), r=['gb1'], w=['g1sd'])
        tap('modT', modT, [128, 4, 16], F32, 'modT')
        tap('A1', A1, [128, 16], F32, 'A1')
        tap('bc0', bcg, [128, D], F32, 'bcg')
        tap('ebt', ebt, [128, 4096], BF16, 'ebt')
        t.barrier()
        if STOP == 'ada':
            return nc

    win_v = win_d.rearrange("(kc p) f -> p kc f", p=128)
    wout_v = wout_d.rearrange("(kc p) f -> p kc f", p=128)

    def rsqrt_into(dst, src_ap, scale, r, w):
        t.op('act', lambda e: e.activation(out=dst, in_=src_ap, func=AF.Sqrt, bias=EPS, scale=scale), r=r, w=w)
        t.op('dve', lambda e: e.reciprocal(out=dst, in_=dst), r=w, w=w)

    for hf in HALVES:
        with ExitStack() as _esh:
            h2T = _esh.enter_context(SBT("h2T", [128, 16, 1024], BF16))
            wt = _esh.enter_context(SBT("wt", [128, 8, 32], F32))
            with ExitStack() as _esm:
                mixedT = _esm.enter_context(SBT("mixedT", [128, 16, 1024], BF16))
                with ExitStack() as _es:
                    qT = _es.enter_context(SBT("qT", [128, 8, 1152], BF16))
                    kT = _es.enter_context(SBT("kT", [128, 1152], BF16))
                    v1 = _es.enter_context(SBT("v1", [128, 9, 2, 65], BF16))
                    hglu = _es.enter_context(SBT("hglu", [128, 8, 1152], BF16))
                    t.op('pool', lambda e: e.memset(v1[:], 1.0), w=['v1'])
                    with ExitStack() as _es:
                        hT = _es.enter_context(SBT("hT", [128, 16, 1152], BF16))
                        xb0 = _es.enter_context(SBT("xb0", [128, D], F32))
                        xs = _es.enter_context(SBT("xs", [128, D], BF16))
                        wi0 = _es.enter_context(SBT("wi0", [128, 16, 256], BF16))
                        wi1 = _es.enter_context(SBT("wi1", [128, 16, 256], BF16))
                        tA = xb0[:, 0:512]
                        tB = xs[:, 0:512]
                        tC = xb0[:, 512:1024]
                        xb = [xb0, xb0]; junk = xs
                        wi = [wi0, wi1]
                        for tt in range(9):
                            xt = xb[tt % 2]
                            xk = 'xb0'
                            r0 = (hf * 8 + tt) * 128
                            t.dma('sp', lambda e: e.dma_start(out=xt[:], in_=xh_d[r0:r0 + 128, :]), w=[xk])
                            t.op('dve', lambda e: e.memset(ssq[:], 0.0), w=['ssq'])
                            t.op('act', lambda e: e.activation(out=junk[:], in_=xt[:], func=AF.Square, accum_out=ssq[:, 0:1]),
                                 r=[xk, 'ssq'], w=['xs', 'ssq'])
                            rsqrt_into(rstd[:], ssq[:], 1.0 / D, ['ssq'], ['rstd'])
                            t.op('dve', lambda e: e.tensor_scalar(out=xs[:], in0=xt[:], scalar1=rstd[:, 0:1], scalar2=None, op0=ALU.mult),
                                 r=[xk, 'rstd'], w=['xs'])
                            for c in range(16):
                                t.op('pe', lambda e: e.transpose(out=psT[c // 8][:, (c % 8) * 128:(c % 8 + 1) * 128],
                                                                 in_=xs[:, c * 128:(c + 1) * 128], identity=identb[:]),
                                     r=['xs', 'identb'], w=[f'psT{c // 8}'])
                            for c in range(16):
                                t.op('dve', lambda e: e.tensor_scalar(out=hT[:, c, tt * 128:(tt + 1) * 128],
                                                                      in0=psT[c // 8][:, (c % 8) * 128:(c % 8 + 1) * 128],
                                                                      scalar1=A1[:, c:c + 1], scalar2=modT[:, 0, c:c + 1],
                                                                      op0=ALU.mult, op1=ALU.add),
                                     r=[f'psT{c // 8}', 'A1', 'modT'], w=['hT'])
                        if STOP == 'norm':
                            tap(f'hT{hf}', hT, [128, 16, 1152], BF16, 'hT')
                            t.barrier()
                            return nc
                        t.barrier()
                        chunks = [(0, 128), (128, 512), (640, 512)]
                        for j in dbg.get('jlist', range(26)):
                            wc = j // 2
                            b = wc % 2
                            if j % 2 == 0 or 'jlist' in dbg:
                                t.dma('pool', lambda e: e.dma_start(out=wi[b][:], in_=win_v[:, :, wc * 256:wc * 256 + 256]),
                                      w=[f'wi{b}'])
                            jo = (j % 2) * 128
                            bias = pv[:, PV_BIN + j:PV_BIN + j + 1]
                            for ci, (c0, cn) in enumerate(chunks):
                                pb = ps[ci % 2]
                                for kc in range(0 if dbg.get('nomm') else 16):
                                    t.op('pe', lambda e: e.matmul(pb[:, 0:cn], lhsT=wi[b][:, kc, jo:jo + 128], rhs=hT[:, kc, c0:c0 + cn],
                                                                  start=(kc == 0), stop=(kc == 15)), r=[f'wi{b}', 'hT'], w=[P(ci % 2)])
                                if dbg.get('noevac'):
                                    continue
                                if j < 9:
                                    gcol = PV_GQ if j < 8 else PV_GK
                                    dst = qT[:, j, c0:c0 + cn] if j < 8 else kT[:, c0:c0 + cn]
                                    dk = 'qT' if j < 8 else 'kT'
                                    t.op('act', lambda e: e.activation(out=tB[:, 0:cn], in_=pb[:, 0:cn], func=AF.Square, bias=bias),
                                         r=[P(ci % 2), 'pv'], w=['tB'])
                                    t.op('act', lambda e: e.activation(out=tA[:, 0:cn], in_=pb[:, 0:cn], func=AF.Identity, bias=bias),
                                         r=[P(ci % 2), 'pv'], w=['tA'])
                                    t.op('pe', lambda e: e.matmul(ps[2][:, 0:cn], lhsT=blkb[:], rhs=tB[:, 0:cn], start=True, stop=True),
                                         r=['blkb', 'tB'], w=[P(2)])
                                    rsqrt_into(tC[:, 0:cn], ps[2][:, 0:cn], 1.0 / 64, [P(2)], ['tC'])
                                    t.op('dve', lambda e: e.scalar_tensor_tensor(out=dst, in0=tA[:, 0:cn], scalar=pv[:, gcol:gcol + 1],
                                                                                 in1=tC[:, 0:cn], op0=ALU.mult, op1=ALU.mult),
                                         r=['tA', 'tC', 'pv'], w=[dk])
                                elif j == 9:
                                    t.op('act', lambda e: e.activation(out=tB[:, 0:cn], in_=pb[:, 0:cn], func=AF.Identity, bias=bias),
                                         r=[P(ci % 2), 'pv'], w=['tB'])
                                    for s_ in range(cn // 128):
                                        tt = c0 // 128 + s_
                                        t.op('pe', lambda e: e.transpose(out=psT[0][:, 0:128], in_=tB[:, s_ * 128:(s_ + 1) * 128],
                                                                         identity=identb[:]), r=['tB', 'identb'], w=['psT0'])
                                        t.op('dve', lambda e: e.tensor_copy(out=v1[:, tt, :, 0:64],
                                                                            in_=psT[0][:, 0:128].rearrange("p (g d) -> p g d", g=2)),
                                             r=['psT0'], w=['v1'])
                                elif j < 18:
                                    c = j - 10
                                    t.op('dve', lambda e: e.tensor_scalar(out=hglu[:, c, c0:c0 + cn], in0=pb[:, 0:cn], scalar1=bias,
                                                                          scalar2=None, op0=ALU.add), r=[P(ci % 2), 'pv'], w=['hglu'])
                                else:
                                    c = j - 18
                                    t.op('act', lambda e: e.activation(out=tB[:, 0:cn], in_=pb[:, 0:cn], func=AF.Sigmoid, bias=bias),
                                         r=[P(ci % 2), 'pv'], w=['tB'])
                                    t.op('pool', lambda e: e.tensor_tensor(out=hglu[:, c, c0:c0 + cn], in0=hglu[:, c, c0:c0 + cn],
                                                                           in1=tB[:, 0:cn], op=ALU.mult), r=['tB', 'hglu'], w=['hglu'])
                        if hf == 0 and not dbg.get('nohv'):
                            for c in range(8):
                                t.op('pool', lambda e: e.tensor_scalar(out=hglu[:, c, 0:128], in0=hglu[:, c, 0:128], scalar1=hv[:, 0:1],
                                                                       scalar2=None, op0=ALU.mult), r=['hglu', 'hv'], w=['hglu'])
                        tap(f'hT{hf}', hT, [128, 16, 1152], BF16, 'hT')
                        tap(f'qT{hf}', qT, [128, 8, 1152], BF16, 'qT')
                        tap(f'kT{hf}', kT, [128, 1152], BF16, 'kT')
                        tap(f'v1{hf}', v1, [128, 9, 2, 65], BF16, 'v1')
                        tap(f'hglu{hf}', hglu, [128, 8, 1152], BF16, 'hglu')
                        t.barrier()
                        if STOP == 'inproj':
                            return nc

                    with ExitStack() as _es:
                        acc = _es.enter_context(SBT("acc", [128, 8, 1024], F32))
                        sq = _es.enter_context(SBT("sq", [128, 512], F32))
                        mean = _es.enter_context(SBT("mean", [128, 512], F32))
                        var = _es.enter_context(SBT("var", [128, 512], F32))
                        tmp = _es.enter_context(SBT("tmp", [128, 512], F32))
                        yatt = _es.enter_context(SBT("yatt", [128, 1024], F32))
                        ynb = _es.enter_context(SBT("ynb", [128, 1024], BF16))
                        pe0 = _es.enter_context(SBT("pe0", [128, 512], F32))
                        pe1 = _es.enter_context(SBT("pe1", [128, 512], F32))
                        pT0 = _es.enter_context(SBT("pT0", [128, 1024], BF16))
                        pT1 = _es.enter_context(SBT("pT1", [128, 1024], BF16))
                        den = _es.enter_context(SBT("den", [128, 8], F32))
                        junk2 = _es.enter_context(SBT("junk2", [128, 1024], BF16))
                        dg0 = _es.enter_context(SBT("dg0", [128, 31, 128], BF16))
                        dgs = [dg0, dg0]
                        for c in range(8):
                            ak = f'acc{c}'
                            wc0 = PV_WDW + c * 31
                            dg, dk = dgs[0], 'dg0'
                            for j in range(31):
                                t.op('dve', lambda e: e.tensor_scalar(out=dg[:, j, :], in0=identb[:], scalar1=pv[:, wc0 + j:wc0 + j + 1],
                                                                      scalar2=None, op0=ALU.mult), r=['identb', 'pv'], w=[dk])
                            for ch in range(2):
                                pi = 2 * (c % 2) + ch
                                for j in range(31):
                                    o = 98 + j + ch * 512
                                    t.op('pe', lambda e: e.matmul(ps[pi][:], lhsT=dg[:, j, :], rhs=hglu[:, c, o:o + 512],
                                                                  start=(j == 0), stop=(j == 30)), r=[dk, 'hglu'], w=[P(pi)])
                                t.op('act', lambda e: e.activation(out=acc[:, c, ch * 512:(ch + 1) * 512], in_=ps[pi][:], func=AF.Identity,
                                                                   bias=pv[:, PV_BDW + c:PV_BDW + c + 1]), r=[P(pi), 'pv'], w=[ak])
                        for ch in range(2):
                            cs = slice(ch * 512, (ch + 1) * 512)
                            for c in range(8):
                                t.op('act', lambda e: e.activation(out=sq[:], in_=acc[:, c, cs], func=AF.Square), r=[f'acc{c}'], w=['sq'])
                                t.op('pe', lambda e: e.matmul(ps[0][:], lhsT=onesf[:], rhs=acc[:, c, cs], start=(c == 0), stop=(c == 7)),
                                     r=['onesf', f'acc{c}'], w=[P(0)])
                                t.op('pe', lambda e: e.matmul(ps[1][:], lhsT=onesf[:], rhs=sq[:], start=(c == 0), stop=(c == 7)),
                                     r=['onesf', 'sq'], w=[P(1)])
                            t.op('dve', lambda e: e.tensor_scalar(out=mean[:], in0=ps[0][:], scalar1=1.0 / 1024, scalar2=None, op0=ALU.mult),
                                 r=[P(0)], w=['mean'])
                            t.op('dve', lambda e: e.tensor_tensor(out=tmp[:], in0=mean[:], in1=mean[:], op=ALU.mult), r=['mean'], w=['tmp'])
                            t.op('dve', lambda e: e.scalar_tensor_tensor(out=var[:], in0=ps[1][:], scalar=1.0 / 1024, in1=tmp[:],
                                                                         op0=ALU.mult, op1=ALU.subtract), r=[P(1), 'tmp'], w=['var'])
                            rsqrt_into(var[:], var[:], 1.0, ['var'], ['var'])
                            for c in range(8):
                                ak = f'acc{c}'
                                t.op('dve', lambda e: e.tensor_tensor(out=tmp[:], in0=acc[:, c, cs], in1=mean[:], op=ALU.subtract),
                                     r=[ak, 'mean'], w=['tmp'])
                                t.op('dve', lambda e: e.tensor_tensor(out=tmp[:], in0=tmp[:], in1=var[:], op=ALU.mult), r=['tmp', 'var'], w=['tmp'])
                                t.op('act', lambda e: e.activation(out=acc[:, c, cs], in_=tmp[:], func=AF.Silu,
                                                                   bias=pv[:, PV_LNB + c:PV_LNB + c + 1], scale=pv[:, PV_LNG + c:PV_LNG + c + 1]),
                                     r=['tmp', 'pv'], w=[ak])
                                t.op('act', lambda e: e.activation(out=sq[:], in_=acc[:, c, cs], func=AF.Square), r=[ak], w=['sq'])
                                t.op('pe', lambda e: e.matmul(ps[2][:], lhsT=onesf[:], rhs=sq[:], start=(c == 0), stop=(c == 7)),
                                     r=['onesf', 'sq'], w=[P(2)])
                            rsqrt_into(var[:], ps[2][:], 1.0 / 1024, [P(2)], ['var'])
                            for c in range(8):
                                t.op('dve', lambda e: e.scalar_tensor_tensor(out=mixedT[:, 8 + c, cs], in0=acc[:, c, cs],
                                                                             scalar=pv[:, PV_GOC + c:PV_GOC + c + 1], in1=var[:],
                                                                             op0=ALU.mult, op1=ALU.mult), r=[f'acc{c}', 'var', 'pv'], w=['mixedT'])

                        pes = [pe0, pe1]
                        pTs = [pT0, pT1]
                        for n in range(8):
                            tt = n + 1
                            for g in range(2):
                                gp = slice(g * 64, (g + 1) * 64)
                                for kt in range(2):
                                    kc0 = (tt - 1 + kt) * 128
                                    for hh in range(2):
                                        pi = 2 + hh
                                        t.op('pe', lambda e: e.matmul(ps[pi][:], lhsT=kT[gp, kc0:kc0 + 128],
                                                                      rhs=qT[gp, 4 * hh:4 * hh + 4, tt * 128:(tt + 1) * 128],
                                                                      start=True, stop=True), r=['kT', 'qT'], w=[P(pi)])
                                        t.op('act', lambda e: e.activation(out=pes[hh][:], in_=ps[pi][:], func=AF.Exp, scale=0.125),
                                             r=[P(pi)], w=[f'pe{hh}'])
                                        if kt == 0 and n == 0 and hf == 0:
                                            eb = ebt0[:, g * 1024 + hh * 512:g * 1024 + (hh + 1) * 512]
                                        else:
                                            o = kt * 2048 + g * 1024 + hh * 512
                                            eb = ebt[:, o:o + 512]
                                        t.op('dve', lambda e: e.tensor_tensor(out=pTs[kt][:, hh * 512:(hh + 1) * 512], in0=pes[hh][:], in1=eb,
                                                                              op=ALU.mult), r=[f'pe{hh}', 'ebt', 'ebt0'], w=[f'pT{kt}'])
                                for jj in range(8):
                                    pi = 4 + jj // 4
                                    oc = (jj % 4) * 65
                                    for kt in range(2):
                                        t.op('pe', lambda e: e.matmul(ps[pi][:, oc:oc + 65], lhsT=pTs[kt][:, jj * 128:(jj + 1) * 128],
                                                                      rhs=v1[:, tt - 1 + kt, g, :], start=(kt == 0), stop=(kt == 1)),
                                             r=[f'pT{kt}', 'v1'], w=[P(pi)])
                                for half in range(2):
                                    pi = 4 + half
                                    pv3 = ps[pi][:, 0:260].rearrange("p (h d) -> p h d", d=65)
                                    hs = 8 * g + 4 * half
                                    t.op('dve', lambda e: e.tensor_tensor(out=den[:, 0:4], in0=pv3[:, :, 64], in1=esink[:, hs:hs + 4], op=ALU.add),
                                         r=[P(pi), 'esink'], w=['den'])
                                    t.op('dve', lambda e: e.reciprocal(out=den[:, 0:4], in_=den[:, 0:4]), r=['den'], w=['den'])
                                    for j4 in range(4):
                                        h = hs + j4
                                        t.op('dve', lambda e: e.tensor_scalar(out=yatt[:, h * 64:(h + 1) * 64], in0=pv3[:, j4, 0:64],
                                                                              scalar1=den[:, j4:j4 + 1], scalar2=None, op0=ALU.mult),
                                             r=[P(pi), 'den'], w=['yatt'])
                            t.op('dve', lambda e: e.memset(ssq[:], 0.0), w=['ssq'])
                            t.op('act', lambda e: e.activation(out=junk2[:], in_=yatt[:], func=AF.Square, accum_out=ssq[:, 0:1]),
                                 r=['yatt', 'ssq'], w=['junk2', 'ssq'])
                            rsqrt_into(rstd[:], ssq[:], 1.0 / 1024, ['ssq'], ['rstd'])
                            t.op('dve', lambda e: e.scalar_tensor_tensor(out=ynb[:], in0=yatt[:], scalar=rstd[:, 0:1], in1=goa[:],
                                                                         op0=ALU.mult, op1=ALU.mult), r=['yatt', 'rstd', 'goa'], w=['ynb'])
                            for c in range(8):
                                t.op('pe', lambda e: e.transpose(out=psT[1][:, c * 128:(c + 1) * 128], in_=ynb[:, c * 128:(c + 1) * 128],
                                                                 identity=identb[:]), r=['ynb', 'identb'], w=['psT1'])
                            t.op('dve', lambda e: e.tensor_copy(out=mixedT[:, 0:8, n * 128:(n + 1) * 128],
                                                                in_=psT[1][:, :].rearrange("p (c q) -> p c q", c=8)), r=['psT1'], w=['mixedT'])
                        tap(f'mixedT{hf}', mixedT, [128, 16, 1024], BF16, 'mixedT')
                        t.barrier()
                        if STOP == 'mixer':
                            return nc


                with ExitStack() as _es:
                    wo = _es.enter_context(SBT("wo", [128, 16, D], BF16))
                    G1 = _es.enter_context(SBT("G1", [128, D], F32))
                    GB1 = _es.enter_context(SBT("GB1", [128, D], F32))
                    xt2 = _es.enter_context(SBT("xt2", [128, D], F32))
                    x1 = _es.enter_context(SBT("x1", [128, D], F32))
                    h2b = _es.enter_context(SBT("h2b", [128, D], BF16))
                    lg = _es.enter_context(SBT("lg", [128, 32], F32))
                    top8 = _es.enter_context(SBT("top8", [128, 8], F32))
                    msk = _es.enter_context(SBT("msk", [128, 32], F32))
                    sm = _es.enter_context(SBT("sm", [128, 2], F32))
                    t.dma('sp', lambda e: e.dma_start(out=G1[:], in_=g1s_d[0]), r=['g1sd'], w=['G1'])
                    t.dma('sp', lambda e: e.dma_start(out=GB1[:], in_=g1s_d[1]), r=['g1sd'], w=['GB1'])
                    for q in range(4):
                        t.dma('pool', lambda e: e.dma_start(out=wo[:, :, q * 512:(q + 1) * 512], in_=wout_v[:, :, q * 512:(q + 1) * 512]),
                              w=['wo'])
                    for lt in range(8):
                        gt = hf * 8 + lt
                        t.dma('sp', lambda e: e.dma_start(out=xt2[:], in_=xh_d[(gt + 1) * 128:(gt + 2) * 128, :]), w=['xt2'])
                        t.op('pool', lambda e: e.tensor_tensor(out=xt2[:], in0=xt2[:], in1=GB1[:], op=ALU.add), r=['xt2', 'GB1'], w=['xt2'])
                        for nq in range(4):
                            for cc in range(16):
                                t.op('pe', lambda e: e.matmul(ps[nq][:], lhsT=mixedT[:, cc, lt * 128:(lt + 1) * 128],
                                                              rhs=wo[:, cc, nq * 512:(nq + 1) * 512], start=(cc == 0), stop=(cc == 15)),
                                     r=['mixedT', 'wo'], w=[P(nq)])
                            ns = slice(nq * 512, (nq + 1) * 512)
                            t.op('dve', lambda e: e.tensor_tensor(out=x1[:, ns], in0=ps[nq][:], in1=G1[:, ns], op=ALU.mult),
                                 r=[P(nq), 'G1'], w=['x1'])
                        t.op('dve', lambda e: e.tensor_tensor(out=x1[:], in0=x1[:], in1=xt2[:], op=ALU.add), r=['x1', 'xt2'], w=['x1'])
                        t.dma('sp', lambda e: e.dma_start(out=x1_d[gt * 128:(gt + 1) * 128, :], in_=x1[:]), r=['x1'], w=['x1d'])
                        t.op('dve', lambda e: e.memset(ssq[:], 0.0), w=['ssq'])
                        t.op('act', lambda e: e.activation(out=h2b[:], in_=x1[:], func=AF.Square, accum_out=ssq[:, 0:1]),
                             r=['x1', 'ssq'], w=['h2b', 'ssq'])
                        rsqrt_into(rstd[:], ssq[:], 1.0 / D, ['ssq'], ['rstd'])
                        t.op('dve', lambda e: e.tensor_scalar(out=h2b[:], in0=x1[:], scalar1=rstd[:, 0:1], scalar2=None, op0=ALU.mult),
                             r=['x1', 'rstd'], w=['h2b'])
                        for cc in range(16):
                            t.op('pe', lambda e: e.transpose(out=psT[cc // 8][:, (cc % 8) * 128:(cc % 8 + 1) * 128],
                                                             in_=h2b[:, cc * 128:(cc + 1) * 128], identity=identb[:]),
                                 r=['h2b', 'identb'], w=[f'psT{cc // 8}'])
                        for cc in range(16):
                            pin = psT[cc // 8][:, (cc % 8) * 128:(cc % 8 + 1) * 128]
                            if cc % 2 == 0:
                                t.op('dve', lambda e: e.tensor_scalar(out=h2T[:, cc, lt * 128:(lt + 1) * 128], in0=pin,
                                                                      scalar1=A2[:, cc:cc + 1], scalar2=modT[:, 2, cc:cc + 1],
                                                                      op0=ALU.mult, op1=ALU.add),
                                     r=[f'psT{cc // 8}', 'A2', 'modT'], w=['h2T'])
                            else:
                                t.op('act', lambda e: e.activation(out=h2T[:, cc, lt * 128:(lt + 1) * 128], in_=pin, func=AF.Identity,
                                                                   bias=modT[:, 2, cc:cc + 1], scale=A2[:, cc:cc + 1]),
                                     r=[f'psT{cc // 8}', 'A2', 'modT'], w=['h2T'])
                        for cc in range(16):
                            t.op('pe', lambda e: e.matmul(ps[5][:, 0:32], lhsT=h2T[:, cc, lt * 128:(lt + 1) * 128],
                                                          rhs=wrb[:, cc * 32:(cc + 1) * 32],
                                                          start=(cc == 0), stop=(cc == 15)), r=['h2T', 'wrb'], w=[P(5)])
                        t.op('dve', lambda e: e.tensor_tensor(out=lg[:], in0=ps[5][:, 0:32], in1=brt[:], op=ALU.add), r=[P(5), 'brt'], w=['lg'])
                        t.op('dve', lambda e: e.max(out=top8[:], in_=lg[:]), r=['lg'], w=['top8'])
                        t.op('dve', lambda e: e.tensor_scalar(out=msk[:], in0=lg[:], scalar1=top8[:, 3:4], scalar2=None, op0=ALU.is_ge),
                             r=['lg', 'top8'], w=['msk'])
                        t.op('dve', lambda e: e.tensor_scalar(out=sm[:, 0:1], in0=top8[:, 0:1], scalar1=-1.0, scalar2=None, op0=ALU.mult),
                             r=['top8'], w=['sm'])
                        t.op('act', lambda e: e.activation(out=lg[:], in_=lg[:], func=AF.Exp, bias=sm[:, 0:1]), r=['lg', 'sm'], w=['lg'])
                        t.op('dve', lambda e: e.tensor_tensor(out=lg[:], in0=lg[:], in1=msk[:], op=ALU.mult), r=['lg', 'msk'], w=['lg'])
                        t.op('dve', lambda e: e.reduce_sum(out=sm[:, 1:2], in_=lg[:], axis=AX.X), r=['lg'], w=['sm'])
                        t.op('dve', lambda e: e.reciprocal(out=sm[:, 1:2], in_=sm[:, 1:2]), r=['sm'], w=['sm'])
                        t.op('dve', lambda e: e.tensor_scalar(out=wt[:, lt, :], in0=lg[:], scalar1=sm[:, 1:2], scalar2=None, op0=ALU.mult),
                             r=['lg', 'sm'], w=['wt'])
                    t.barrier()
                    if STOP == 'outproj':
                        return nc

            with ExitStack() as _es:
                yacc = _es.enter_context(SBT("yacc", [128, 8, D], F32))
                actT = _es.enter_context(SBT("actT", [128, 16, 1024], BF16))
                ring = _es.enter_context(SBT("ring", [128, 16384], BF16))
                wm = [ring[:, i * 4096:(i + 1) * 4096].rearrange("p (k f) -> p k f", f=256) for i in range(4)]
                wbig = [ring[:, j * 8192:(j + 1) * 8192].rearrange("p (k f) -> p k f", f=512) for j in range(2)]
                bdb0 = _es.enter_context(SBT("bdb0", [128, 512], F32))
                bdb1 = _es.enter_context(SBT("bdb1", [128, 512], F32))
                g1 = _es.enter_context(SBT("g1", [128, 512], F32))
                sg = _es.enter_context(SBT("sg", [128, 512], F32))
                u1 = _es.enter_context(SBT("u1", [128, 512], F32))
                bdb = [bdb0, bdb1]
                wmi = 0
                t.op('dve', lambda e: e.memset(yacc[:], 0.0), w=['yacc'])
                units = []
                for ex in range(NEXP):
                    for qf in range(8):
                        units.append(('A', ex, qf, wmi % 4, (wmi + 1) % 4))
                        wmi += 2
                    for dq in range(4):
                        units.append(('B', ex, dq, wmi % 4, dq % 2))
                        wmi += 2

                def issue(u):
                    kind, ex, idx, a, b = u
                    if kind == 'A':
                        t.dma('pool', lambda e: e.dma_start(out=wm[a], in_=wg_d[ex, idx]), w=[f'wm{a}'])
                        t.dma('pool', lambda e: e.dma_start(out=wm[b], in_=wu_d[ex, idx]), w=[f'wm{b}'])
                    else:
                        ds_ = slice(idx * 512, (idx + 1) * 512)
                        t.dma('pool', lambda e: e.dma_start(out=wbig[a // 2], in_=wd_d[ex, idx]), w=[f'wm{a}', f'wm{a + 1}'])
                        t.dma('sp', lambda e: e.dma_start(out=bdb[b][:], in_=bd_d[ex:ex + 1, ds_].partition_broadcast(128)),
                              w=[f'bdb{b}'])

                def compute(u):
                    kind, ex, idx, a, b = u
                    if kind == 'A':
                        qf, bg_, bu_ = idx, a, b
                        for ft in range(2):
                            f = qf * 2 + ft
                            bgc = pv[:, PV_BG + ex * 16 + f:PV_BG + ex * 16 + f + 1]
                            buc = pv[:, PV_BU + ex * 16 + f:PV_BU + ex * 16 + f + 1]
                            for ch in range(2):
                                cs = slice(ch * 512, (ch + 1) * 512)
                                pg, pu = ps[2 * ch], ps[2 * ch + 1]
                                kg, ku = P(2 * ch), P(2 * ch + 1)
                                for kc in range(16):
                                    t.op('pe', lambda e: e.matmul(pg[:], lhsT=wm[bg_][:, kc, ft * 128:(ft + 1) * 128], rhs=h2T[:, kc, cs],
                                                                  start=(kc == 0), stop=(kc == 15)), r=[f'wm{bg_}', 'h2T'], w=[kg])
                                for kc in range(16):
                                    t.op('pe', lambda e: e.matmul(pu[:], lhsT=wm[bu_][:, kc, ft * 128:(ft + 1) * 128], rhs=h2T[:, kc, cs],
                                                                  start=(kc == 0), stop=(kc == 15)), r=[f'wm{bu_}', 'h2T'], w=[ku])
                                t.op('dve', lambda e: e.tensor_scalar(out=g1[:], in0=pg[:], scalar1=bgc, scalar2=7.0, op0=ALU.add, op1=ALU.min),
                                     r=[kg, 'pv'], w=['g1'])
                                t.op('act', lambda e: e.activation(out=sg[:], in_=g1[:], func=AF.Sigmoid, scale=1.702), r=['g1'], w=['sg'])
                                t.op('dve', lambda e: e.tensor_scalar(out=u1[:], in0=pu[:], scalar1=buc, scalar2=7.0, op0=ALU.add, op1=ALU.min),
                                     r=[ku, 'pv'], w=['u1'])
                                t.op('dve', lambda e: e.tensor_scalar(out=u1[:], in0=u1[:], scalar1=-7.0, scalar2=1.0, op0=ALU.max, op1=ALU.add),
                                     r=['u1'], w=['u1'])
                                t.op('pool', lambda e: e.tensor_tensor(out=g1[:], in0=g1[:], in1=sg[:], op=ALU.mult), r=['g1', 'sg'], w=['g1'])
                                t.op('dve', lambda e: e.tensor_tensor(out=actT[:, f, cs], in0=g1[:], in1=u1[:], op=ALU.mult),
                                     r=['g1', 'u1'], w=['actT'])
                    else:
                        dq, bd_, bb = idx, a, b
                        ds_ = slice(dq * 512, (dq + 1) * 512)
                        wb_ = wbig[bd_ // 2]
                        for ti in range(8):
                            pd, kd = ps[4 + ti % 2], P(4 + ti % 2)
                            for fc in range(16):
                                t.op('pe', lambda e: e.matmul(pd[:], lhsT=actT[:, fc, ti * 128:(ti + 1) * 128], rhs=wb_[:, fc, :],
                                                              start=(fc == 0), stop=(fc == 15)), r=['actT', f'wm{bd_}', f'wm{bd_ + 1}'], w=[kd])
                            t.op('dve', lambda e: e.tensor_tensor(out=sg[:], in0=pd[:], in1=bdb[bb][:], op=ALU.add),
                                 r=[kd, f'bdb{bb}'], w=['sg'])
                            t.op('dve', lambda e: e.scalar_tensor_tensor(out=yacc[:, ti, ds_], in0=sg[:], scalar=wt[:, ti, ex:ex + 1],
                                                                         in1=yacc[:, ti, ds_], op0=ALU.mult, op1=ALU.add),
                                 r=['sg', 'wt', 'yacc'], w=['yacc'])

                issue(units[0])
                for ui, u in enumerate(units):
                    if ui + 1 < len(units):
                        issue(units[ui + 1])
                    compute(u)
                for ti in range(8):
                    gt = hf * 8 + ti
                    for q in range(4):
                        qs = slice(q * 512, (q + 1) * 512)
                        t.dma('sp', lambda e: e.dma_start(out=g1[:], in_=x1_d[gt * 128:(gt + 1) * 128, qs]), r=['x1d'], w=['g1'])
                        t.op('dve', lambda e: e.tensor_tensor(out=u1[:], in0=yacc[:, ti, qs], in1=G2[:, qs], op=ALU.mult),
                             r=['yacc', 'G2'], w=['u1'])
                        t.op('dve', lambda e: e.tensor_tensor(out=g1[:], in0=g1[:], in1=u1[:], op=ALU.add), r=['g1', 'u1'], w=['g1'])
                        t.dma('sp', lambda e: e.dma_start(out=out_d[gt * 128:(gt + 1) * 128, qs], in_=g1[:]), r=['g1'], w=['outd'])
                t.barrier()
    return nc


def _t5_bucket(dist):
    max_exact = 16
    d = np.maximum(dist, 0)
    lr = np.log(np.maximum(d, max_exact).astype(np.float32) / max_exact)
    large = max_exact + (lr / math.log(128 / max_exact) * (32 - max_exact)).astype(np.int32)
    large = np.minimum(large, 31)
    return np.where(d < max_exact, d, large)


def _prep(inputs):
    f = np.float32
    x = np.asarray(inputs["x"], f)[0]
    g = lambda k: np.asarray(inputs[k], f)[0]
    pp = lambda v: np.ascontiguousarray(v.reshape(-1, 128).T)
    perm = []
    for jj in range(8):
        perm += list(range(jj * 64, jj * 64 + 64)) + list(range((8 + jj) * 64, (8 + jj) * 64 + 64))
    perm += list(range(1024, 3328))
    perm = np.array(perm)
    win = np.ascontiguousarray(g("w_in")[:, perm])
    b_in = g("b_in")[perm]
    pv = np.zeros((128, NPV), f)
    pv[:, PV_C:PV_C + 16] = pp(np.asarray(inputs["c"], f)[0])
    pv[:, PV_G1:PV_G1 + 16] = pp(g("g_norm1"))
    pv[:, PV_BIN:PV_BIN + 26] = pp(b_in)
    pv[:, PV_GQ] = np.tile(g("g_q"), 2)
    pv[:, PV_GK] = np.tile(g("g_k"), 2)
    pv[:, PV_WDW:PV_WDW + 248] = g("w_dw").reshape(31, 8, 128).transpose(2, 1, 0).reshape(128, 248)
    pv[:, PV_BDW:PV_BDW + 8] = pp(g("b_dw"))
    pv[:, PV_LNG:PV_LNG + 8] = pp(g("ln_g"))
    pv[:, PV_LNB:PV_LNB + 8] = pp(g("ln_b"))
    pv[:, PV_GOC:PV_GOC + 8] = pp(g("g_out_conv"))
    pv[:, PV_BG:PV_BG + 512] = g("b_gate").reshape(32, 16, 128).transpose(2, 0, 1).reshape(128, 512)
    pv[:, PV_BU:PV_BU + 512] = g("b_up").reshape(32, 16, 128).transpose(2, 0, 1).reshape(128, 512)
    pv[:, PV_G2:PV_G2 + 16] = pp(g("g_norm2"))
    pv[:, PV_WR:PV_WR + 512] = g("w_router").reshape(16, 128, 32).transpose(1, 0, 2).reshape(128, 512)
    rv = np.zeros((1, NRV), f)
    rv[0, RV_GOA:RV_GOA + 1024] = g("g_out_attn")
    rv[0, RV_BOUT:RV_BOUT + D] = g("b_out")
    rv[0, RV_G2:RV_G2 + D] = g("g_norm2")
    rv[0, RV_BR:RV_BR + 32] = g("b_router")
    rv[0, RV_SINK:RV_SINK + 16] = g("sinks")
    rv = np.ascontiguousarray(np.broadcast_to(rv, (128, NRV)))
    rb = np.asarray(inputs["rel_bias"], f)
    kk = np.arange(128)[:, None]
    qq = np.arange(128)[None, :]
    bt = np.zeros((128, 2, 2, 8, 128), f)
    for kt in range(2):
        dist = qq + 128 - kk if kt == 0 else qq - kk
        valid = (dist >= 0) & (dist < 128)
        bk = _t5_bucket(dist)
        for gg in range(2):
            for jj in range(8):
                bt[:, kt, gg, jj, :] = np.where(valid, rb[bk, 8 * gg + jj], f(-30000.0))
    bt = bt.reshape(128, 4096)
    blk = np.zeros((128, 128), f)
    blk[:64, :64] = 1
    blk[64:, 64:] = 1
    def tile_w(w):
        return np.ascontiguousarray(w.reshape(E, 16, 128, 8, 256).transpose(0, 3, 2, 1, 4))

    common = dict(pv=pv, rv=rv, bada=g("b_ada")[None, :], wada=g("w_ada"), win=win, wout=g("w_out"), biast=bt,
                  wg=tile_w(g("w_gate")), wu=tile_w(g("w_up")), wd=np.ascontiguousarray(g("w_down").reshape(E, 16, 128, 4, 512).transpose(0, 3, 2, 1, 4)), bd=g("b_down"),
                  identf=np.eye(128, dtype=f), blk=blk)
    in_maps = []
    for c in range(NCORE):
        xh = np.zeros((17 * 128, D), f)
        if c == 0:
            xh[128:] = x[0:TOK]
        else:
            xh[:] = x[c * TOK - 128:(c + 1) * TOK]
        hvv = np.full((128, 1), 0.0 if c == 0 else 1.0, f)
        m = dict(common)
        m["xh"] = xh
        m["hv"] = hvv
        in_maps.append(m)
    return in_maps


def kernel(**inputs):
    in_maps = _prep(inputs)
    nc = build_nc()
    res = run_bass_kernel_spmd(nc, in_maps, core_ids=list(range(NCORE)))
    out = np.concatenate([np.asarray(r["out"], np.float32) for r in res.results], axis=0)
    return out.reshape(1, NCORE * TOK, D)
```
